# Optimizing a Trainium2 kernel written in Bass

```python
import jax, jax.numpy as jnp
from jax import lax
import numpy as np

D_MODEL = 2048
BATCH = 4
SEQ = 4096
DEPTH = 2

N_MIXERS = 4
GROUP_WIDTH = D_MODEL // N_MIXERS
GDN_HEADS = 4
GDN_HEAD_DIM = GROUP_WIDTH // GDN_HEADS
MLSTM_HEADS = 4
MLSTM_HEAD_DIM = GROUP_WIDTH // MLSTM_HEADS
RGLRU_BLOCKS = 4
RGLRU_BLOCK_DIM = GROUP_WIDTH // RGLRU_BLOCKS
RGLRU_C = 8.0
RWKV_HEAD_DIM = 64
RWKV_HEADS = GROUP_WIDTH // RWKV_HEAD_DIM
RWKV_DECAY_LORA = 64
RWKV_A_LORA = 64
RWKV_GATE_LORA = 128
CONV_WIDTH = 4
CHUNK = 64
FFN_HIDDEN = -(-8 * D_MODEL // (3 * 256)) * 256
NORM_EPS = 1e-6
RWKV_LN_EPS = 64e-5

GDN_COLS = 4 * GROUP_WIDTH + 2 * GDN_HEADS
MLSTM_COLS = 4 * GROUP_WIDTH + 2 * MLSTM_HEADS
RGLRU_COLS = 2 * GROUP_WIDTH
RWKV_COLS = 3 * GROUP_WIDTH + RWKV_DECAY_LORA + RWKV_A_LORA + RWKV_GATE_LORA
IN_COLS = GDN_COLS + MLSTM_COLS + RGLRU_COLS + RWKV_COLS
MIXER_SPLITS = [GDN_COLS, GDN_COLS + MLSTM_COLS, GDN_COLS + MLSTM_COLS + RGLRU_COLS]

kernel_name = 'hybrid_parallel_heads_gdn_mlstm_rglru_rwkv7'


def rmsnorm(x, w, eps=NORM_EPS):
    xf = x.astype(jnp.float32)
    y = xf * lax.rsqrt(jnp.mean(xf * xf, axis=-1, keepdims=True) + eps)
    return (y * w.astype(jnp.float32)).astype(x.dtype)


def l2norm(x, eps=1e-6):
    xf = x.astype(jnp.float32)
    return xf * lax.rsqrt(jnp.sum(xf * xf, axis=-1, keepdims=True) + eps)


def head_layernorm(y, w, b, eps):
    mu = jnp.mean(y, axis=-1, keepdims=True)
    var = jnp.mean(jnp.square(y - mu), axis=-1, keepdims=True)
    return (y - mu) * lax.rsqrt(var + eps) * w + b


def causal_conv(x, w):
    return lax.conv_general_dilated(
        x, w[:, None, :].astype(x.dtype), window_strides=(1,), padding=[(w.shape[0] - 1, 0)],
        dimension_numbers=('NWC', 'WIO', 'NWC'), feature_group_count=x.shape[-1])


def heads(t, h):
    return t.reshape(t.shape[:-1] + (h, t.shape[-1] // h))


def to_chunks(t):
    b, T, h = t.shape[:3]
    t = t.reshape((b, T // CHUNK, CHUNK, h) + t.shape[3:])
    return jnp.transpose(t, (1, 0, 3, 2) + tuple(range(4, t.ndim)))


def from_chunks(t):
    nc, b, h, c, d = t.shape
    return jnp.transpose(t, (1, 0, 3, 2, 4)).reshape(b, nc * c, h, d)


def gated_delta_rule(q, k, v, g, beta):
    q, k, v = [to_chunks(t.astype(jnp.float32)) for t in (q, k, v)]
    g, beta = [to_chunks(t.astype(jnp.float32)) for t in (g, beta)]
    g = jnp.cumsum(g, axis=-1)
    causal = jnp.tril(jnp.ones((CHUNK, CHUNK), dtype=bool))
    decay = jnp.exp(jnp.where(causal, g[..., :, None] - g[..., None, :], -jnp.inf))
    kb = k * beta[..., None]
    lower = jnp.tril(jnp.einsum('nbhcd,nbhsd->nbhcs', kb, k) * decay, -1)
    eye = jnp.eye(CHUNK, dtype=jnp.float32)
    dv = v.shape[-1]
    rhs = jnp.concatenate([v * beta[..., None], kb * jnp.exp(g)[..., None]], axis=-1)
    sol = lax.linalg.triangular_solve(lower + eye, rhs, left_side=True, lower=True, unit_diagonal=True)
    u, w = sol[..., :dv], sol[..., dv:]
    attn = jnp.einsum('nbhcd,nbhsd->nbhcs', q, k) * decay
    q_dec = q * jnp.exp(g)[..., None]
    k_dec = k * jnp.exp(g[..., -1:] - g)[..., None]
    chunk_decay = jnp.exp(g[..., -1])

    def step(S, xs):
        u_c, w_c, attn_c, q_c, k_c, cd = xs
        v_new = u_c - jnp.einsum('bhcd,bhde->bhce', w_c, S)
        o = jnp.einsum('bhcd,bhde->bhce', q_c, S) + jnp.einsum('bhcs,bhse->bhce', attn_c, v_new)
        S = S * cd[..., None, None] + jnp.einsum('bhcd,bhce->bhde', k_c, v_new)
        return S, o

    S0 = jnp.zeros(q.shape[1:3] + (q.shape[-1], dv), jnp.float32)
    _, o = lax.scan(step, S0, (u, w, attn, q_dec, k_dec, chunk_decay))
    return from_chunks(o)


def gated_deltanet(u, conv_w, a_log, dt_bias, norm_w):
    W, H = GROUP_WIDTH, GDN_HEADS
    qkv, z, a_raw, b_raw = jnp.split(u, [3 * W, 4 * W, 4 * W + H], axis=-1)
    qkv = jax.nn.silu(causal_conv(qkv, conv_w))
    q, k, v = [heads(t, H) for t in jnp.split(qkv, 3, axis=-1)]
    q = l2norm(q) * GDN_HEAD_DIM ** -0.5
    k = l2norm(k)
    beta = jax.nn.sigmoid(b_raw.astype(jnp.float32))
    g = -jnp.exp(a_log.astype(jnp.float32)) * jax.nn.softplus(a_raw.astype(jnp.float32) + dt_bias)
    o = gated_delta_rule(q, k, v, g, beta)
    o = rmsnorm(o, norm_w) * jax.nn.silu(heads(z, H).astype(jnp.float32))
    return o.reshape(o.shape[:2] + (W,))


def mlstm_chunkwise(q, k, v, i_pre, log_f):
    q, k, v = [to_chunks(t.astype(jnp.float32)) for t in (q, k, v)]
    i_pre, log_f = [to_chunks(t) for t in (i_pre, log_f)]
    causal = jnp.tril(jnp.ones((CHUNK, CHUNK), dtype=bool))

    def step(carry, xs):
        C, n, m = carry
        q_c, k_c, v_c, i_c, f_c = xs
        b = jnp.cumsum(f_c, axis=-1)
        d = jnp.where(causal, b[..., :, None] - b[..., None, :] + i_c[..., None, :], -jnp.inf)
        inter = b + m[..., None]
        m_t = jnp.maximum(inter, jnp.max(d, axis=-1))
        s = jnp.einsum('bhtd,bhsd->bhts', q_c, k_c) * jnp.exp(d - m_t[..., None])
        a = jnp.exp(inter - m_t)
        num = a[..., None] * jnp.einsum('bhtd,bhde->bhte', q_c, C) + jnp.einsum('bhts,bhse->bhte', s, v_c)
        den = a * jnp.einsum('bhtd,bhd->bht', q_c, n) + jnp.sum(s, axis=-1)
        h = num / jnp.maximum(jnp.abs(den), jnp.exp(-m_t))[..., None]
        g = b[..., -1]
        w_log = g[..., None] - b + i_c
        m_new = jnp.maximum(g + m, jnp.max(w_log, axis=-1))
        scale_old = jnp.exp(g + m - m_new)
        w_in = jnp.exp(w_log - m_new[..., None])
        C = scale_old[..., None, None] * C + jnp.einsum('bhsd,bhse->bhde', k_c * w_in[..., None], v_c)
        n = scale_old[..., None] * n + jnp.einsum('bhsd,bhs->bhd', k_c, w_in)
        return (C, n, m_new), h

    bsz, hh, dk, dv = q.shape[1], q.shape[2], q.shape[-1], v.shape[-1]
    carry0 = (jnp.zeros((bsz, hh, dk, dv), jnp.float32), jnp.zeros((bsz, hh, dk), jnp.float32),
              jnp.zeros((bsz, hh), jnp.float32))
    _, h = lax.scan(step, carry0, (q, k, v, i_pre, log_f))
    return from_chunks(h)


def mlstm(u, conv_w, b_i, b_f, norm_w):
    W, H = GROUP_WIDTH, MLSTM_HEADS
    qk, v, o_raw, i_raw, f_raw = jnp.split(u, [2 * W, 3 * W, 4 * W, 4 * W + H], axis=-1)
    qk = jax.nn.silu(causal_conv(qk, conv_w))
    q, k = [heads(t, H) for t in jnp.split(qk, 2, axis=-1)]
    k = k * MLSTM_HEAD_DIM ** -0.5
    i_pre = i_raw.astype(jnp.float32) + b_i
    log_f = jax.nn.log_sigmoid(f_raw.astype(jnp.float32) + b_f)
    h_tilde = mlstm_chunkwise(q, k, heads(v, H), i_pre, log_f)
    h = jax.nn.sigmoid(heads(o_raw, H).astype(jnp.float32)) * h_tilde
    h = rmsnorm(h, norm_w)
    return h.reshape(h.shape[:2] + (W,))


def linear_scan_combine(left, right):
    a_l, b_l = left
    a_r, b_r = right
    return a_l * a_r, a_r * b_l + b_r


def rglru_block(u, conv_w, conv_b, w_a, b_a, w_x, b_x, lam):
    xb, gate = jnp.split(u, 2, axis=-1)
    xb = causal_conv(xb, conv_w) + conv_b
    xh = heads(xb, RGLRU_BLOCKS)
    r = jax.nn.sigmoid((jnp.einsum('btni,nij->btnj', xh, w_a).reshape(xb.shape) + b_a).astype(jnp.float32))
    i = jax.nn.sigmoid((jnp.einsum('btni,nij->btnj', xh, w_x).reshape(xb.shape) + b_x).astype(jnp.float32))
    log_a = -RGLRU_C * r * jax.nn.softplus(-lam.astype(jnp.float32))
    a = jnp.exp(log_a)
    inp = jnp.sqrt(-jnp.expm1(2.0 * log_a)) * (i * xb.astype(jnp.float32))
    _, h = lax.associative_scan(linear_scan_combine, (a, inp), axis=1)
    return h * jax.nn.gelu(gate.astype(jnp.float32))


def rwkv7_scan(r, w, k, v, kk, a):
    xs = [jnp.swapaxes(t.astype(jnp.float32), 0, 1) for t in (r, w, k, v, -kk, kk * a)]

    def step(S, inp):
        r_t, w_t, k_t, v_t, a_t, b_t = inp
        sa = jnp.einsum('bhij,bhj->bhi', S, a_t)
        S = S * w_t[:, :, None, :] + sa[..., None] * b_t[:, :, None, :] + v_t[..., None] * k_t[:, :, None, :]
        return S, jnp.einsum('bhij,bhj->bhi', S, r_t)

    S0 = jnp.zeros(r.shape[:1] + r.shape[2:] + r.shape[-1:], jnp.float32)
    _, y = lax.scan(step, S0, xs)
    return jnp.swapaxes(y, 0, 1)


def rwkv7_time_mix(u, mu, w0, w2, a0, a2, g2, k_k, k_a, r_k, ln_w, ln_b):
    W, H = GROUP_WIDTH, RWKV_HEADS
    u_prev = jnp.pad(u, ((0, 0), (1, 0), (0, 0)))[:, :-1]
    u = u + (u_prev - u) * mu
    r, k, v, w_lo, a_lo, g_lo = jnp.split(
        u, [W, 2 * W, 3 * W, 3 * W + RWKV_DECAY_LORA, 3 * W + RWKV_DECAY_LORA + RWKV_A_LORA], axis=-1)
    w_raw = -jax.nn.softplus(-(w0 + jnp.tanh(w_lo) @ w2).astype(jnp.float32)) - 0.5
    decay = jnp.exp(-jnp.exp(w_raw))
    a = jax.nn.sigmoid((a0 + a_lo @ a2).astype(jnp.float32))
    g = (jax.nn.sigmoid(g_lo) @ g2).astype(jnp.float32)
    kk = l2norm(heads(k * k_k, H))
    k = k.astype(jnp.float32) * (1.0 + (a - 1.0) * k_a)
    r, k, v, decay, a = [heads(t.astype(jnp.float32), H) for t in (r, k, v, decay, a)]
    y = rwkv7_scan(r, decay, k, v, kk, a)
    y = head_layernorm(y, ln_w, ln_b, RWKV_LN_EPS)
    y = y + jnp.sum(r * k * r_k, axis=-1, keepdims=True) * v
    return y.reshape(y.shape[:2] + (W,)) * g


def setup_inputs(seed: int = 0) -> dict:
    key = jax.random.key(seed)
    ks = iter(jax.random.split(key, 48))
    L, D, W = DEPTH, D_MODEL, GROUP_WIDTH
    f32 = jnp.float32

    def normal(shape, scale):
        return jax.random.normal(next(ks), shape, f32) * scale

    def uniform(shape, lo, hi):
        return jax.random.uniform(next(ks), shape, f32, lo, hi)

    def gain(shape):
        return 1.0 + normal(shape, 0.02)

    dt = jnp.exp(uniform((L, GDN_HEADS), float(np.log(1e-3)), float(np.log(0.1))))
    a_pow = uniform((L, W), 0.9, 0.999) ** (1.0 / RGLRU_C)
    return {
        'x': normal((BATCH, SEQ, D), 1.0),
        'attn_norm': gain((L, D)),
        'w_in': normal((L, D, IN_COLS), D ** -0.5),
        'gdn_conv': normal((L, CONV_WIDTH, 3 * W), CONV_WIDTH ** -0.5),
        'gdn_a_log': jnp.log(uniform((L, GDN_HEADS), 1.0, 16.0)),
        'gdn_dt_bias': dt + jnp.log(-jnp.expm1(-dt)),
        'gdn_norm': gain((L, GDN_HEAD_DIM)),
        'mlstm_conv': normal((L, CONV_WIDTH, 2 * W), CONV_WIDTH ** -0.5),
        'mlstm_b_i': normal((L, MLSTM_HEADS), 0.1),
        'mlstm_b_f': uniform((L, MLSTM_HEADS), 3.0, 6.0),
        'mlstm_norm': gain((L, MLSTM_HEADS, MLSTM_HEAD_DIM)),
        'rglru_conv': normal((L, CONV_WIDTH, W), CONV_WIDTH ** -0.5),
        'rglru_conv_b': normal((L, W), 0.01),
        'rglru_w_a': normal((L, RGLRU_BLOCKS, RGLRU_BLOCK_DIM, RGLRU_BLOCK_DIM), RGLRU_BLOCK_DIM ** -0.5),
        'rglru_b_a': normal((L, W), 0.01),
        'rglru_w_x': normal((L, RGLRU_BLOCKS, RGLRU_BLOCK_DIM, RGLRU_BLOCK_DIM), RGLRU_BLOCK_DIM ** -0.5),
        'rglru_b_x': normal((L, W), 0.01),
        'rglru_lambda': jnp.log(a_pow) - jnp.log1p(-a_pow),
        'rwkv_mu': uniform((L, RWKV_COLS), 0.0, 1.0),
        'rwkv_w0': uniform((L, W), -6.0, 1.0),
        'rwkv_w2': normal((L, RWKV_DECAY_LORA, W), 0.1),
        'rwkv_a0': normal((L, W), 0.1),
        'rwkv_a2': normal((L, RWKV_A_LORA, W), 0.5 * RWKV_A_LORA ** -0.5),
        'rwkv_g2': normal((L, RWKV_GATE_LORA, W), RWKV_GATE_LORA ** -0.5),
        'rwkv_k_k': 0.85 + normal((L, W), 0.05),
        'rwkv_k_a': 1.0 + normal((L, W), 0.05),
        'rwkv_r_k': normal((L, RWKV_HEADS, RWKV_HEAD_DIM), 0.1),
        'rwkv_ln_w': gain((L, RWKV_HEADS, RWKV_HEAD_DIM)),
        'rwkv_ln_b': normal((L, RWKV_HEADS, RWKV_HEAD_DIM), 0.01),
        'w_out': normal((L, N_MIXERS * W, D), (N_MIXERS * W) ** -0.5),
        'ffn_norm': gain((L, D)),
        'ffn_w_gate': normal((L, D, FFN_HIDDEN), D ** -0.5),
        'ffn_w_up': normal((L, D, FFN_HIDDEN), D ** -0.5),
        'ffn_w_down': normal((L, FFN_HIDDEN, D), FFN_HIDDEN ** -0.5),
        'final_norm': gain((D,)),
    }


def reference(x, attn_norm, w_in, gdn_conv, gdn_a_log, gdn_dt_bias, gdn_norm,
              mlstm_conv, mlstm_b_i, mlstm_b_f, mlstm_norm,
              rglru_conv, rglru_conv_b, rglru_w_a, rglru_b_a, rglru_w_x, rglru_b_x, rglru_lambda,
              rwkv_mu, rwkv_w0, rwkv_w2, rwkv_a0, rwkv_a2, rwkv_g2, rwkv_k_k, rwkv_k_a, rwkv_r_k,
              rwkv_ln_w, rwkv_ln_b, w_out, ffn_norm, ffn_w_gate, ffn_w_up, ffn_w_down, final_norm):
    for l in range(DEPTH):
        n = rmsnorm(x, attn_norm[l])
        u = n @ w_in[l]
        u_gdn, u_mlstm, u_rglru, u_rwkv = jnp.split(u, MIXER_SPLITS, axis=-1)
        y_gdn = gated_deltanet(u_gdn, gdn_conv[l], gdn_a_log[l], gdn_dt_bias[l], gdn_norm[l])
        y_mlstm = mlstm(u_mlstm, mlstm_conv[l], mlstm_b_i[l], mlstm_b_f[l], mlstm_norm[l])
        y_rglru = rglru_block(u_rglru, rglru_conv[l], rglru_conv_b[l], rglru_w_a[l], rglru_b_a[l],
                              rglru_w_x[l], rglru_b_x[l], rglru_lambda[l])
        y_rwkv = rwkv7_time_mix(u_rwkv, rwkv_mu[l], rwkv_w0[l], rwkv_w2[l], rwkv_a0[l], rwkv_a2[l],
                                rwkv_g2[l], rwkv_k_k[l], rwkv_k_a[l], rwkv_r_k[l], rwkv_ln_w[l], rwkv_ln_b[l])
        mix = jnp.concatenate([y.astype(x.dtype) for y in (y_gdn, y_mlstm, y_rglru, y_rwkv)], axis=-1)
        x = x + mix @ w_out[l]
        n = rmsnorm(x, ffn_norm[l])
        x = x + (jax.nn.silu(n @ ffn_w_gate[l]) * (n @ ffn_w_up[l])) @ ffn_w_down[l]
    return rmsnorm(x, final_norm)
```

```python
import math
from contextlib import ExitStack

import numpy as np
import concourse.bass as bass
import concourse.mybir as mybir
from concourse.bass_utils import run_bass_kernel_spmd

F32 = mybir.dt.float32
BF16 = mybir.dt.bfloat16
AF = mybir.ActivationFunctionType
ALU = mybir.AluOpType
AX = mybir.AxisListType

D = 2048
NK = D // 128
W = 512
FFN = 5632
NF = FFN // 128
IN_COLS = 6928
GDN0 = 0
ML0 = 2056
RG0 = 4112
RW0 = 5136
NORM_EPS = 1e-6
RWKV_LN_EPS = 64e-5
RGLRU_C = 8.0


class Cfg:
    def __init__(self, T=4096, depth=2, TT=512, mixers=("gdn", "mlstm", "rglru", "rwkv")):
        self.T, self.depth, self.TT = T, depth, TT
        self.NB = TT // 128
        self.NT = T // TT
        self.mixers = mixers


PP_SPEC = [
    ("gdn_conv", 48), ("mlstm_conv", 32), ("rglru_conv", 16), ("rglru_conv_b", 4),
    ("rglru_b_a", 4), ("rglru_b_x", 4), ("rglru_lambda", 4), ("rwkv_mu", 14),
    ("rwkv_w0", 4), ("rwkv_a0", 4), ("rwkv_k_k", 4), ("rwkv_k_a", 4), ("rwkv_r_k", 4),
    ("gdn_a_log", 1), ("gdn_dt_bias", 1), ("mlstm_b_i", 1), ("mlstm_b_f", 1),
]
PP_OFF = {}
_o = 0
for _n, _c in PP_SPEC:
    PP_OFF[_n] = _o
    _o += _c
NPP = _o

RB_SPEC = [("attn_norm", 2048), ("ffn_norm", 2048), ("gdn_norm", 128), ("mlstm_norm", 512),
           ("rwkv_ln_w", 512), ("rwkv_ln_b", 512)]
RB_OFF = {}
_o = 0
for _n, _c in RB_SPEC:
    RB_OFF[_n] = _o
    _o += _c
NRB = _o


def _chunks(v, n):
    return np.ascontiguousarray(np.asarray(v, np.float32).reshape(n, 128).T)


def pack_params(inp, L):
    pp = np.zeros((L, 128, NPP), np.float32)
    rb = np.zeros((L, NRB), np.float32)
    for l in range(L):
        def put(name, arr):
            o = PP_OFF[name]
            pp[l, :, o:o + arr.shape[1]] = arr
        for name, nch in (("gdn_conv", 12), ("mlstm_conv", 8), ("rglru_conv", 4)):
            cw = np.asarray(inp[name][l], np.float32)
            a = cw.reshape(4, nch, 128).transpose(2, 1, 0).reshape(128, nch * 4)
            put(name, a)
        for name in ("rglru_conv_b", "rglru_b_a", "rglru_b_x", "rglru_lambda", "rwkv_w0", "rwkv_a0",
                     "rwkv_k_k", "rwkv_k_a"):
            put(name, _chunks(inp[name][l], 4))
        put("rwkv_r_k", _chunks(np.asarray(inp["rwkv_r_k"][l]).reshape(-1), 4))
        put("rwkv_mu", _chunks(inp["rwkv_mu"][l], 14))
        for name in ("gdn_a_log", "gdn_dt_bias", "mlstm_b_i", "mlstm_b_f"):
            col = np.zeros((128, 1), np.float32)
            col[0:4, 0] = np.asarray(inp[name][l], np.float32)
            put(name, col)
        for name, n in RB_SPEC:
            rb[l, RB_OFF[name]:RB_OFF[name] + n] = np.asarray(inp[name][l], np.float32).reshape(-1)
    return pp, rb


C_IDENT = 0
C_MSU = 128
C_MIU = 256
C_MSL = 384
C_SEL = 512
C_CMASK = 1024
C_ONES = 1536
C_SEL2 = 1664
NCONST = 1666


def make_consts():
    c = np.zeros((128, NCONST), np.float32)
    s = np.arange(128)
    c[:, C_IDENT:C_IDENT + 128] = np.eye(128)
    c[:, C_MSU:C_MSU + 128] = (s[:, None] < s[None, :])
    c[:, C_MIU:C_MIU + 128] = (s[:, None] <= s[None, :])
    c[:, C_MSL:C_MSL + 128] = (s[None, :] < s[:, None])
    for h in range(4):
        c[h, C_SEL + h * 128:C_SEL + (h + 1) * 128] = 1.0
    c[:, C_CMASK:C_CMASK + 512] = 1.0
    c[:, C_CMASK:C_CMASK + 512:128] = 0.0
    c[:, C_ONES:C_ONES + 128] = 1.0
    c[0:64, C_SEL2] = 1.0
    c[64:128, C_SEL2 + 1] = 1.0
    return c


class Buf:
    __slots__ = ("name", "w", "r")

    def __init__(self, name):
        self.name = name
        self.w = None
        self.r = {}


class Tile:
    def __init__(self, h, buf, bufs=None):
        self.h = h
        self.buf = buf
        self.bufs = bufs


class Prog:
    ENG = ("pe", "dve", "act", "pool", "sp")
    NDMA = 6
    SAME_ENGINE_SYNC = True

    def __init__(self, nc, stack):
        self.nc = nc
        self.stack = stack
        self.q = {e: [] for e in self.ENG}
        self.sem = {}
        self.cnt = {}
        self.waited = {e: {} for e in self.ENG}
        for e in ("pe", "dve", "act", "pool"):
            self.sem[e] = stack.enter_context(nc.semaphore("s_" + e))
            self.cnt[e] = 0
        self.dma_rr = {}
        for e in ("sp", "pool", "act"):
            self.dma_rr[e] = 0
            for j in range(self.NDMA):
                k = (e, j)
                self.sem[k] = stack.enter_context(nc.semaphore("d_%s%d" % (e, j)))
                self.cnt[k] = 0
        self.n_inst = 0
        self.n_wait = 0

    def sb(self, name, shape, dtype, nbufs=0):
        h = self.stack.enter_context(self.nc.sbuf_tensor("sb_" + name, list(shape), dtype))
        bufs = [Buf("%s#%d" % (name, i)) for i in range(nbufs)] if nbufs else None
        return Tile(h, Buf(name), bufs)

    def ps(self, name, shape, dtype=F32):
        h = self.stack.enter_context(self.nc.psum_tensor("pm_" + name, list(shape), dtype))
        return Tile(h, Buf(name))

    def _need(self, e, tok):
        if tok is None:
            return
        k, v = tok
        if k == e:
            if e == "pe" or not self.SAME_ENGINE_SYNC:
                return
        if self.waited[e].get(k, 0) >= v:
            return
        self.waited[e][k] = v
        sem = self.sem[k]
        self.q[e].append(lambda eng, sem=sem, v=v: eng.wait_ge(sem, v))
        self.n_wait += 1

    def _deps(self, e, reads, writes):
        for b in reads:
            self._need(e, b.w)
        for b in writes:
            self._need(e, b.w)
            for k, v in b.r.items():
                self._need(e, (k, v))

    def _mark(self, tok, reads, writes):
        k, v = tok
        for b in reads:
            if b.r.get(k, 0) < v:
                b.r[k] = v
        for b in writes:
            b.w = tok
            b.r = {}

    @staticmethod
    def _bufs(xs):
        out = []
        for x in xs:
            if isinstance(x, Tile):
                out.append(x.buf)
            elif isinstance(x, Buf):
                out.append(x)
            elif x is None:
                pass
            else:
                out.extend(Prog._bufs(x))
        return out

    def op(self, e, fns, reads=(), writes=()):
        if e == "pool":
            e = "dve"
        reads = self._bufs(reads)
        writes = self._bufs(writes)
        if not isinstance(fns, (list, tuple)):
            fns = [fns]
        self._deps(e, reads, writes)
        sem = self.sem[e]
        n = len(fns)
        for i, fn in enumerate(fns):
            if i == n - 1:
                self.q[e].append(lambda eng, fn=fn, sem=sem: fn(eng).then_inc(sem, 1))
            else:
                self.q[e].append(fn)
        self.n_inst += n
        self.cnt[e] += 1
        self._mark((e, self.cnt[e]), reads, writes)

    def dma(self, e, out, in_, reads=(), writes=()):
        reads = self._bufs(reads)
        writes = self._bufs(writes)
        j = self.dma_rr[e] % self.NDMA
        self.dma_rr[e] += 1
        k = (e, j)
        if self.cnt[k] > 0:
            self._need(e, (k, self.cnt[k]))
        self._deps(e, reads, writes)
        sem = self.sem[k]
        self.q[e].append(lambda eng, out=out, in_=in_, sem=sem: eng.dma_start(out=out, in_=in_).then_inc(sem, 16))
        self.n_inst += 1
        self.cnt[k] += 16
        self._mark((k, self.cnt[k]), reads, writes)

    def finish(self):
        for k, v in self.cnt.items():
            if isinstance(k, tuple) and v > 0:
                self._need("sp", (k, v))
        for e in ("pe", "dve", "act", "pool"):
            if self.cnt[e] > 0:
                self._need("sp", (e, self.cnt[e]))

    def emit(self):
        nc = self.nc
        q = self.q
        with nc.Block() as block:
            @block.tensor
            def _(eng):
                for fn in q["pe"]:
                    fn(eng)

            @block.vector
            def _(eng):
                for fn in q["dve"]:
                    fn(eng)

            @block.scalar
            def _(eng):
                for fn in q["act"]:
                    fn(eng)

            @block.gpsimd
            def _(eng):
                for fn in q["pool"]:
                    fn(eng)

            @block.sync
            def _(eng):
                for fn in q["sp"]:
                    fn(eng)


class Pool:
    def __init__(self, P, n):
        self.t = P.sb("arena", [128, n, 512], F32, nbufs=n)
        self.free = list(range(n))
        self.n = n

    def get(self):
        if not self.free:
            import os
            if os.environ.get("RWKV_STOP"):
                self.free = list(range(self.n))
        assert self.free, "arena exhausted"
        return self.free.pop(0)

    def get_block(self, n):
        for i in range(self.n - n + 1):
            if all((i + j) in self.free for j in range(n)):
                for j in range(n):
                    self.free.remove(i + j)
                return i
        raise AssertionError("no contiguous arena block")

    def blk_f32(self, i, n):
        return self.t.h[:, i:i + n, :].rearrange("p n c -> p (n c)")

    def put(self, *idx):
        import os
        for i in idx:
            if i in self.free and os.environ.get("RWKV_STOP"):
                continue
            assert i not in self.free
            self.free.append(i)

    def f32(self, i):
        return self.t.h[:, i, :]

    def bf(self, i):
        return self.t.h[:, i, :].bitcast(BF16)

    def buf(self, i):
        return self.t.bufs[i]


GELU_C = 1.5957691216057308


class Kern:
    def __init__(self, cfg):
        self.cfg = cfg
        self.stack = ExitStack()
        nc = bass.Bass("TRN2", target_bir_lowering=False)
        self.nc = nc
        L, T = cfg.depth, cfg.T
        dt = nc.dram_tensor
        self.x_d = dt("x", [T, D], F32, kind="ExternalInput").ap()
        self.out_d = dt("out", [T, D], F32, kind="ExternalOutput").ap()
        self.w_in_d = dt("w_in", [L, D, IN_COLS], F32, kind="ExternalInput").ap()
        self.w_out_d = dt("w_out", [L, D, D], F32, kind="ExternalInput").ap()
        self.wg_d = dt("ffn_w_gate", [L, D, FFN], F32, kind="ExternalInput").ap()
        self.wu_d = dt("ffn_w_up", [L, D, FFN], F32, kind="ExternalInput").ap()
        self.wd_d = dt("ffn_w_down", [L, FFN, D], F32, kind="ExternalInput").ap()
        self.pp_d = dt("pp", [L, 128, NPP], F32, kind="ExternalInput").ap()
        self.rb_d = dt("rb", [L, NRB], F32, kind="ExternalInput").ap()
        self.fin_d = dt("final_norm", [1, D], F32, kind="ExternalInput").ap()
        self.const_d = dt("consts", [128, NCONST], F32, kind="ExternalInput").ap()
        self.rg_wa_d = dt("rglru_w_a", [L, 4, 128, 128], F32, kind="ExternalInput").ap()
        self.rg_wx_d = dt("rglru_w_x", [L, 4, 128, 128], F32, kind="ExternalInput").ap()
        self.rw_w2_d = dt("rwkv_w2", [L, 64, 512], F32, kind="ExternalInput").ap()
        self.rw_a2_d = dt("rwkv_a2", [L, 64, 512], F32, kind="ExternalInput").ap()
        self.rw_g2_d = dt("rwkv_g2", [L, 128, 512], F32, kind="ExternalInput").ap()

    def build(self):
        with self.stack:
            self._build()
        return self.nc

    def act(self, out, in_, func, reads, writes, **kw):
        self.P.op("act", lambda e: e.activation(out=out, in_=in_, func=func, **kw), reads, writes)

    def tt(self, eng, out, in0, in1, op, reads, writes):
        self.P.op(eng, lambda e: e.tensor_tensor(out=out, in0=in0, in1=in1, op=op), reads, writes)

    def ts(self, eng, out, in0, s1, s2, op0, op1, reads, writes, **kw):
        if s2 is None:
            self.P.op(eng, lambda e: e.tensor_scalar(out=out, in0=in0, scalar1=s1, scalar2=None, op0=op0, **kw),
                      reads, writes)
        else:
            self.P.op(eng, lambda e: e.tensor_scalar(out=out, in0=in0, scalar1=s1, scalar2=s2, op0=op0, op1=op1, **kw),
                      reads, writes)

    def stt(self, out, in0, scalar, in1, op0, op1, reads, writes):
        self.P.op("dve", lambda e: e.scalar_tensor_tensor(out=out, in0=in0, scalar=scalar, in1=in1, op0=op0, op1=op1),
                  reads, writes)

    def cp(self, eng, out, in_, reads, writes):
        if eng == "act":
            self.P.op("act", lambda e: e.copy(out=out, in_=in_), reads, writes)
        else:
            self.P.op(eng, lambda e: e.tensor_copy(out=out, in_=in_), reads, writes)

    @staticmethod
    def trn(out, in_, identity):
        return lambda e: e.transpose(out=out, in_=in_, identity=identity)

    @staticmethod
    def mm(out, lhsT, rhs, start, stop):
        return lambda e: e.matmul(out=out, lhsT=lhsT, rhs=rhs, start=start, stop=stop)

    def _build(self):
        cfg = self.cfg
        nc = self.nc
        P = Prog(nc, self.stack)
        self.P = P
        NB, TT, L = cfg.NB, cfg.TT, cfg.depth
        self.x = [P.sb("x%d" % b, [128, D], F32) for b in range(NB)]
        self.nT = P.sb("nT", [128, NK, TT], BF16, nbufs=NB)
        self.mixT = P.sb("mixT", [128, NK, TT], BF16, nbufs=NK)
        self.NSLOT = 3
        self.wslot = [P.sb("wslot%d" % i, [128, NK, 520], BF16) for i in range(self.NSLOT)]
        self.wrr = 0
        self.xs = [P.sb("xs%d" % i, [128, D], BF16) for i in range(1)]
        self.cbuf = [P.sb("cbuf%d" % i, [128, 515], F32) for i in range(2)]
        self.cb_rr = 0
        self.pp = [P.sb("pp%d" % l, [128, NPP], F32) for l in range(L)]
        self.consts = P.sb("consts", [128, NCONST], F32)
        self.ident_b = P.sb("ident_b", [128, 128], BF16)
        self.small = P.sb("small", [128, 64], F32, nbufs=64)
        self.small_rr = 0
        self.psum = [P.ps("ps%d" % i, [128, 512], F32) for i in range(8)]
        self.ps_rr = 0
        self.pool = Pool(P, 23)
        self.small8 = P.sb("small8", [128, 64], F32, nbufs=2)
        self.small4 = P.sb("small4", [128, 64], F32, nbufs=16)

        P.dma("sp", self.consts.h[:], self.const_d[:, :], writes=[self.consts])
        for l in range(L):
            P.dma("sp", self.pp[l].h[:], self.pp_d[l], writes=[self.pp[l]])
        self.cp("dve", self.ident_b.h[:], self.consts.h[:, 0:128], [self.consts], [self.ident_b])
        self.setup_mixers()

        for t in range(cfg.NT):
            self.load_x(t)
            for l in range(L):
                self.layer(l, t)
            self.final_norm_store(t)
        P.finish()
        self.sbuf_left = nc.sbuf_bytes_remaining
        P.emit()

    def next_ps(self):
        i = self.ps_rr % 8
        self.ps_rr += 1
        return self.psum[i]

    def scal(self):
        i = self.small_rr % 64
        self.small_rr += 1
        return self.small.h[:, i:i + 1], self.small.bufs[i]

    def ppc(self, l, name, j=0):
        o = PP_OFF[name] + j
        return self.pp[l].h[:, o:o + 1]

    def load_w(self, src_ap, nk=NK, ncols=512):
        s = self.wslot[self.wrr % self.NSLOT]
        self.wrr += 1
        self.P.dma("pool", s.h[:, 0:nk, 0:ncols], src_ap.rearrange("(k p) c -> p k c", p=128), writes=[s])
        return s

    def load_x(self, t):
        P, cfg = self.P, self.cfg
        for b in range(cfg.NB):
            r0 = t * cfg.TT + b * 128
            P.dma("sp", self.x[b].h[:], self.x_d[r0:r0 + 128, :], writes=[self.x[b]])

    def load_rowbcast(self, dst, dcol0, src_row_ap):
        n = src_row_ap.shape[-1]
        self.P.dma("sp", dst.h[:, dcol0:dcol0 + n], src_row_ap.partition_broadcast(128), writes=[dst])

    def wrow_get(self, src_row_ap):
        i = self.pool.get_block(4)
        ap = self.pool.blk_f32(i, 4)
        bufs = [self.pool.buf(i + j) for j in range(4)]
        self.P.dma("sp", ap, src_row_ap.partition_broadcast(128), writes=bufs)
        return ap, bufs, i

    def wrow_put(self, i):
        self.pool.put(i, i + 1, i + 2, i + 3)

    def row_rstd(self, xb):
        xs = self.xs[0]
        ss, ssb = self.scal()
        rs, rsb = self.scal()
        self.act(xs.h[:], xb.h[:], AF.Square, [xb], [xs, ssb], accum_out=ss)
        self.act(rs, ss, AF.Sqrt, [ssb], [rsb], bias=NORM_EPS, scale=1.0 / D)
        self.P.op("dve", lambda e: e.reciprocal(out=rs, in_=rs), [rsb], [rsb])
        return rs, rsb, xs

    def rmsnorm_T(self, src_row_ap):
        P, cfg = self.P, self.cfg
        wr, wrb, wi = self.wrow_get(src_row_ap)
        for b in range(cfg.NB):
            xb = self.x[b]
            self.cb_rr += 1
            rs, rsb, xs = self.row_rstd(xb)
            self.stt(xs.h[:], xb.h[:], rs, wr, ALU.mult, ALU.mult, [xb, rsb, wrb], [xs])
            for g in range(4):
                ps = self.next_ps()
                psb = ps.h[:].bitcast(BF16)
                fns = []
                for j in range(4):
                    k = g * 4 + j
                    fns.append(lambda e, psb=psb, j=j, k=k, xs=xs: e.transpose(
                        out=psb[:, j * 128:(j + 1) * 128], in_=xs.h[:, k * 128:(k + 1) * 128],
                        identity=self.ident_b.h[:]))
                P.op("pe", fns, [xs, self.ident_b], [ps])
                dst = self.nT.h[:, g * 4:(g + 1) * 4, b * 128:(b + 1) * 128]
                srcv = psb[:, 0:512].rearrange("p (j c) -> p j c", j=4)
                self.cp("act" if g % 2 else "dve", dst, srcv, [ps], [self.nT.bufs[b]])
        self.wrow_put(wi)

    def inproj_fm(self, ws, wc, m):
        ps = self.next_ps()
        TT = self.cfg.TT
        fns = [self.mm(ps.h[0:m, 0:TT], ws.h[:, k, wc:wc + m], self.nT.h[:, k, :], k == 0, k == NK - 1)
               for k in range(NK)]
        self.P.op("pe", fns, [ws, self.nT.bufs], [ps])
        return ps

    def inproj_tm(self, ws, wc, n, b):
        ps = self.next_ps()
        fns = [self.mm(ps.h[:, 0:n], self.nT.h[:, k, b * 128:(b + 1) * 128], ws.h[:, k, wc:wc + n], k == 0, k == NK - 1)
               for k in range(NK)]
        self.P.op("pe", fns, [ws, self.nT.bufs[b]], [ps])
        return ps

    def conv4(self, l, ps, m, cname, cidx, halo, halo_buf, hcol, bias=None):
        TT = self.cfg.TT
        pool = self.pool
        cb = self.cbuf[self.cb_rr % 2]
        self.cb_rr += 1
        self.cp("act", cb.h[0:m, 3:3 + TT], ps.h[0:m, 0:TT], [ps], [cb])
        self.cp("dve", cb.h[0:m, 0:3], halo.h[0:m, hcol:hcol + 3], [halo_buf], [cb])
        o = pool.get()
        acc = pool.f32(o)[0:m, 0:TT]
        w = lambda j: self.ppc(l, cname, cidx * 4 + j)[0:m]
        if bias is None:
            self.ts("dve", acc, cb.h[0:m, 3:3 + TT], w(3), None, ALU.mult, None, [cb, self.pp[l]], [pool.buf(o)])
        else:
            self.ts("dve", acc, cb.h[0:m, 3:3 + TT], w(3), bias, ALU.mult, ALU.add, [cb, self.pp[l]], [pool.buf(o)])
        for j in (2, 1, 0):
            self.stt(acc, cb.h[0:m, j:j + TT], w(j), acc, ALU.mult, ALU.add, [cb, self.pp[l], pool.buf(o)], [pool.buf(o)])
        self.cp("dve", halo.h[0:m, hcol:hcol + 3], cb.h[0:m, TT:TT + 3], [cb], [halo_buf])
        return o

    def layer(self, l, t):
        P, cfg, pool = self.P, self.cfg, self.pool
        NB, TT = cfg.NB, cfg.TT
        self.rmsnorm_T(self.rb_d[l:l + 1, RB_OFF["attn_norm"]:RB_OFF["attn_norm"] + D])
        for name in ("gdn", "mlstm", "rglru", "rwkv"):
            if name in cfg.mixers:
                getattr(self, name)(l, t)
            else:
                base = {"gdn": 0, "mlstm": 4, "rglru": 8, "rwkv": 12}[name]
                if t == 0 and l == 0:
                    for c in range(4):
                        P.op("pool", lambda e, c=c, base=base: e.memset(self.mixT.h[:, base + c, :], 0.0),
                             [], [self.mixT.bufs[base + c]])
        for dg in range(4):
            ws = self.load_w(self.w_out_d[l, :, dg * 512:(dg + 1) * 512])
            for b in range(NB):
                ps = self.next_ps()
                fns = [self.mm(ps.h[:, :], self.mixT.h[:, k, b * 128:(b + 1) * 128], ws.h[:, k, 0:512], k == 0, k == NK - 1)
                       for k in range(NK)]
                P.op("pe", fns, [ws, self.mixT.bufs], [ps])
                xv = self.x[b].h[:, dg * 512:(dg + 1) * 512]
                self.tt("dve", xv, xv, ps.h[:, :], ALU.add, [ps, self.x[b]], [self.x[b]])
        self.rmsnorm_T(self.rb_d[l:l + 1, RB_OFF["ffn_norm"]:RB_OFF["ffn_norm"] + D])
        hs = [pool.get() for _ in range(NF // 2)]

        def hT(c):
            return pool.bf(hs[c // 2])[:, (c % 2) * 512:(c % 2) * 512 + TT], pool.buf(hs[c // 2])
        for fg in range(FFN // 512):
            wg = self.load_w(self.wg_d[l, :, fg * 512:(fg + 1) * 512])
            wu = self.load_w(self.wu_d[l, :, fg * 512:(fg + 1) * 512])
            for j in range(4):
                c = fg * 4 + j
                pg = self.inproj_fm(wg, j * 128, 128)
                pu = self.inproj_fm(wu, j * 128, 128)
                sg = pool.get()
                self.act(pool.f32(sg)[:, 0:TT], pg.h[:, 0:TT], AF.Silu, [pg], [pool.buf(sg)])
                h_ap, h_buf = hT(c)
                self.tt("dve", h_ap, pool.f32(sg)[:, 0:TT], pu.h[:, 0:TT], ALU.mult, [pu, pool.buf(sg)], [h_buf])
                pool.put(sg)
        fsl = [(0, 16), (16, 16), (32, 12)]
        for dg in range(4):
            pss = [self.next_ps() for _ in range(NB)]
            for si, (f0, nf) in enumerate(fsl):
                ws = self.load_w(self.wd_d[l, f0 * 128:(f0 + nf) * 128, dg * 512:(dg + 1) * 512], nk=nf)
                for b in range(NB):
                    fns = []
                    rd = [ws]
                    for kk in range(nf):
                        h_ap, h_buf = hT(f0 + kk)
                        rd.append(h_buf)
                        fns.append(self.mm(pss[b].h[:, :], h_ap[:, b * 128:(b + 1) * 128], ws.h[:, kk, 0:512],
                                           si == 0 and kk == 0, si == 2 and kk == nf - 1))
                    P.op("pe", fns, rd, [pss[b]])
            for b in range(NB):
                xv = self.x[b].h[:, dg * 512:(dg + 1) * 512]
                self.tt("dve", xv, xv, pss[b].h[:, :], ALU.add, [pss[b], self.x[b]], [self.x[b]])
        pool.put(*hs)

    def final_norm_store(self, t):
        P, cfg = self.P, self.cfg
        wr, wrb, wi = self.wrow_get(self.fin_d[0:1, :])
        for b in range(cfg.NB):
            xb = self.x[b]
            self.cb_rr += 1
            rs, rsb, xs = self.row_rstd(xb)
            self.stt(xb.h[:], xb.h[:], rs, wr, ALU.mult, ALU.mult, [xb, rsb, wrb], [xb])
            r0 = t * cfg.TT + b * 128
            P.dma("sp", self.out_d[r0:r0 + 128, :], xb.h[:], reads=[xb])
        self.wrow_put(wi)

    def setup_mixers(self):
        P, cfg = self.P, self.cfg
        L = cfg.depth
        self.tok = P.sb("tok", [128, 64], F32)
        self.rown = P.sb("rown", [128, 512], F32)
        for name in ("gdn", "mlstm", "rwkv"):
            if name in cfg.mixers:
                globals()["setup_" + name](self)
        self.halo = [P.sb("halo%d" % l, [128, 72], F32, nbufs=24) for l in range(L)]
        self.rg_h = [P.sb("rg_h%d" % l, [128, 4], F32, nbufs=4) for l in range(L)]
        self.rg_w = [P.sb("rg_w%d" % l, [128, 8, 128], BF16) for l in range(L)]
        self.rg_cp = [P.sb("rg_cp%d" % l, [128, 8], F32) for l in range(L)]
        for l in range(L):
            P.op("pool", lambda e, l=l: e.memset(self.halo[l].h[:], 0.0), [], self.halo[l].bufs)
            P.op("pool", lambda e, l=l: e.memset(self.rg_h[l].h[:], 0.0), [], self.rg_h[l].bufs)
            P.dma("pool", self.rg_w[l].h[:, 0:4, :], self.rg_wa_d[l].rearrange("n i j -> i n j"), writes=[self.rg_w[l]])
            P.dma("pool", self.rg_w[l].h[:, 4:8, :], self.rg_wx_d[l].rearrange("n i j -> i n j"), writes=[self.rg_w[l]])
            lam = self.pp[l].h[:, PP_OFF["rglru_lambda"]:PP_OFF["rglru_lambda"] + 4]
            cpt = self.rg_cp[l]
            self.act(cpt.h[:, 0:4], lam, AF.Exp, [self.pp[l]], [cpt], scale=-1.0)
            self.act(cpt.h[:, 0:4], cpt.h[:, 0:4], AF.Ln, [cpt], [cpt], bias=1.0)
            self.ts("dve", cpt.h[:, 4:8], cpt.h[:, 0:4], -2.0 * RGLRU_C, None, ALU.mult, None, [cpt], [cpt])
            self.ts("dve", cpt.h[:, 0:4], cpt.h[:, 0:4], -RGLRU_C, None, ALU.mult, None, [cpt], [cpt])

    def rglru(self, l, t):
        P, cfg, pool = self.P, self.cfg, self.pool
        TT = cfg.TT
        wx = self.load_w(self.w_in_d[l, :, RG0:RG0 + 512])
        wgt = self.load_w(self.w_in_d[l, :, RG0 + 512:RG0 + 1024])
        pl = self.pp[l]
        for c in range(4):
            ps = self.inproj_fm(wx, c * 128, 128)
            hb = 20 + c
            xo = self.conv4(l, ps, 128, "rglru_conv", c, self.halo[l], self.halo[l].bufs[hb], hb * 3,
                            bias=self.ppc(l, "rglru_conv_b", c))
            xf = pool.f32(xo)[:, 0:TT]
            xbf = pool.get()
            xb16 = pool.bf(xbf)[:, 0:TT]
            self.cp("act", xb16, xf, [pool.buf(xo)], [pool.buf(xbf)])
            pr = self.next_ps()
            P.op("pe", [self.mm(pr.h[:, 0:TT], self.rg_w[l].h[:, c, :], xb16, True, True)], [self.rg_w[l], pool.buf(xbf)], [pr])
            pi = self.next_ps()
            P.op("pe", [self.mm(pi.h[:, 0:TT], self.rg_w[l].h[:, 4 + c, :], xb16, True, True)], [self.rg_w[l], pool.buf(xbf)], [pi])
            r_ = pool.get()
            i_ = pool.get()
            rf, if_ = pool.f32(r_)[:, 0:TT], pool.f32(i_)[:, 0:TT]
            self.act(rf, pr.h[:, 0:TT], AF.Sigmoid, [pr, pl], [pool.buf(r_)], bias=self.ppc(l, "rglru_b_a", c))
            self.act(if_, pi.h[:, 0:TT], AF.Sigmoid, [pi, pl], [pool.buf(i_)], bias=self.ppc(l, "rglru_b_x", c))
            a_ = xbf
            af = pool.f32(a_)[:, 0:TT]
            self.act(af, rf, AF.Exp, [pool.buf(r_), self.rg_cp[l], pool.buf(a_)], [pool.buf(a_)], scale=self.rg_cp[l].h[:, c:c + 1])
            self.act(rf, rf, AF.Exp, [pool.buf(r_), self.rg_cp[l]], [pool.buf(r_)], scale=self.rg_cp[l].h[:, 4 + c:5 + c])
            self.ts("dve", rf, rf, -1.0, 1.0, ALU.mult, ALU.add, [pool.buf(r_)], [pool.buf(r_)])
            self.act(rf, rf, AF.Sqrt, [pool.buf(r_)], [pool.buf(r_)])
            self.tt("dve", if_, if_, xf, ALU.mult, [pool.buf(i_), pool.buf(xo)], [pool.buf(i_)])
            self.tt("dve", if_, if_, rf, ALU.mult, [pool.buf(i_), pool.buf(r_)], [pool.buf(i_)])
            hst = self.rg_h[l].h[:, c:c + 1]
            hstb = self.rg_h[l].bufs[c]
            P.op("dve", lambda e, xf=xf, af=af, if_=if_, hst=hst: e.tensor_tensor_scan(
                out=xf, data0=af, data1=if_, initial=hst, op0=ALU.mult, op1=ALU.add),
                [pool.buf(a_), pool.buf(i_), hstb], [pool.buf(xo)])
            self.cp("dve", hst, xf[:, TT - 1:TT], [pool.buf(xo)], [hstb])
            pg = self.inproj_fm(wgt, c * 128, 128)
            g = pg.h[:, 0:TT]
            self.act(rf, g, AF.Square, [pg], [pool.buf(r_)])
            self.ts("dve", rf, rf, 0.044715, 1.0, ALU.mult, ALU.add, [pool.buf(r_)], [pool.buf(r_)])
            self.tt("dve", rf, rf, g, ALU.mult, [pool.buf(r_), pg], [pool.buf(r_)])
            self.act(rf, rf, AF.Sigmoid, [pool.buf(r_)], [pool.buf(r_)], scale=GELU_C)
            self.tt("dve", rf, rf, g, ALU.mult, [pool.buf(r_), pg], [pool.buf(r_)])
            self.tt("dve", self.mixT.h[:, 8 + c, :], rf, xf, ALU.mult, [pool.buf(r_), pool.buf(xo)], [self.mixT.bufs[8 + c]])
            pool.put(xo, a_, r_, i_)

    def gdn(self, l, t):
        raise NotImplementedError

    def rwkv(self, l, t):
        raise NotImplementedError


_W_NAMES = ("w_in", "w_out", "ffn_w_gate", "ffn_w_up", "ffn_w_down", "rglru_w_a", "rglru_w_x",
            "rwkv_w2", "rwkv_a2", "rwkv_g2")


def make_in_maps(inputs, cfg, n_cores):
    L = cfg.depth
    pp, rb = pack_params(inputs, L)
    consts = make_consts()
    shared = {k: np.ascontiguousarray(np.asarray(inputs[k], np.float32)) for k in _W_NAMES}
    shared["pp"] = pp
    shared["rb"] = rb
    shared["final_norm"] = np.ascontiguousarray(np.asarray(inputs["final_norm"], np.float32).reshape(1, D))
    shared["consts"] = consts
    x = np.asarray(inputs["x"], np.float32)
    B = x.shape[0]
    maps = []
    for c in range(n_cores):
        m = dict(shared)
        m["x"] = np.ascontiguousarray(x[c % B])
        maps.append(m)
    return maps


def kernel(**inputs):
    x = np.asarray(inputs["x"])
    B, T, _ = x.shape
    L = np.asarray(inputs["w_in"]).shape[0]
    cfg = Cfg(T=T, depth=L)
    nc = Kern(cfg).build()
    n_cores = B
    in_maps = make_in_maps(inputs, cfg, n_cores)
    res = run_bass_kernel_spmd(nc, in_maps, core_ids=list(range(n_cores)))
    out = np.stack([np.asarray(res.results[c]["out"], np.float32) for c in range(B)], axis=0)
    return out

def _cs(ap, j, n=128):
    return ap[:, j * n:(j + 1) * n]


def setup_mlstm(self):
    P, cfg = self.P, self.cfg
    L = cfg.depth
    self.ml_C = [P.sb("ml_C%d" % l, [128, 4, 129], F32, nbufs=4) for l in range(L)]
    self.ml_Cb = [P.sb("ml_Cb%d" % l, [128, 4, 130], BF16, nbufs=4) for l in range(L)]
    self.ml_sc = [P.sb("ml_sc%d" % l, [128, 2], F32) for l in range(L)]
    self.vp = [P.sb("vp%d" % c, [128, 4, 130], BF16) for c in range(4)]
    for c in range(4):
        P.op("pool", lambda e, c=c: e.memset(self.vp[c].h[:], 1.0), [], [self.vp[c]])
    for l in range(L):
        P.op("pool", lambda e, l=l: e.memset(self.ml_C[l].h[:], 0.0), [], self.ml_C[l].bufs)
        P.op("pool", lambda e, l=l: e.memset(self.ml_Cb[l].h[:], 0.0), [], self.ml_Cb[l].bufs)
        self.ts("dve", self.ml_sc[l].h[:, 0:1], self.ppc(l, "mlstm_b_f"), -1.0, None, ALU.mult, None,
                [self.pp[l]], [self.ml_sc[l]])


def gate_rows_to_bcast(self, rows_ap, rows_buf, h):
    ps = self.next_ps()
    TT = self.cfg.TT
    sel = self.consts.h[0:4, C_SEL + h * 128:C_SEL + (h + 1) * 128]
    self.P.op("pe", [self.mm(ps.h[:, 0:TT], sel, rows_ap, True, True)], [self.consts, rows_buf], [ps])
    return ps


def rows_to_tok(self, rows_list, col0):
    P = self.P
    NCH = self.cfg.TT // 128
    ps = self.next_ps()
    fns = []
    rd = [self.consts]
    for q, (ap, buf) in enumerate(rows_list):
        rd.append(buf)
        for c in range(NCH):
            o = (q * NCH + c) * 4
            fns.append(self.trn(ps.h[:, o:o + 4], ap[0:4, c * 128:(c + 1) * 128], self.consts.h[0:4, C_IDENT:C_IDENT + 4]))
    P.op("pe", fns, rd, [ps])
    n = len(rows_list) * NCH * 4
    self.cp("dve", self.tok.h[:, col0:col0 + n], ps.h[:, 0:n], [ps], [self.tok])


def mlstm(self, l, t):
    P, cfg, pool = self.P, self.cfg, self.pool
    TT = cfg.TT
    NCH = TT // 128
    pl = self.pp[l]
    cst, cbf = self.ml_C[l], self.ml_Cb[l]
    self.load_rowbcast(self.rown, 0, self.rb_d[l:l + 1, RB_OFF["mlstm_norm"]:RB_OFF["mlstm_norm"] + 512])
    wo = self.load_w(self.w_in_d[l, :, ML0 + 1536:ML0 + 2056], ncols=520)
    ps_i = self.inproj_fm(wo, 512, 4)
    ps_f = self.inproj_fm(wo, 516, 4)
    s_i, s_b, s_c = pool.get(), pool.get(), pool.get()
    r_i, r_b, r_c = pool.f32(s_i)[0:4, 0:TT], pool.f32(s_b)[0:4, 0:TT], pool.f32(s_c)[0:4, 0:TT]
    self.act(r_i, ps_i.h[0:4, 0:TT], AF.Identity, [ps_i, pl], [pool.buf(s_i)], bias=self.ppc(l, "mlstm_b_i")[0:4])
    self.act(r_b, ps_f.h[0:4, 0:TT], AF.Exp, [ps_f, self.ml_sc[l]], [pool.buf(s_b)], bias=self.ml_sc[l].h[0:4, 0:1], scale=-1.0)
    self.act(r_b, r_b, AF.Ln, [pool.buf(s_b)], [pool.buf(s_b)], bias=1.0)
    cm = self.consts.h[0:4, C_CMASK:C_CMASK + TT]
    P.op("dve", lambda e: e.tensor_tensor_scan(out=r_c, data0=cm, data1=r_b, initial=0.0, op0=ALU.mult, op1=ALU.add),
         [self.consts, pool.buf(s_b)], [pool.buf(s_c)])
    self.ts("dve", r_b, r_c, -1.0, None, ALU.mult, None, [pool.buf(s_c)], [pool.buf(s_b)])
    self.tt("dve", r_c, r_i, r_b, ALU.subtract, [pool.buf(s_i), pool.buf(s_b)], [pool.buf(s_c)])
    rows_to_tok(self, [(r_c, pool.buf(s_c))], 0)
    bbc = []
    for h in range(4):
        ps = gate_rows_to_bcast(self, r_b, pool.buf(s_b), h)
        o = pool.get()
        self.cp("act", pool.f32(o)[:, 0:TT], ps.h[:, 0:TT], [ps], [pool.buf(o)])
        bbc.append(o)
    pool.put(s_i, s_b, s_c)
    wv = self.load_w(self.w_in_d[l, :, ML0 + 1024:ML0 + 1536])
    osg = [pool.get() for _ in range(NCH // 2)]

    def osig(c):
        return pool.bf(osg[c // 2])[:, (c % 2) * 512:(c % 2) * 512 + 512], pool.buf(osg[c // 2])
    for c in range(NCH):
        ps_v = self.inproj_tm(wv, 0, 512, c)
        self.cp("act", self.vp[c].h[:, :, 0:128], ps_v.h[:, 0:512].rearrange("p (h e) -> p h e", h=4), [ps_v], [self.vp[c]])
        ps_o = self.inproj_tm(wo, 0, 512, c)
        oa, ob = osig(c)
        self.act(oa, ps_o.h[:, 0:512], AF.Sigmoid, [ps_o], [ob])
    wq = self.load_w(self.w_in_d[l, :, ML0:ML0 + 512])
    wk = self.load_w(self.w_in_d[l, :, ML0 + 512:ML0 + 1024])
    qk = [pool.get() for _ in range(4)]
    qd = [pool.get() for _ in range(2)]
    for h in range(4):
        ps = self.inproj_fm(wq, h * 128, 128)
        o = self.conv4(l, ps, 128, "mlstm_conv", h, self.halo[l], self.halo[l].bufs[12 + h], (12 + h) * 3)
        qf = pool.f32(o)[:, 0:TT]
        self.act(qf, qf, AF.Silu, [pool.buf(o)], [pool.buf(o)])
        self.cp("act", pool.bf(qk[h])[:, 0:TT], qf, [pool.buf(o)], [pool.buf(qk[h])])
        e_ = pool.get()
        self.act(pool.f32(e_)[:, 0:TT], pool.f32(bbc[h])[:, 0:TT], AF.Exp, [pool.buf(bbc[h])], [pool.buf(e_)])
        self.tt("dve", pool.bf(qd[h // 2])[:, (h % 2) * 512:(h % 2) * 512 + TT], qf, pool.f32(e_)[:, 0:TT], ALU.mult,
                [pool.buf(o), pool.buf(e_)], [pool.buf(qd[h // 2])])
        pool.put(o, e_)
        ps = self.inproj_fm(wk, h * 128, 128)
        o = self.conv4(l, ps, 128, "mlstm_conv", 4 + h, self.halo[l], self.halo[l].bufs[16 + h], (16 + h) * 3)
        kf = pool.f32(o)[:, 0:TT]
        self.act(kf, kf, AF.Silu, [pool.buf(o)], [pool.buf(o)])
        self.ts("dve", pool.bf(qk[h])[:, 512:512 + TT], kf, 128.0 ** -0.5, None, ALU.mult, None, [pool.buf(o)], [pool.buf(qk[h])])
        pool.put(o)
    for c in range(NCH):
        cs = slice(c * 128, (c + 1) * 128)
        st_ps = self.next_ps()
        fns = [self.mm(st_ps.h[:, h * 128:(h + 1) * 128], pool.bf(qk[h])[:, 512 + c * 128:512 + (c + 1) * 128],
                       pool.bf(qk[h])[:, cs], True, True) for h in range(4)]
        P.op("pe", fns, [pool.buf(qk[h]) for h in range(4)], [st_ps])
        dt_ = pool.get()
        dtf = pool.f32(dt_)
        for h in range(4):
            self.act(dtf[:, h * 128:(h + 1) * 128], pool.f32(bbc[h])[:, cs], AF.Exp, [pool.buf(bbc[h]), self.tok],
                     [pool.buf(dt_)], bias=self.tok.h[:, c * 4 + h:c * 4 + h + 1])
        for h in range(4):
            self.tt("dve", dtf[:, h * 128:(h + 1) * 128], dtf[:, h * 128:(h + 1) * 128],
                    self.consts.h[:, C_MIU:C_MIU + 128], ALU.mult, [pool.buf(dt_), self.consts], [pool.buf(dt_)])
        st_ = pool.get()
        stb = pool.bf(st_)[:, 0:512]
        self.tt("dve", stb, dtf, st_ps.h[:, 0:512], ALU.mult, [pool.buf(dt_), st_ps], [pool.buf(st_)])
        kw, kwb = self.scal4()
        for h in range(4):
            self.act(kw[:, h:h + 1], self.tok.h[:, c * 4 + h:c * 4 + h + 1], AF.Exp, [self.tok, pool.buf(bbc[h])], [kwb],
                     bias=pool.f32(bbc[h])[:, c * 128 + 127:c * 128 + 128])
        eg, egb = self.scal4()
        for h in range(4):
            self.act(eg[:, h:h + 1], pool.f32(bbc[h])[:, c * 128 + 127:c * 128 + 128], AF.Exp, [pool.buf(bbc[h])], [egb])
        kt_ps = self.next_ps()
        ktb = kt_ps.h[:].bitcast(BF16)
        fns = [self.trn(ktb[:, h * 128:(h + 1) * 128], pool.bf(qk[h])[:, 512 + c * 128:512 + (c + 1) * 128], self.ident_b.h[:])
               for h in range(4)]
        P.op("pe", fns, [pool.buf(qk[h]) for h in range(4)] + [self.ident_b], [kt_ps])
        kw_ = pool.get()
        kwt = pool.bf(kw_)[:, 0:512]
        for h in range(4):
            self.ts("dve", kwt[:, h * 128:(h + 1) * 128], ktb[:, h * 128:(h + 1) * 128], kw[:, h:h + 1], None, ALU.mult, None,
                    [kt_ps, kwb], [pool.buf(kw_)])
        y_ = pool.get()
        yb = pool.bf(y_)[:, 0:512]
        oa, ob = osig(c)
        for h in range(4):
            pn = self.next_ps()
            P.op("pe", [self.mm(pn.h[:, 0:129], pool.bf(qd[h // 2])[:, (h % 2) * 512 + c * 128:(h % 2) * 512 + (c + 1) * 128],
                                cbf.h[:, h, 0:129], True, False),
                        self.mm(pn.h[:, 0:129], stb[:, h * 128:(h + 1) * 128], self.vp[c].h[:, h, 0:129], False, True)],
                 [pool.buf(qd[h // 2]), cbf.bufs[h], pool.buf(st_), self.vp[c]], [pn])
            dn, dnb = self.scal()
            self.act(dn, pn.h[:, 128:129], AF.Abs, [pn], [dnb])
            self.ts("dve", dn, dn, 1.0, None, ALU.max, None, [dnb], [dnb])
            P.op("dve", lambda e, dn=dn: e.reciprocal(out=dn, in_=dn), [dnb], [dnb])
            hh_ = pool.get()
            hf = pool.f32(hh_)[:, 0:128]
            self.stt(hf, pn.h[:, 0:128], dn, oa[:, h * 128:(h + 1) * 128], ALU.mult, ALU.mult, [pn, dnb, ob], [pool.buf(hh_)])
            ss, ssb = self.scal()
            self.act(pool.f32(hh_)[:, 128:256], hf, AF.Square, [pool.buf(hh_)], [pool.buf(hh_), ssb], accum_out=ss)
            self.act(ss, ss, AF.Sqrt, [ssb], [ssb], bias=NORM_EPS, scale=1.0 / 128)
            P.op("dve", lambda e, ss=ss: e.reciprocal(out=ss, in_=ss), [ssb], [ssb])
            self.stt(yb[:, h * 128:(h + 1) * 128], hf, ss, self.rown.h[:, h * 128:(h + 1) * 128], ALU.mult, ALU.mult,
                     [pool.buf(hh_), ssb, self.rown], [pool.buf(y_)])
            pool.put(hh_)
            pu = self.next_ps()
            P.op("pe", [self.mm(pu.h[:, 0:129], kwt[:, h * 128:(h + 1) * 128], self.vp[c].h[:, h, 0:129], True, True)],
                 [pool.buf(kw_), self.vp[c]], [pu])
            self.stt(cst.h[:, h, :], cst.h[:, h, :], eg[:, h:h + 1], pu.h[:, 0:129], ALU.mult, ALU.add,
                     [cst.bufs[h], egb, pu], [cst.bufs[h]])
            self.cp("act", cbf.h[:, h, 0:129], cst.h[:, h, :], [cst.bufs[h]], [cbf.bufs[h]])
        yt_ps = self.next_ps()
        ytb = yt_ps.h[:].bitcast(BF16)
        fns = [self.trn(ytb[:, h * 128:(h + 1) * 128], yb[:, h * 128:(h + 1) * 128], self.ident_b.h[:]) for h in range(4)]
        P.op("pe", fns, [pool.buf(y_), self.ident_b], [yt_ps])
        self.cp("act", self.mixT.h[:, 4:8, cs], ytb[:, 0:512].rearrange("p (h c) -> p h c", h=4), [yt_ps],
                [self.mixT.bufs[4 + h] for h in range(4)])
        pool.put(dt_, st_, kw_, y_)
    pool.put(*bbc)
    pool.put(*osg)
    pool.put(*qk)
    pool.put(*qd)


def scal4(self):
    i = self.small_rr % 16
    self.small_rr += 1
    return self.small4.h[:, i * 4:(i + 1) * 4], self.small4.bufs[i]


Kern.mlstm = mlstm
Kern.scal4 = scal4

def setup_gdn(self):
    P, cfg = self.P, self.cfg
    L = cfg.depth
    self.gd_S = [P.sb("gd_S%d" % l, [128, 4, 128], F32, nbufs=4) for l in range(L)]
    self.gd_Sb = [P.sb("gd_Sb%d" % l, [128, 4, 128], BF16, nbufs=4) for l in range(L)]
    self.gd_sc = [P.sb("gd_sc%d" % l, [128, 2], F32) for l in range(L)]
    self.ones_b = P.sb("ones_b", [128, 128], BF16)
    self.cp("dve", self.ones_b.h[:], self.consts.h[:, C_ONES:C_ONES + 128], [self.consts], [self.ones_b])
    for l in range(L):
        P.op("pool", lambda e, l=l: e.memset(self.gd_S[l].h[:], 0.0), [], self.gd_S[l].bufs)
        P.op("pool", lambda e, l=l: e.memset(self.gd_Sb[l].h[:], 0.0), [], self.gd_Sb[l].bufs)
        self.act(self.gd_sc[l].h[:, 0:1], self.ppc(l, "gdn_a_log"), AF.Exp, [self.pp[l]], [self.gd_sc[l]])
        self.ts("dve", self.gd_sc[l].h[:, 0:1], self.gd_sc[l].h[:, 0:1], -1.0, None, ALU.mult, None,
                [self.gd_sc[l]], [self.gd_sc[l]])


def l2norm_fm(self, xo, scale):
    P, pool = self.P, self.pool
    TT = self.cfg.TT
    xf = pool.f32(xo)[:, 0:TT]
    sq = pool.get()
    self.act(pool.bf(sq)[:, 0:TT], xf, AF.Square, [pool.buf(xo)], [pool.buf(sq)])
    ps = self.next_ps()
    P.op("pe", [self.mm(ps.h[:, 0:TT], self.ones_b.h[:], pool.bf(sq)[:, 0:TT], True, True)], [self.ones_b, pool.buf(sq)], [ps])
    rn = pool.f32(sq)[:, 0:TT]
    self.act(rn, ps.h[:, 0:TT], AF.Sqrt, [ps, pool.buf(sq)], [pool.buf(sq)], bias=1e-6)
    P.op("dve", lambda e: e.reciprocal(out=rn, in_=rn), [pool.buf(sq)], [pool.buf(sq)])
    self.stt(xf, xf, float(scale), rn, ALU.mult, ALU.mult, [pool.buf(xo), pool.buf(sq)], [pool.buf(xo)])
    pool.put(sq)


def tri_inverse_T(self, a_, u_, n=4):
    P, pool = self.P, self.pool
    W_ = n * 128
    ident = self.consts.h[:, C_IDENT:C_IDENT + 128]
    r_ = pool.get()
    rf = pool.f32(r_)
    for h in range(n):
        self.tt("dve", rf[:, h * 128:(h + 1) * 128], ident, pool.f32(u_)[:, h * 128:(h + 1) * 128], ALU.subtract,
                [self.consts, pool.buf(u_)], [pool.buf(r_)])
    p_, q_ = u_, a_
    for k in range(1, 7):
        last = k == 6
        q2 = pool.get()
        psq = self.next_ps()
        P.op("pe", [self.mm(psq.h[:, h * 128:(h + 1) * 128], pool.f32(p_)[:, h * 128:(h + 1) * 128],
                            pool.f32(q_)[:, h * 128:(h + 1) * 128], True, True) for h in range(n)],
             [pool.buf(p_), pool.buf(q_)], [psq])
        self.cp("act", pool.f32(q2)[:, 0:W_], psq.h[:, 0:W_], [psq], [pool.buf(q2)])
        if not last:
            p2 = pool.get()
            psp = self.next_ps()
            P.op("pe", [self.mm(psp.h[:, h * 128:(h + 1) * 128], pool.f32(q_)[:, h * 128:(h + 1) * 128],
                                pool.f32(p_)[:, h * 128:(h + 1) * 128], True, True) for h in range(n)],
                 [pool.buf(p_), pool.buf(q_)], [psp])
            self.cp("dve", pool.f32(p2)[:, 0:W_], psp.h[:, 0:W_], [psp], [pool.buf(p2)])
        psr = self.next_ps()
        P.op("pe", [self.mm(psr.h[:, h * 128:(h + 1) * 128], pool.f32(q2)[:, h * 128:(h + 1) * 128],
                            rf[:, h * 128:(h + 1) * 128], True, True) for h in range(n)],
             [pool.buf(q2), pool.buf(r_)], [psr])
        self.tt("dve", rf[:, 0:W_], rf[:, 0:W_], psr.h[:, 0:W_], ALU.add, [pool.buf(r_), psr], [pool.buf(r_)])
        pool.put(p_, q_)
        if not last:
            p_, q_ = p2, q2
        else:
            pool.put(q2)
    return r_


def gdn(self, l, t):
    P, cfg, pool = self.P, self.cfg, self.pool
    TT = cfg.TT
    NCH = TT // 128
    pl = self.pp[l]
    S, Sb = self.gd_S[l], self.gd_Sb[l]
    for h in range(4):
        self.load_rowbcast(self.rown, h * 128, self.rb_d[l:l + 1, RB_OFF["gdn_norm"]:RB_OFF["gdn_norm"] + 128])
    wz = self.load_w(self.w_in_d[l, :, GDN0 + 1536:GDN0 + 2056], ncols=520)
    ps_a = self.inproj_fm(wz, 512, 4)
    ps_b = self.inproj_fm(wz, 516, 4)
    sl = [pool.get() for _ in range(5)]
    r_beta, r_g, r_ng, r_bg, r_kd = [pool.f32(s)[0:4, 0:TT] for s in sl]
    b_beta, b_g, b_ng, b_bg, b_kd = [pool.buf(s) for s in sl]
    self.act(r_beta, ps_b.h[0:4, 0:TT], AF.Sigmoid, [ps_b], [b_beta])
    self.act(r_ng, ps_a.h[0:4, 0:TT], AF.Exp, [ps_a, pl], [b_ng], bias=self.ppc(l, "gdn_dt_bias")[0:4])
    self.act(r_ng, r_ng, AF.Ln, [b_ng], [b_ng], bias=1.0)
    self.ts("dve", r_ng, r_ng, self.gd_sc[l].h[0:4, 0:1], None, ALU.mult, None, [b_ng, self.gd_sc[l]], [b_ng])
    cm = self.consts.h[0:4, C_CMASK:C_CMASK + TT]
    P.op("dve", lambda e: e.tensor_tensor_scan(out=r_g, data0=cm, data1=r_ng, initial=0.0, op0=ALU.mult, op1=ALU.add),
         [self.consts, b_ng], [b_g])
    self.ts("dve", r_ng, r_g, -1.0, None, ALU.mult, None, [b_g], [b_ng])
    self.act(r_bg, r_g, AF.Exp, [b_g], [b_bg])
    self.tt("dve", r_bg, r_bg, r_beta, ALU.mult, [b_bg, b_beta], [b_bg])
    for c in range(NCH):
        self.ts("dve", r_kd[:, c * 128:(c + 1) * 128], r_ng[:, c * 128:(c + 1) * 128], r_g[:, c * 128 + 127:c * 128 + 128],
                None, ALU.add, None, [b_ng, b_g], [b_kd])
    self.act(r_kd, r_kd, AF.Exp, [b_kd], [b_kd])
    rows_to_tok(self, [(r_ng, b_ng), (r_beta, b_beta), (r_bg, b_bg), (r_kd, b_kd)], 0)

    def tk(q, c, h):
        o = (q * NCH + c) * 4 + h
        return self.tok.h[:, o:o + 1]
    gbc = []
    for h in range(4):
        ps = gate_rows_to_bcast(self, r_g, b_g, h)
        o = pool.get()
        self.cp("act", pool.f32(o)[:, 0:TT], ps.h[:, 0:TT], [ps], [pool.buf(o)])
        gbc.append(o)
    pool.put(*sl)
    gzs = [pool.get() for _ in range(NCH // 2)]

    def gz(c):
        return pool.bf(gzs[c // 2])[:, (c % 2) * 512:(c % 2) * 512 + 512], pool.buf(gzs[c // 2])
    for c in range(NCH):
        ps_z = self.inproj_tm(wz, 0, 512, c)
        zt = pool.get()
        self.act(pool.f32(zt), ps_z.h[:, 0:512], AF.Silu, [ps_z], [pool.buf(zt)])
        ga, gb = gz(c)
        self.tt("dve", ga, pool.f32(zt), self.rown.h[:, 0:512], ALU.mult, [pool.buf(zt), self.rown], [gb])
        pool.put(zt)
    wq = self.load_w(self.w_in_d[l, :, GDN0:GDN0 + 512])
    wk = self.load_w(self.w_in_d[l, :, GDN0 + 512:GDN0 + 1024])
    wv = self.load_w(self.w_in_d[l, :, GDN0 + 1024:GDN0 + 1536])
    qk = [pool.get() for _ in range(4)]
    vq = [pool.get() for _ in range(4)]
    for h in range(4):
        ps = self.inproj_fm(wq, h * 128, 128)
        o = self.conv4(l, ps, 128, "gdn_conv", h, self.halo[l], self.halo[l].bufs[h], h * 3)
        qf = pool.f32(o)[:, 0:TT]
        self.act(qf, qf, AF.Silu, [pool.buf(o)], [pool.buf(o)])
        l2norm_fm(self, o, 128.0 ** -0.5)
        self.cp("act", pool.bf(qk[h])[:, 0:TT], qf, [pool.buf(o)], [pool.buf(qk[h])])
        e_ = pool.get()
        self.act(pool.f32(e_)[:, 0:TT], pool.f32(gbc[h])[:, 0:TT], AF.Exp, [pool.buf(gbc[h])], [pool.buf(e_)])
        self.tt("dve", pool.bf(vq[h])[:, 512:512 + TT], qf, pool.f32(e_)[:, 0:TT], ALU.mult,
                [pool.buf(o), pool.buf(e_)], [pool.buf(vq[h])])
        pool.put(o, e_)
        ps = self.inproj_fm(wk, h * 128, 128)
        o = self.conv4(l, ps, 128, "gdn_conv", 4 + h, self.halo[l], self.halo[l].bufs[4 + h], (4 + h) * 3)
        kf = pool.f32(o)[:, 0:TT]
        self.act(kf, kf, AF.Silu, [pool.buf(o)], [pool.buf(o)])
        l2norm_fm(self, o, 1.0)
        self.cp("act", pool.bf(qk[h])[:, 512:512 + TT], kf, [pool.buf(o)], [pool.buf(qk[h])])
        pool.put(o)
        ps = self.inproj_fm(wv, h * 128, 128)
        o = self.conv4(l, ps, 128, "gdn_conv", 8 + h, self.halo[l], self.halo[l].bufs[8 + h], (8 + h) * 3)
        vf = pool.f32(o)[:, 0:TT]
        self.act(pool.bf(vq[h])[:, 0:TT], vf, AF.Silu, [pool.buf(o)], [pool.buf(vq[h])])
        pool.put(o)
    ident = self.consts.h[:, C_IDENT:C_IDENT + 128]
    for c in range(NCH):
        cs = slice(c * 128, (c + 1) * 128)
        ks = slice(512 + c * 128, 512 + (c + 1) * 128)
        kk_ps = self.next_ps()
        P.op("pe", [self.mm(kk_ps.h[:, h * 128:(h + 1) * 128], pool.bf(qk[h])[:, ks], pool.bf(qk[h])[:, ks], True, True)
                    for h in range(4)], [pool.buf(qk[h]) for h in range(4)], [kk_ps])
        kq_ps = self.next_ps()
        P.op("pe", [self.mm(kq_ps.h[:, h * 128:(h + 1) * 128], pool.bf(qk[h])[:, ks], pool.bf(qk[h])[:, cs], True, True)
                    for h in range(4)], [pool.buf(qk[h]) for h in range(4)], [kq_ps])
        a_ = pool.get()
        af = pool.f32(a_)
        d_ = pool.get()
        df = pool.f32(d_)
        for h in range(4):
            hs = slice(h * 128, (h + 1) * 128)
            self.ts("dve", af[:, hs], pool.f32(gbc[h])[:, cs], tk(0, c, h), 0.0, ALU.add, ALU.max,
                    [pool.buf(gbc[h]), self.tok], [pool.buf(a_)])
            self.act(af[:, hs], af[:, hs], AF.Exp, [pool.buf(a_)], [pool.buf(a_)], scale=-1.0)
            self.stt(af[:, hs], af[:, hs], tk(1, c, h), self.consts.h[:, C_MSL:C_MSL + 128], ALU.mult, ALU.mult,
                     [pool.buf(a_), self.tok, self.consts], [pool.buf(a_)])
            self.ts("dve", df[:, hs], pool.f32(gbc[h])[:, cs], tk(0, c, h), 0.0, ALU.add, ALU.min,
                    [pool.buf(gbc[h]), self.tok], [pool.buf(d_)])
            self.act(df[:, hs], df[:, hs], AF.Exp, [pool.buf(d_)], [pool.buf(d_)])
            self.tt("dve", df[:, hs], df[:, hs], self.consts.h[:, C_MIU:C_MIU + 128], ALU.mult,
                    [pool.buf(d_), self.consts], [pool.buf(d_)])
        self.tt("dve", af, af, kk_ps.h[:, 0:512], ALU.mult, [pool.buf(a_), kk_ps], [pool.buf(a_)])
        at_ = pool.get()
        atb = pool.bf(at_)[:, 0:512]
        self.tt("dve", atb, df, kq_ps.h[:, 0:512], ALU.mult, [pool.buf(d_), kq_ps], [pool.buf(at_)])
        pool.put(d_)
        u_ = pool.get()
        ut_ps = self.next_ps()
        P.op("pe", [self.trn(ut_ps.h[:, h * 128:(h + 1) * 128], af[:, h * 128:(h + 1) * 128], ident) for h in range(4)],
             [pool.buf(a_), self.consts], [ut_ps])
        self.cp("act", pool.f32(u_), ut_ps.h[:, 0:512], [ut_ps], [pool.buf(u_)])
        r_ = tri_inverse_T(self, a_, u_)
        rf = pool.f32(r_)
        kt_ps = self.next_ps()
        ktb = kt_ps.h[:].bitcast(BF16)
        P.op("pe", [self.trn(ktb[:, h * 128:(h + 1) * 128], pool.bf(qk[h])[:, ks], self.ident_b.h[:]) for h in range(4)],
             [pool.buf(qk[h]) for h in range(4)] + [self.ident_b], [kt_ps])
        vt_ps = self.next_ps()
        vtb = vt_ps.h[:].bitcast(BF16)
        P.op("pe", [self.trn(vtb[:, h * 128:(h + 1) * 128], pool.bf(vq[h])[:, cs], self.ident_b.h[:]) for h in range(4)],
             [pool.buf(vq[h]) for h in range(4)] + [self.ident_b], [vt_ps])
        vb_, kg_, kd_ = pool.get(), pool.get(), pool.get()
        for h in range(4):
            hs = slice(h * 128, (h + 1) * 128)
            self.ts("dve", pool.f32(vb_)[:, hs], vtb[:, hs], tk(1, c, h), None, ALU.mult, None, [vt_ps, self.tok], [pool.buf(vb_)])
            self.ts("dve", pool.f32(kg_)[:, hs], ktb[:, hs], tk(2, c, h), None, ALU.mult, None, [kt_ps, self.tok], [pool.buf(kg_)])
            self.ts("dve", pool.bf(kd_)[:, hs], ktb[:, hs], tk(3, c, h), None, ALU.mult, None, [kt_ps, self.tok], [pool.buf(kd_)])
        u_ps = self.next_ps()
        P.op("pe", [self.mm(u_ps.h[:, h * 128:(h + 1) * 128], rf[:, h * 128:(h + 1) * 128], pool.f32(vb_)[:, h * 128:(h + 1) * 128],
                            True, True) for h in range(4)], [pool.buf(r_), pool.buf(vb_)], [u_ps])
        w_ps = self.next_ps()
        P.op("pe", [self.mm(w_ps.h[:, h * 128:(h + 1) * 128], pool.f32(kg_)[:, h * 128:(h + 1) * 128], rf[:, h * 128:(h + 1) * 128],
                            True, True) for h in range(4)], [pool.buf(r_), pool.buf(kg_)], [w_ps])
        self.cp("act", pool.f32(vb_), u_ps.h[:, 0:512], [u_ps], [pool.buf(vb_)])
        self.cp("dve", pool.bf(kg_)[:, 0:512], w_ps.h[:, 0:512], [w_ps], [pool.buf(kg_)])
        pool.put(r_)
        uf = pool.f32(vb_)
        wtb = pool.bf(kg_)[:, 0:512]
        kdb = pool.bf(kd_)[:, 0:512]
        y_ = pool.get()
        yb = pool.bf(y_)[:, 0:512]
        vn_ = pool.get()
        ga, gb = gz(c)
        for h in range(4):
            hs = slice(h * 128, (h + 1) * 128)
            ws_ps = self.next_ps()
            P.op("pe", [self.mm(ws_ps.h[:, 0:128], wtb[:, hs], Sb.h[:, h, :], True, True)], [pool.buf(kg_), Sb.bufs[h]], [ws_ps])
            vnb = pool.bf(vn_)[:, hs]
            self.tt("dve", vnb, uf[:, hs], ws_ps.h[:, 0:128], ALU.subtract, [pool.buf(vb_), ws_ps], [pool.buf(vn_)])
            o_ps = self.next_ps()
            P.op("pe", [self.mm(o_ps.h[:, 0:128], pool.bf(vq[h])[:, ks], Sb.h[:, h, :], True, False),
                        self.mm(o_ps.h[:, 0:128], atb[:, hs], vnb, False, True)],
                 [pool.buf(vq[h]), Sb.bufs[h], pool.buf(at_), pool.buf(vn_)], [o_ps])
            up_ps = self.next_ps()
            P.op("pe", [self.mm(up_ps.h[:, 0:128], kdb[:, hs], vnb, True, True)], [pool.buf(kd_), pool.buf(vn_)], [up_ps])
            eg, egb = self.scal()
            self.act(eg, pool.f32(gbc[h])[:, c * 128 + 127:c * 128 + 128], AF.Exp, [pool.buf(gbc[h])], [egb])
            self.stt(S.h[:, h, :], S.h[:, h, :], eg, up_ps.h[:, 0:128], ALU.mult, ALU.add, [S.bufs[h], egb, up_ps], [S.bufs[h]])
            self.cp("act", Sb.h[:, h, :], S.h[:, h, :], [S.bufs[h]], [Sb.bufs[h]])
            ss, ssb = self.scal()
            j_ = pool.get()
            self.act(pool.f32(j_)[:, 0:128], o_ps.h[:, 0:128], AF.Square, [o_ps], [pool.buf(j_), ssb], accum_out=ss)
            pool.put(j_)
            self.act(ss, ss, AF.Sqrt, [ssb], [ssb], bias=NORM_EPS, scale=1.0 / 128)
            P.op("dve", lambda e, ss=ss: e.reciprocal(out=ss, in_=ss), [ssb], [ssb])
            self.stt(yb[:, hs], o_ps.h[:, 0:128], ss, ga[:, hs], ALU.mult, ALU.mult, [o_ps, ssb, gb], [pool.buf(y_)])
        yt_ps = self.next_ps()
        ytb = yt_ps.h[:].bitcast(BF16)
        P.op("pe", [self.trn(ytb[:, h * 128:(h + 1) * 128], yb[:, h * 128:(h + 1) * 128], self.ident_b.h[:]) for h in range(4)],
             [pool.buf(y_), self.ident_b], [yt_ps])
        self.cp("act", self.mixT.h[:, 0:4, cs], ytb[:, 0:512].rearrange("p (h c) -> p h c", h=4), [yt_ps],
                [self.mixT.bufs[h] for h in range(4)])
        pool.put(at_, vb_, kg_, kd_, y_, vn_)
    pool.put(*gbc)
    pool.put(*gzs)
    pool.put(*qk)
    pool.put(*vq)


Kern.gdn = gdn

RW_LW = 0.6065306597126334


def setup_rwkv(self):
    P, cfg = self.P, self.cfg
    L = cfg.depth
    self.rw_M = [P.sb("rw_M%d" % l, [128, 4, 64], F32, nbufs=4) for l in range(L)]
    self.rw_Mb = [P.sb("rw_Mb%d" % l, [128, 4, 64], BF16, nbufs=4) for l in range(L)]
    self.rw_halo = [P.sb("rw_halo%d" % l, [128, 14], F32, nbufs=14) for l in range(L)]
    self.rw_sc = [P.sb("rw_sc%d" % l, [128, 4], F32) for l in range(L)]
    self.rw_w2p = P.sb("rw_w2p", [128, 512], BF16)
    self.rw_a2p = P.sb("rw_a2p", [128, 512], BF16)
    self.rw_g2 = P.sb("rw_g2", [128, 512], BF16)
    self.rowb = P.sb("rowb", [128, 512], F32)
    self.blk_b = P.sb("blk_b", [128, 128], BF16)
    self.sel2_b = P.sb("sel2_b", [128, 2], BF16)
    self.rkd = P.sb("rkd", [128, 4, 8], F32, nbufs=4)
    self.gC = P.sb("gC", [128, 4], F32)
    P.op("pool", lambda e: e.memset(self.rw_w2p.h[:], 0.0), [], [self.rw_w2p])
    P.op("pool", lambda e: e.memset(self.rw_a2p.h[:], 0.0), [], [self.rw_a2p])
    P.op("pool", lambda e: e.memset(self.blk_b.h[:], 0.0), [], [self.blk_b])
    P.op("pool", lambda e: e.memset(self.blk_b.h[0:64, 0:64], 1.0), [], [self.blk_b])
    P.op("pool", lambda e: e.memset(self.blk_b.h[64:128, 64:128], 1.0), [], [self.blk_b])
    self.cp("dve", self.sel2_b.h[:], self.consts.h[:, C_SEL2:C_SEL2 + 2], [self.consts], [self.sel2_b])
    for l in range(L):
        P.op("pool", lambda e, l=l: e.memset(self.rw_M[l].h[:], 0.0), [], self.rw_M[l].bufs)
        P.op("pool", lambda e, l=l: e.memset(self.rw_Mb[l].h[:], 0.0), [], self.rw_Mb[l].bufs)
        P.op("pool", lambda e, l=l: e.memset(self.rw_halo[l].h[:], 0.0), [], self.rw_halo[l].bufs)
        ka = self.pp[l].h[:, PP_OFF["rwkv_k_a"]:PP_OFF["rwkv_k_a"] + 4]
        self.ts("dve", self.rw_sc[l].h[:], ka, -1.0, 1.0, ALU.mult, ALU.add, [self.pp[l]], [self.rw_sc[l]])


def shift_lerp(self, l, ps, ci):
    pool = self.pool
    TT = self.cfg.TT
    cb = self.cbuf[self.cb_rr % 2]
    self.cb_rr += 1
    hl = self.rw_halo[l]
    self.cp("act", cb.h[:, 1:1 + TT], ps.h[:, 0:TT], [ps], [cb])
    self.cp("dve", cb.h[:, 0:1], hl.h[:, ci:ci + 1], [hl.bufs[ci]], [cb])
    o = pool.get()
    of = pool.f32(o)[:, 0:TT]
    self.tt("dve", of, cb.h[:, 0:TT], cb.h[:, 1:1 + TT], ALU.subtract, [cb], [pool.buf(o)])
    self.stt(of, of, self.ppc(l, "rwkv_mu", ci), cb.h[:, 1:1 + TT], ALU.mult, ALU.add, [pool.buf(o), self.pp[l], cb], [pool.buf(o)])
    self.cp("dve", hl.h[:, ci:ci + 1], cb.h[:, TT:TT + 1], [cb], [hl.bufs[ci]])
    return o


def tri_inverse_bf(self, a_, u_, n=2):
    P, pool = self.P, self.pool
    W_ = n * 128
    ident = self.consts.h[:, C_IDENT:C_IDENT + 128]
    r_ = pool.get()
    rf = pool.f32(r_)[:, 0:W_]
    rb = pool.bf(r_)[:, 512:512 + W_]
    for h in range(n):
        self.tt("dve", rf[:, h * 128:(h + 1) * 128], ident, pool.bf(u_)[:, h * 128:(h + 1) * 128], ALU.subtract,
                [self.consts, pool.buf(u_)], [pool.buf(r_)])
    self.cp("act", rb, rf, [pool.buf(r_)], [pool.buf(r_)])
    p_, q_ = u_, a_
    for k in range(1, 7):
        last = k == 6
        q2 = pool.get()
        psq = self.next_ps()
        P.op("pe", [self.mm(psq.h[:, h * 128:(h + 1) * 128], pool.bf(p_)[:, h * 128:(h + 1) * 128],
                            pool.bf(q_)[:, h * 128:(h + 1) * 128], True, True) for h in range(n)],
             [pool.buf(p_), pool.buf(q_)], [psq])
        self.cp("act", pool.bf(q2)[:, 0:W_], psq.h[:, 0:W_], [psq], [pool.buf(q2)])
        if not last:
            p2 = pool.get()
            psp = self.next_ps()
            P.op("pe", [self.mm(psp.h[:, h * 128:(h + 1) * 128], pool.bf(q_)[:, h * 128:(h + 1) * 128],
                                pool.bf(p_)[:, h * 128:(h + 1) * 128], True, True) for h in range(n)],
                 [pool.buf(p_), pool.buf(q_)], [psp])
            self.cp("dve", pool.bf(p2)[:, 0:W_], psp.h[:, 0:W_], [psp], [pool.buf(p2)])
        psr = self.next_ps()
        P.op("pe", [self.mm(psr.h[:, h * 128:(h + 1) * 128], pool.bf(q2)[:, h * 128:(h + 1) * 128],
                            rb[:, h * 128:(h + 1) * 128], True, True) for h in range(n)],
             [pool.buf(q2), pool.buf(r_)], [psr])
        self.tt("dve", rf, rf, psr.h[:, 0:W_], ALU.add, [pool.buf(r_), psr], [pool.buf(r_)])
        self.cp("act", rb, rf, [pool.buf(r_)], [pool.buf(r_)])
        pool.put(p_, q_)
        if not last:
            p_, q_ = p2, q2
        else:
            pool.put(q2)
    return r_


def rwkv(self, l, t):
    import os
    STOP = float(os.environ.get("RWKV_STOP", "99"))
    P, cfg, pool = self.P, self.cfg, self.pool
    TT = cfg.TT
    NCH = TT // 128
    pl = self.pp[l]
    M, Mb = self.rw_M[l], self.rw_Mb[l]
    ident = self.consts.h[:, C_IDENT:C_IDENT + 128]
    msl = self.consts.h[:, C_MSL:C_MSL + 128]
    msu = self.consts.h[:, C_MSU:C_MSU + 128]
    miu = self.consts.h[:, C_MIU:C_MIU + 128]
    P.dma("pool", self.rw_w2p.h[0:64, :], self.rw_w2_d[l], writes=[self.rw_w2p])
    P.dma("pool", self.rw_a2p.h[64:128, :], self.rw_a2_d[l], writes=[self.rw_a2p])
    P.dma("pool", self.rw_g2.h[:], self.rw_g2_d[l], writes=[self.rw_g2])
    self.load_rowbcast(self.rown, 0, self.rb_d[l:l + 1, RB_OFF["rwkv_ln_w"]:RB_OFF["rwkv_ln_w"] + 512])
    self.load_rowbcast(self.rowb, 0, self.rb_d[l:l + 1, RB_OFF["rwkv_ln_b"]:RB_OFF["rwkv_ln_b"] + 512])
    wl = self.load_w(self.w_in_d[l, :, RW0 + 1536:RW0 + 1792], ncols=256)
    ps = self.inproj_fm(wl, 0, 128)
    lo = shift_lerp(self, l, ps, 12)
    lo_ = pool.get()
    self.act(pool.bf(lo_)[:, 0:TT], pool.f32(lo)[:, 0:TT], AF.Tanh, [pool.buf(lo)], [pool.buf(lo_)])
    self.cp("dve", pool.bf(lo_)[:, 512:512 + TT], pool.f32(lo)[:, 0:TT], [pool.buf(lo)], [pool.buf(lo_)])
    pool.put(lo)
    ps = self.inproj_fm(wl, 128, 128)
    go = shift_lerp(self, l, ps, 13)
    sg_ = pool.get()
    self.act(pool.bf(sg_)[:, 0:TT], pool.f32(go)[:, 0:TT], AF.Sigmoid, [pool.buf(go)], [pool.buf(sg_)])
    pool.put(go)
    if STOP <= 1:
        return
    wr = self.load_w(self.w_in_d[l, :, RW0:RW0 + 512])
    wk = self.load_w(self.w_in_d[l, :, RW0 + 512:RW0 + 1024])
    wv = self.load_w(self.w_in_d[l, :, RW0 + 1024:RW0 + 1536])
    ytok = [pool.get() for _ in range(NCH)]
    vtk = [pool.get() for _ in range(NCH // 2)]

    def vtok(b):
        return pool.bf(vtk[b // 2])[:, (b % 2) * 512:(b % 2) * 512 + 512], pool.buf(vtk[b // 2])
    for cp in range(4):
        pcs = slice(cp * 128, (cp + 1) * 128)
        ps = self.inproj_fm(wr, cp * 128, 128)
        r_ = shift_lerp(self, l, ps, cp)
        ps = self.inproj_fm(wk, cp * 128, 128)
        k_ = shift_lerp(self, l, ps, 4 + cp)
        ps = self.inproj_fm(wv, cp * 128, 128)
        v_ = shift_lerp(self, l, ps, 8 + cp)
        rf, kf, vf = [pool.f32(s)[:, 0:TT] for s in (r_, k_, v_)]
        vk_ = pool.get()
        self.cp("act", pool.bf(vk_)[:, 0:TT], vf, [pool.buf(v_)], [pool.buf(vk_)])
        pool.put(v_)
        ps_w = self.next_ps()
        P.op("pe", [self.mm(ps_w.h[:, 0:TT], self.rw_w2p.h[:, pcs], pool.bf(lo_)[:, 0:TT], True, True)],
             [self.rw_w2p, pool.buf(lo_)], [ps_w])
        sw_ = pool.get()
        swf = pool.f32(sw_)[:, 0:TT]
        self.act(swf, ps_w.h[:, 0:TT], AF.Sigmoid, [ps_w, pl], [pool.buf(sw_)], bias=self.ppc(l, "rwkv_w0", cp))
        ps_a = self.next_ps()
        P.op("pe", [self.mm(ps_a.h[:, 0:TT], self.rw_a2p.h[:, pcs], pool.bf(lo_)[:, 512:512 + TT], True, True)],
             [self.rw_a2p, pool.buf(lo_)], [ps_a])
        a_ = pool.get()
        af = pool.f32(a_)[:, 0:TT]
        self.act(af, ps_a.h[:, 0:TT], AF.Sigmoid, [ps_a, pl], [pool.buf(a_)], bias=self.ppc(l, "rwkv_a0", cp))
        kk_ = pool.get()
        kkf = pool.f32(kk_)[:, 0:TT]
        self.ts("dve", kkf, kf, self.ppc(l, "rwkv_k_k", cp), None, ALU.mult, None, [pool.buf(k_), pl], [pool.buf(kk_)])
        sq = pool.get()
        self.act(pool.bf(sq)[:, 0:TT], kkf, AF.Square, [pool.buf(kk_)], [pool.buf(sq)])
        pss = self.next_ps()
        P.op("pe", [self.mm(pss.h[:, 0:TT], self.blk_b.h[:], pool.bf(sq)[:, 0:TT], True, True)], [self.blk_b, pool.buf(sq)], [pss])
        rn = pool.f32(sq)[:, 0:TT]
        self.act(rn, pss.h[:, 0:TT], AF.Sqrt, [pss, pool.buf(sq)], [pool.buf(sq)], bias=1e-6)
        P.op("dve", lambda e, rn=rn: e.reciprocal(out=rn, in_=rn), [pool.buf(sq)], [pool.buf(sq)])
        self.tt("dve", kkf, kkf, rn, ALU.mult, [pool.buf(kk_), pool.buf(sq)], [pool.buf(kk_)])
        fac = pool.f32(sq)[:, 0:TT]
        self.ts("dve", fac, af, self.ppc(l, "rwkv_k_a", cp), self.rw_sc[l].h[:, cp:cp + 1], ALU.mult, ALU.add,
                [pool.buf(a_), pl, self.rw_sc[l], pool.buf(sq)], [pool.buf(sq)])
        self.tt("dve", kf, kf, fac, ALU.mult, [pool.buf(k_), pool.buf(sq)], [pool.buf(k_)])
        self.stt(pool.bf(vk_)[:, 512:512 + TT], rf, self.ppc(l, "rwkv_r_k", cp), kf, ALU.mult, ALU.mult,
                 [pool.buf(r_), pl, pool.buf(k_)], [pool.buf(vk_)])
        self.tt("dve", af, af, kkf, ALU.mult, [pool.buf(a_), pool.buf(kk_)], [pool.buf(a_)])
        if STOP <= 2:
            continue
        cs_ = pool.get()
        csf = pool.f32(cs_)[:, 0:TT]
        cm = self.consts.h[:, C_CMASK:C_CMASK + TT]
        P.op("dve", lambda e, csf=csf, cm=cm, swf=swf: e.tensor_tensor_scan(out=csf, data0=cm, data1=swf, initial=0.0,
                                                                               op0=ALU.mult, op1=ALU.add),
             [self.consts, pool.buf(sw_)], [pool.buf(cs_)])
        self.tt("dve", swf, csf, swf, ALU.subtract, [pool.buf(cs_), pool.buf(sw_)], [pool.buf(sw_)])
        if STOP <= 2.2:
            continue
        ex = pool.f32(sq)[:, 0:TT]
        for b in range(NCH):
            self.act(self.gC.h[:, b:b + 1], csf[:, b * 128 + 127:b * 128 + 128], AF.Exp, [pool.buf(cs_)], [self.gC], scale=-RW_LW)
        if STOP <= 2.4:
            continue
        ar_ = [pool.get(), pool.get()]
        for e in range(2):
            zap = pool.bf(ar_[e])[(1 - e) * 64:(2 - e) * 64, :]
            P.op("pool", lambda en, zap=zap: en.memset(zap, 0.0), [], [pool.buf(ar_[e])])
        if STOP <= 2.6:
            continue
        self.act(ex, swf, AF.Exp, [pool.buf(sw_), pool.buf(sq)], [pool.buf(sq)], scale=-RW_LW)
        for e in range(2):
            hp = slice(e * 64, (e + 1) * 64)
            self.stt(pool.bf(ar_[e])[hp, 0:TT], kkf[hp], -1.0, ex[hp], ALU.mult, ALU.mult,
                     [pool.buf(kk_), pool.buf(sq)], [pool.buf(ar_[e])])
        if STOP <= 2.8:
            continue
        self.act(ex, csf, AF.Exp, [pool.buf(cs_), pool.buf(sq)], [pool.buf(sq)], scale=-RW_LW)
        for e in range(2):
            hp = slice(e * 64, (e + 1) * 64)
            self.tt("dve", pool.bf(ar_[e])[hp, 512:512 + TT], rf[hp], ex[hp], ALU.mult,
                    [pool.buf(r_), pool.buf(sq)], [pool.buf(ar_[e])])
        self.act(ex, csf, AF.Exp, [pool.buf(cs_), pool.buf(sq)], [pool.buf(sq)], scale=RW_LW)
        bk_ = pool.get()
        self.tt("dve", pool.bf(bk_)[:, 0:TT], af, ex, ALU.mult, [pool.buf(a_), pool.buf(sq)], [pool.buf(bk_)])
        self.tt("dve", pool.bf(bk_)[:, 512:512 + TT], kf, ex, ALU.mult, [pool.buf(k_), pool.buf(sq)], [pool.buf(bk_)])
        pool.put(r_, k_, sw_, a_, kk_, sq, cs_)
        bt = pool.bf(bk_)[:, 0:TT]
        kt = pool.bf(bk_)[:, 512:512 + TT]
        for b in range(NCH if STOP > 3 else 0):
            bs = slice(b * 128, (b + 1) * 128)
            at = [pool.bf(ar_[e])[:, b * 128:(b + 1) * 128] for e in range(2)]
            rt = [pool.bf(ar_[e])[:, 512 + b * 128:512 + (b + 1) * 128] for e in range(2)]
            arb = [pool.buf(ar_[0]), pool.buf(ar_[1])]
            bkb = pool.buf(bk_)
            l_ps, u_ps, ak_ps, rb_ps, rk_ps = [self.next_ps() for _ in range(5)]
            P.op("pe", [self.mm(l_ps.h[:, e * 128:(e + 1) * 128], at[e], bt[:, bs], True, True) for e in range(2)], arb + [bkb], [l_ps])
            P.op("pe", [self.mm(u_ps.h[:, e * 128:(e + 1) * 128], bt[:, bs], at[e], True, True) for e in range(2)], arb + [bkb], [u_ps])
            P.op("pe", [self.mm(ak_ps.h[:, e * 128:(e + 1) * 128], kt[:, bs], at[e], True, True) for e in range(2)], arb + [bkb], [ak_ps])
            P.op("pe", [self.mm(rb_ps.h[:, e * 128:(e + 1) * 128], bt[:, bs], rt[e], True, True) for e in range(2)], arb + [bkb], [rb_ps])
            P.op("pe", [self.mm(rk_ps.h[:, e * 128:(e + 1) * 128], kt[:, bs], rt[e], True, True) for e in range(2)], arb + [bkb], [rk_ps])
            la_, ua_, m3_ = pool.get(), pool.get(), pool.get()
            m3 = pool.bf(m3_)
            for e in range(2):
                es = slice(e * 128, (e + 1) * 128)
                self.stt(pool.bf(la_)[:, es], l_ps.h[:, es], -1.0, msl, ALU.mult, ALU.mult, [l_ps, self.consts], [pool.buf(la_)])
                self.stt(pool.bf(ua_)[:, es], u_ps.h[:, es], -1.0, msu, ALU.mult, ALU.mult, [u_ps, self.consts], [pool.buf(ua_)])
                self.tt("dve", m3[:, e * 128:(e + 1) * 128], ak_ps.h[:, es], msu, ALU.mult, [ak_ps, self.consts], [pool.buf(m3_)])
                self.tt("dve", m3[:, 256 + e * 128:256 + (e + 1) * 128], rb_ps.h[:, es], miu, ALU.mult, [rb_ps, self.consts], [pool.buf(m3_)])
                self.tt("dve", m3[:, 512 + e * 128:512 + (e + 1) * 128], rk_ps.h[:, es], miu, ALU.mult, [rk_ps, self.consts], [pool.buf(m3_)])
            if STOP <= 4:
                continue
            r2_ = tri_inverse_bf(self, la_, ua_, n=2)
            if STOP <= 5:
                continue
            tr_ps = self.next_ps()
            trb = tr_ps.h[:].bitcast(BF16)
            P.op("pe", [self.trn(trb[:, 0:128], bt[:, bs], self.ident_b.h[:]),
                        self.trn(trb[:, 128:256], kt[:, bs], self.ident_b.h[:]),
                        self.trn(trb[:, 256:384], pool.bf(vk_)[:, bs], self.ident_b.h[:])],
                 [bkb, pool.buf(vk_), self.ident_b], [tr_ps])
            tk_ = pool.get()
            tkb = pool.bf(tk_)
            self.cp("act", tkb[:, 0:384], trb[:, 0:384], [tr_ps], [pool.buf(tk_)])
            va, vbuf = vtok(b)
            self.cp("dve", va[:, pcs], trb[:, 256:384], [tr_ps], [vbuf])
            rk2 = self.next_ps()
            P.op("pe", [self.mm(rk2.h[:, 0:2], pool.bf(vk_)[:, 512 + b * 128:512 + (b + 1) * 128], self.sel2_b.h[:], True, True)],
                 [pool.buf(vk_), self.sel2_b], [rk2])
            self.cp("dve", self.rkd.h[:, b, cp * 2:cp * 2 + 2], rk2.h[:, 0:2], [rk2], [self.rkd.bufs[b]])
            if STOP <= 6:
                continue
            x_ps = self.next_ps()
            fns = []
            for e in range(2):
                es64 = slice(e * 64, (e + 1) * 64)
                fns.append(self.mm(x_ps.h[:, es64], at[e], Mb.h[:, cp, :], True, False))
                fns.append(self.mm(x_ps.h[:, es64], m3[:, e * 128:(e + 1) * 128], tkb[:, 256 + e * 64:256 + (e + 1) * 64], False, True))
            P.op("pe", fns, arb + [Mb.bufs[cp], pool.buf(m3_), pool.buf(tk_)], [x_ps])
            x_ = pool.get()
            xf = pool.bf(x_)[:, 0:128]
            self.cp("act", xf, x_ps.h[:, 0:128], [x_ps], [pool.buf(x_)])
            p_ps = self.next_ps()
            P.op("pe", [self.mm(p_ps.h[:, e * 64:(e + 1) * 64], pool.bf(r2_)[:, 512 + e * 128:512 + (e + 1) * 128], xf[:, e * 64:(e + 1) * 64], True, True)
                        for e in range(2)], [pool.buf(r2_), pool.buf(x_)], [p_ps])
            self.cp("act", tkb[:, 384:512], p_ps.h[:, 0:128], [p_ps], [pool.buf(tk_)])
            pool.put(r2_, x_)
            if STOP <= 7:
                continue
            y_ps = self.next_ps()
            fns = []
            for e in range(2):
                es64 = slice(e * 64, (e + 1) * 64)
                fns.append(self.mm(y_ps.h[:, es64], rt[e], Mb.h[:, cp, :], True, False))
                fns.append(self.mm(y_ps.h[:, es64], m3[:, 256 + e * 128:256 + (e + 1) * 128], tkb[:, 384 + e * 64:384 + (e + 1) * 64], False, False))
                fns.append(self.mm(y_ps.h[:, es64], m3[:, 512 + e * 128:512 + (e + 1) * 128], tkb[:, 256 + e * 64:256 + (e + 1) * 64], False, True))
            P.op("pe", fns, arb + [Mb.bufs[cp], pool.buf(m3_), pool.buf(tk_)], [y_ps])
            self.cp("act", pool.f32(ytok[b])[:, pcs], y_ps.h[:, 0:128], [y_ps], [pool.buf(ytok[b])])
            if STOP <= 8:
                continue
            m_ps = self.next_ps()
            fns = []
            for e in range(2):
                es64 = slice(e * 64, (e + 1) * 64)
                fns.append(self.mm(m_ps.h[:, es64], tkb[:, 0:128], tkb[:, 384 + e * 64:384 + (e + 1) * 64], True, False))
                fns.append(self.mm(m_ps.h[:, es64], tkb[:, 128:256], tkb[:, 256 + e * 64:256 + (e + 1) * 64], False, True))
            P.op("pe", fns, [pool.buf(tk_)], [m_ps])
            for e in range(2):
                es64 = slice(e * 64, (e + 1) * 64)
                self.tt("dve", M.h[es64, cp, :], M.h[es64, cp, :], m_ps.h[es64, e * 64:(e + 1) * 64], ALU.add,
                        [M.bufs[cp], m_ps], [M.bufs[cp]])
            self.ts("dve", M.h[:, cp, :], M.h[:, cp, :], self.gC.h[:, b:b + 1], None, ALU.mult, None, [M.bufs[cp], self.gC], [M.bufs[cp]])
            self.cp("act", Mb.h[:, cp, :], M.h[:, cp, :], [M.bufs[cp]], [Mb.bufs[cp]])
            pool.put(m3_, tk_)
        pool.put(vk_, bk_, *ar_)
    pool.put(lo_)
    for b in range(NCH if STOP > 9 else 0):
        bs = slice(b * 128, (b + 1) * 128)
        yf = pool.f32(ytok[b])
        y3 = yf.rearrange("p (h i) -> p h i", h=8)
        st, stb = self.scal8()
        mean, ex2 = st[:, 0:8], st[:, 8:16]
        P.op("dve", lambda e, mean=mean, y3=y3: e.tensor_reduce(out=mean, in_=y3, axis=AX.X, op=ALU.add), [pool.buf(ytok[b])], [stb])
        t_ = pool.get()
        tf = pool.f32(t_)
        t3 = tf.rearrange("p (h i) -> p h i", h=8)
        self.act(tf, yf, AF.Square, [pool.buf(ytok[b])], [pool.buf(t_)])
        P.op("dve", lambda e, ex2=ex2, t3=t3: e.tensor_reduce(out=ex2, in_=t3, axis=AX.X, op=ALU.add), [pool.buf(t_)], [stb])
        self.ts("dve", mean, mean, 1.0 / 64, None, ALU.mult, None, [stb], [stb])
        self.ts("dve", ex2, ex2, 1.0 / 64, None, ALU.mult, None, [stb], [stb])
        msq = st[:, 16:24]
        self.tt("dve", msq, mean, mean, ALU.mult, [stb], [stb])
        self.tt("dve", ex2, ex2, msq, ALU.subtract, [stb], [stb])
        self.act(ex2, ex2, AF.Sqrt, [stb], [stb], bias=RWKV_LN_EPS)
        P.op("dve", lambda e, ex2=ex2: e.reciprocal(out=ex2, in_=ex2), [stb], [stb])
        for h in range(8):
            hs = slice(h * 64, (h + 1) * 64)
            self.ts("dve", tf[:, hs], yf[:, hs], mean[:, h:h + 1], ex2[:, h:h + 1], ALU.subtract, ALU.mult,
                    [pool.buf(ytok[b]), stb], [pool.buf(t_)])
        self.tt("dve", tf, tf, self.rown.h[:, 0:512], ALU.mult, [pool.buf(t_), self.rown], [pool.buf(t_)])
        self.tt("dve", tf, tf, self.rowb.h[:, 0:512], ALU.add, [pool.buf(t_), self.rowb], [pool.buf(t_)])
        va, vbuf = vtok(b)
        for h in range(8):
            hs = slice(h * 64, (h + 1) * 64)
            self.stt(tf[:, hs], va[:, hs], self.rkd.h[:, b, h:h + 1], tf[:, hs], ALU.mult, ALU.add,
                     [vbuf, self.rkd.bufs[b], pool.buf(t_)], [pool.buf(t_)])
        g_ps = self.next_ps()
        P.op("pe", [self.mm(g_ps.h[:, 0:512], pool.bf(sg_)[:, bs], self.rw_g2.h[:], True, True)], [pool.buf(sg_), self.rw_g2], [g_ps])
        yb_ = pool.get()
        yb = pool.bf(yb_)[:, 0:512]
        self.tt("dve", yb, tf, g_ps.h[:, 0:512], ALU.mult, [pool.buf(t_), g_ps], [pool.buf(yb_)])
        yt_ps = self.next_ps()
        ytb = yt_ps.h[:].bitcast(BF16)
        P.op("pe", [self.trn(ytb[:, c * 128:(c + 1) * 128], yb[:, c * 128:(c + 1) * 128], self.ident_b.h[:]) for c in range(4)],
             [pool.buf(yb_), self.ident_b], [yt_ps])
        self.cp("act", self.mixT.h[:, 12:16, bs], ytb[:, 0:512].rearrange("p (h c) -> p h c", h=4), [yt_ps],
                [self.mixT.bufs[12 + c] for c in range(4)])
        pool.put(t_, yb_)
    pool.put(sg_, *ytok)
    pool.put(*vtk)


def scal8(self):
    i = self.small_rr % 2
    self.small_rr += 1
    return self.small8.h[:, i * 32:(i + 1) * 32], self.small8.bufs[i]


Kern.rwkv = rwkv
Kern.scal8 = scal8
```

```python
import math
from contextlib import ExitStack

import numpy as np
import concourse.bass as bass
import concourse.mybir as mybir
from concourse.bass_utils import run_bass_kernel_spmd

F32 = mybir.dt.float32
BF16 = mybir.dt.bfloat16
AF = mybir.ActivationFunctionType
ALU = mybir.AluOpType
AX = mybir.AxisListType

D = 2048
NK = D // 128
W = 512
FFN = 5632
NF = FFN // 128
IN_COLS = 6928
GDN0 = 0
ML0 = 2056
RG0 = 4112
RW0 = 5136
NORM_EPS = 1e-6
RWKV_LN_EPS = 64e-5
RGLRU_C = 8.0


class Cfg:
    def __init__(self, T=4096, depth=2, TT=512, mixers=("gdn", "mlstm", "rglru", "rwkv")):
        self.T, self.depth, self.TT = T, depth, TT
        self.NB = TT // 128
        self.NT = T // TT
        self.mixers = mixers


PP_SPEC = [
    ("gdn_conv", 48), ("mlstm_conv", 32), ("rglru_conv", 16), ("rglru_conv_b", 4),
    ("rglru_b_a", 4), ("rglru_b_x", 4), ("rglru_lambda", 4), ("rwkv_mu", 14),
    ("rwkv_w0", 4), ("rwkv_a0", 4), ("rwkv_k_k", 4), ("rwkv_k_a", 4), ("rwkv_r_k", 4),
    ("gdn_a_log", 1), ("gdn_dt_bias", 1), ("mlstm_b_i", 1), ("mlstm_b_f", 1),
]
PP_OFF = {}
_o = 0
for _n, _c in PP_SPEC:
    PP_OFF[_n] = _o
    _o += _c
NPP = _o

RB_SPEC = [("attn_norm", 2048), ("ffn_norm", 2048), ("gdn_norm", 128), ("mlstm_norm", 512),
           ("rwkv_ln_w", 512), ("rwkv_ln_b", 512)]
RB_OFF = {}
_o = 0
for _n, _c in RB_SPEC:
    RB_OFF[_n] = _o
    _o += _c
NRB = _o


def _chunks(v, n):
    return np.ascontiguousarray(np.asarray(v, np.float32).reshape(n, 128).T)


def pack_params(inp, L):
    pp = np.zeros((L, 128, NPP), np.float32)
    rb = np.zeros((L, NRB), np.float32)
    for l in range(L):
        def put(name, arr):
            o = PP_OFF[name]
            pp[l, :, o:o + arr.shape[1]] = arr
        for name, nch in (("gdn_conv", 12), ("mlstm_conv", 8), ("rglru_conv", 4)):
            cw = np.asarray(inp[name][l], np.float32)
            a = cw.reshape(4, nch, 128).transpose(2, 1, 0).reshape(128, nch * 4)
            put(name, a)
        for name in ("rglru_conv_b", "rglru_b_a", "rglru_b_x", "rglru_lambda", "rwkv_w0", "rwkv_a0",
                     "rwkv_k_k", "rwkv_k_a"):
            put(name, _chunks(inp[name][l], 4))
        put("rwkv_r_k", _chunks(np.asarray(inp["rwkv_r_k"][l]).reshape(-1), 4))
        put("rwkv_mu", _chunks(inp["rwkv_mu"][l], 14))
        for name in ("gdn_a_log", "gdn_dt_bias", "mlstm_b_i", "mlstm_b_f"):
            col = np.zeros((128, 1), np.float32)
            col[0:4, 0] = np.asarray(inp[name][l], np.float32)
            put(name, col)
        for name, n in RB_SPEC:
            rb[l, RB_OFF[name]:RB_OFF[name] + n] = np.asarray(inp[name][l], np.float32).reshape(-1)
    return pp, rb


C_IDENT = 0
C_MSU = 128
C_MIU = 256
C_MSL = 384
C_SEL = 512
C_CMASK = 1024
C_ONES = 1536
C_SEL2 = 1664
NCONST = 1666


def make_consts():
    c = np.zeros((128, NCONST), np.float32)
    s = np.arange(128)
    c[:, C_IDENT:C_IDENT + 128] = np.eye(128)
    c[:, C_MSU:C_MSU + 128] = (s[:, None] < s[None, :])
    c[:, C_MIU:C_MIU + 128] = (s[:, None] <= s[None, :])
    c[:, C_MSL:C_MSL + 128] = (s[None, :] < s[:, None])
    for h in range(4):
        c[h, C_SEL + h * 128:C_SEL + (h + 1) * 128] = 1.0
    c[:, C_CMASK:C_CMASK + 512] = 1.0
    c[:, C_CMASK:C_CMASK + 512:128] = 0.0
    c[:, C_ONES:C_ONES + 128] = 1.0
    c[0:64, C_SEL2] = 1.0
    c[64:128, C_SEL2 + 1] = 1.0
    return c


class Buf:
    __slots__ = ("name", "w", "r")

    def __init__(self, name):
        self.name = name
        self.w = None
        self.r = {}


class Tile:
    def __init__(self, h, buf, bufs=None):
        self.h = h
        self.buf = buf
        self.bufs = bufs


class Prog:
    ENG = ("pe", "dve", "act", "pool", "sp")
    NDMA = 6
    SAME_ENGINE_SYNC = True

    def __init__(self, nc, stack):
        self.nc = nc
        self.stack = stack
        self.q = {e: [] for e in self.ENG}
        self.sem = {}
        self.cnt = {}
        self.waited = {e: {} for e in self.ENG}
        for e in ("pe", "dve", "act", "pool"):
            self.sem[e] = stack.enter_context(nc.semaphore("s_" + e))
            self.cnt[e] = 0
        self.dma_rr = {}
        for e in ("sp", "pool", "act"):
            self.dma_rr[e] = 0
            for j in range(self.NDMA):
                k = (e, j)
                self.sem[k] = stack.enter_context(nc.semaphore("d_%s%d" % (e, j)))
                self.cnt[k] = 0
        self.n_inst = 0
        self.n_wait = 0

    def sb(self, name, shape, dtype, nbufs=0):
        h = self.stack.enter_context(self.nc.sbuf_tensor("sb_" + name, list(shape), dtype))
        bufs = [Buf("%s#%d" % (name, i)) for i in range(nbufs)] if nbufs else None
        return Tile(h, Buf(name), bufs)

    def ps(self, name, shape, dtype=F32):
        h = self.stack.enter_context(self.nc.psum_tensor("pm_" + name, list(shape), dtype))
        return Tile(h, Buf(name))

    def _need(self, e, tok):
        if tok is None:
            return
        k, v = tok
        if k == e:
            if e == "pe" or not self.SAME_ENGINE_SYNC:
                return
        if self.waited[e].get(k, 0) >= v:
            return
        self.waited[e][k] = v
        sem = self.sem[k]
        self.q[e].append(lambda eng, sem=sem, v=v: eng.wait_ge(sem, v))
        self.n_wait += 1

    def _deps(self, e, reads, writes):
        for b in reads:
            self._need(e, b.w)
        for b in writes:
            self._need(e, b.w)
            for k, v in b.r.items():
                self._need(e, (k, v))

    def _mark(self, tok, reads, writes):
        k, v = tok
        for b in reads:
            if b.r.get(k, 0) < v:
                b.r[k] = v
        for b in writes:
            b.w = tok
            b.r = {}

    @staticmethod
    def _bufs(xs):
        out = []
        for x in xs:
            if isinstance(x, Tile):
                out.append(x.buf)
            elif isinstance(x, Buf):
                out.append(x)
            elif x is None:
                pass
            else:
                out.extend(Prog._bufs(x))
        return out

    def op(self, e, fns, reads=(), writes=()):
        if e == "pool":
            e = "dve"
        reads = self._bufs(reads)
        writes = self._bufs(writes)
        if not isinstance(fns, (list, tuple)):
            fns = [fns]
        self._deps(e, reads, writes)
        sem = self.sem[e]
        n = len(fns)
        for i, fn in enumerate(fns):
            if i == n - 1:
                self.q[e].append(lambda eng, fn=fn, sem=sem: fn(eng).then_inc(sem, 1))
            else:
                self.q[e].append(fn)
        self.n_inst += n
        self.cnt[e] += 1
        self._mark((e, self.cnt[e]), reads, writes)

    def dma(self, e, out, in_, reads=(), writes=()):
        reads = self._bufs(reads)
        writes = self._bufs(writes)
        j = self.dma_rr[e] % self.NDMA
        self.dma_rr[e] += 1
        k = (e, j)
        if self.cnt[k] > 0:
            self._need(e, (k, self.cnt[k]))
        self._deps(e, reads, writes)
        sem = self.sem[k]
        self.q[e].append(lambda eng, out=out, in_=in_, sem=sem: eng.dma_start(out=out, in_=in_).then_inc(sem, 16))
        self.n_inst += 1
        self.cnt[k] += 16
        self._mark((k, self.cnt[k]), reads, writes)

    def finish(self):
        for k, v in self.cnt.items():
            if isinstance(k, tuple) and v > 0:
                self._need("sp", (k, v))
        for e in ("pe", "dve", "act", "pool"):
            if self.cnt[e] > 0:
                self._need("sp", (e, self.cnt[e]))

    def emit(self):
        nc = self.nc
        q = self.q
        with nc.Block() as block:
            @block.tensor
            def _(eng):
                for fn in q["pe"]:
                    fn(eng)

            @block.vector
            def _(eng):
                for fn in q["dve"]:
                    fn(eng)

            @block.scalar
            def _(eng):
                for fn in q["act"]:
                    fn(eng)

            @block.gpsimd
            def _(eng):
                for fn in q["pool"]:
                    fn(eng)

            @block.sync
            def _(eng):
                for fn in q["sp"]:
                    fn(eng)


class Pool:
    def __init__(self, P, n):
        self.t = P.sb("arena", [128, n, 512], F32, nbufs=n)
        self.free = list(range(n))
        self.n = n

    def get(self):
        assert self.free, "arena exhausted"
        return self.free.pop(0)

    def get_block(self, n):
        for i in range(self.n - n + 1):
            if all((i + j) in self.free for j in range(n)):
                for j in range(n):
                    self.free.remove(i + j)
                return i
        raise AssertionError("no contiguous arena block")

    def blk_f32(self, i, n):
        return self.t.h[:, i:i + n, :].rearrange("p n c -> p (n c)")

    def put(self, *idx):
        for i in idx:
            assert i not in self.free
            self.free.append(i)

    def f32(self, i):
        return self.t.h[:, i, :]

    def bf(self, i):
        return self.t.h[:, i, :].bitcast(BF16)

    def buf(self, i):
        return self.t.bufs[i]


GELU_C = 1.5957691216057308


class Kern:
    def __init__(self, cfg):
        self.cfg = cfg
        self.stack = ExitStack()
        nc = bass.Bass("TRN2", target_bir_lowering=False)
        self.nc = nc
        L, T = cfg.depth, cfg.T
        dt = nc.dram_tensor
        self.x_d = dt("x", [T, D], F32, kind="ExternalInput").ap()
        self.out_d = dt("out", [T, D], F32, kind="ExternalOutput").ap()
        self.w_in_d = dt("w_in", [L, D, IN_COLS], F32, kind="ExternalInput").ap()
        self.w_out_d = dt("w_out", [L, D, D], F32, kind="ExternalInput").ap()
        self.wg_d = dt("ffn_w_gate", [L, D, FFN], F32, kind="ExternalInput").ap()
        self.wu_d = dt("ffn_w_up", [L, D, FFN], F32, kind="ExternalInput").ap()
        self.wd_d = dt("ffn_w_down", [L, FFN, D], F32, kind="ExternalInput").ap()
        self.pp_d = dt("pp", [L, 128, NPP], F32, kind="ExternalInput").ap()
        self.rb_d = dt("rb", [L, NRB], F32, kind="ExternalInput").ap()
        self.fin_d = dt("final_norm", [1, D], F32, kind="ExternalInput").ap()
        self.const_d = dt("consts", [128, NCONST], F32, kind="ExternalInput").ap()
        self.rg_wa_d = dt("rglru_w_a", [L, 4, 128, 128], F32, kind="ExternalInput").ap()
        self.rg_wx_d = dt("rglru_w_x", [L, 4, 128, 128], F32, kind="ExternalInput").ap()
        self.rw_w2_d = dt("rwkv_w2", [L, 64, 512], F32, kind="ExternalInput").ap()
        self.rw_a2_d = dt("rwkv_a2", [L, 64, 512], F32, kind="ExternalInput").ap()
        self.rw_g2_d = dt("rwkv_g2", [L, 128, 512], F32, kind="ExternalInput").ap()

    def build(self):
        with self.stack:
            self._build()
        return self.nc

    def act(self, out, in_, func, reads, writes, **kw):
        self.P.op("act", lambda e: e.activation(out=out, in_=in_, func=func, **kw), reads, writes)

    def tt(self, eng, out, in0, in1, op, reads, writes):
        self.P.op(eng, lambda e: e.tensor_tensor(out=out, in0=in0, in1=in1, op=op), reads, writes)

    def ts(self, eng, out, in0, s1, s2, op0, op1, reads, writes, **kw):
        if s2 is None:
            self.P.op(eng, lambda e: e.tensor_scalar(out=out, in0=in0, scalar1=s1, scalar2=None, op0=op0, **kw),
                      reads, writes)
        else:
            self.P.op(eng, lambda e: e.tensor_scalar(out=out, in0=in0, scalar1=s1, scalar2=s2, op0=op0, op1=op1, **kw),
                      reads, writes)

    def stt(self, out, in0, scalar, in1, op0, op1, reads, writes):
        self.P.op("dve", lambda e: e.scalar_tensor_tensor(out=out, in0=in0, scalar=scalar, in1=in1, op0=op0, op1=op1),
                  reads, writes)

    def cp(self, eng, out, in_, reads, writes):
        if eng == "act":
            self.P.op("act", lambda e: e.copy(out=out, in_=in_), reads, writes)
        else:
            self.P.op(eng, lambda e: e.tensor_copy(out=out, in_=in_), reads, writes)

    @staticmethod
    def trn(out, in_, identity):
        return lambda e: e.transpose(out=out, in_=in_, identity=identity)

    @staticmethod
    def mm(out, lhsT, rhs, start, stop):
        return lambda e: e.matmul(out=out, lhsT=lhsT, rhs=rhs, start=start, stop=stop)

    def _build(self):
        cfg = self.cfg
        nc = self.nc
        P = Prog(nc, self.stack)
        self.P = P
        NB, TT, L = cfg.NB, cfg.TT, cfg.depth
        self.x = [P.sb("x%d" % b, [128, D], F32) for b in range(NB)]
        self.nT = P.sb("nT", [128, NK, TT], BF16, nbufs=NB)
        self.mixT = P.sb("mixT", [128, NK, TT], BF16, nbufs=NK)
        self.NSLOT = 3
        self.wslot = [P.sb("wslot%d" % i, [128, NK, 520], BF16) for i in range(self.NSLOT)]
        self.wrr = 0
        self.xs = [P.sb("xs%d" % i, [128, D], BF16) for i in range(1)]
        self.cbuf = [P.sb("cbuf%d" % i, [128, 515], F32) for i in range(2)]
        self.cb_rr = 0
        self.pp = [P.sb("pp%d" % l, [128, NPP], F32) for l in range(L)]
        self.consts = P.sb("consts", [128, NCONST], F32)
        self.ident_b = P.sb("ident_b", [128, 128], BF16)
        self.small = P.sb("small", [128, 64], F32, nbufs=64)
        self.small_rr = 0
        self.psum = [P.ps("ps%d" % i, [128, 512], F32) for i in range(8)]
        self.ps_rr = 0
        self.pool = Pool(P, 23)
        self.small8 = P.sb("small8", [128, 64], F32, nbufs=2)
        self.small4 = P.sb("small4", [128, 64], F32, nbufs=16)

        P.dma("sp", self.consts.h[:], self.const_d[:, :], writes=[self.consts])
        for l in range(L):
            P.dma("sp", self.pp[l].h[:], self.pp_d[l], writes=[self.pp[l]])
        self.cp("dve", self.ident_b.h[:], self.consts.h[:, 0:128], [self.consts], [self.ident_b])
        self.setup_mixers()

        for t in range(cfg.NT):
            self.load_x(t)
            for l in range(L):
                self.layer(l, t)
            self.final_norm_store(t)
        P.finish()
        self.sbuf_left = nc.sbuf_bytes_remaining
        P.emit()

    def next_ps(self):
        i = self.ps_rr % 8
        self.ps_rr += 1
        return self.psum[i]

    def scal(self):
        i = self.small_rr % 64
        self.small_rr += 1
        return self.small.h[:, i:i + 1], self.small.bufs[i]

    def ppc(self, l, name, j=0):
        o = PP_OFF[name] + j
        return self.pp[l].h[:, o:o + 1]

    def load_w(self, src_ap, nk=NK, ncols=512):
        s = self.wslot[self.wrr % self.NSLOT]
        self.wrr += 1
        self.P.dma("pool", s.h[:, 0:nk, 0:ncols], src_ap.rearrange("(k p) c -> p k c", p=128), writes=[s])
        return s

    def load_x(self, t):
        P, cfg = self.P, self.cfg
        for b in range(cfg.NB):
            r0 = t * cfg.TT + b * 128
            P.dma("sp", self.x[b].h[:], self.x_d[r0:r0 + 128, :], writes=[self.x[b]])

    def load_rowbcast(self, dst, dcol0, src_row_ap):
        n = src_row_ap.shape[-1]
        self.P.dma("sp", dst.h[:, dcol0:dcol0 + n], src_row_ap.partition_broadcast(128), writes=[dst])

    def wrow_get(self, src_row_ap):
        i = self.pool.get_block(4)
        ap = self.pool.blk_f32(i, 4)
        bufs = [self.pool.buf(i + j) for j in range(4)]
        self.P.dma("sp", ap, src_row_ap.partition_broadcast(128), writes=bufs)
        return ap, bufs, i

    def wrow_put(self, i):
        self.pool.put(i, i + 1, i + 2, i + 3)

    def row_rstd(self, xb):
        xs = self.xs[0]
        ss, ssb = self.scal()
        rs, rsb = self.scal()
        self.act(xs.h[:], xb.h[:], AF.Square, [xb], [xs, ssb], accum_out=ss)
        self.act(rs, ss, AF.Sqrt, [ssb], [rsb], bias=NORM_EPS, scale=1.0 / D)
        self.P.op("dve", lambda e: e.reciprocal(out=rs, in_=rs), [rsb], [rsb])
        return rs, rsb, xs

    def rmsnorm_T(self, src_row_ap):
        P, cfg = self.P, self.cfg
        wr, wrb, wi = self.wrow_get(src_row_ap)
        for b in range(cfg.NB):
            xb = self.x[b]
            self.cb_rr += 1
            rs, rsb, xs = self.row_rstd(xb)
            self.stt(xs.h[:], xb.h[:], rs, wr, ALU.mult, ALU.mult, [xb, rsb, wrb], [xs])
            for g in range(4):
                ps = self.next_ps()
                psb = ps.h[:].bitcast(BF16)
                fns = []
                for j in range(4):
                    k = g * 4 + j
                    fns.append(lambda e, psb=psb, j=j, k=k, xs=xs: e.transpose(
                        out=psb[:, j * 128:(j + 1) * 128], in_=xs.h[:, k * 128:(k + 1) * 128],
                        identity=self.ident_b.h[:]))
                P.op("pe", fns, [xs, self.ident_b], [ps])
                dst = self.nT.h[:, g * 4:(g + 1) * 4, b * 128:(b + 1) * 128]
                srcv = psb[:, 0:512].rearrange("p (j c) -> p j c", j=4)
                self.cp("act" if g % 2 else "dve", dst, srcv, [ps], [self.nT.bufs[b]])
        self.wrow_put(wi)

    def inproj_fm(self, ws, wc, m):
        ps = self.next_ps()
        TT = self.cfg.TT
        fns = [self.mm(ps.h[0:m, 0:TT], ws.h[:, k, wc:wc + m], self.nT.h[:, k, :], k == 0, k == NK - 1)
               for k in range(NK)]
        self.P.op("pe", fns, [ws, self.nT.bufs], [ps])
        return ps

    def inproj_tm(self, ws, wc, n, b):
        ps = self.next_ps()
        fns = [self.mm(ps.h[:, 0:n], self.nT.h[:, k, b * 128:(b + 1) * 128], ws.h[:, k, wc:wc + n], k == 0, k == NK - 1)
               for k in range(NK)]
        self.P.op("pe", fns, [ws, self.nT.bufs[b]], [ps])
        return ps

    def conv4(self, l, ps, m, cname, cidx, halo, halo_buf, hcol, bias=None):
        TT = self.cfg.TT
        pool = self.pool
        cb = self.cbuf[self.cb_rr % 2]
        self.cb_rr += 1
        self.cp("act", cb.h[0:m, 3:3 + TT], ps.h[0:m, 0:TT], [ps], [cb])
        self.cp("dve", cb.h[0:m, 0:3], halo.h[0:m, hcol:hcol + 3], [halo_buf], [cb])
        o = pool.get()
        acc = pool.f32(o)[0:m, 0:TT]
        w = lambda j: self.ppc(l, cname, cidx * 4 + j)[0:m]
        if bias is None:
            self.ts("dve", acc, cb.h[0:m, 3:3 + TT], w(3), None, ALU.mult, None, [cb, self.pp[l]], [pool.buf(o)])
        else:
            self.ts("dve", acc, cb.h[0:m, 3:3 + TT], w(3), bias, ALU.mult, ALU.add, [cb, self.pp[l]], [pool.buf(o)])
        for j in (2, 1, 0):
            self.stt(acc, cb.h[0:m, j:j + TT], w(j), acc, ALU.mult, ALU.add, [cb, self.pp[l], pool.buf(o)], [pool.buf(o)])
        self.cp("dve", halo.h[0:m, hcol:hcol + 3], cb.h[0:m, TT:TT + 3], [cb], [halo_buf])
        return o

    def layer(self, l, t):
        P, cfg, pool = self.P, self.cfg, self.pool
        NB, TT = cfg.NB, cfg.TT
        self.rmsnorm_T(self.rb_d[l:l + 1, RB_OFF["attn_norm"]:RB_OFF["attn_norm"] + D])
        for name in ("gdn", "mlstm", "rglru", "rwkv"):
            if name in cfg.mixers:
                getattr(self, name)(l, t)
            else:
                base = {"gdn": 0, "mlstm": 4, "rglru": 8, "rwkv": 12}[name]
                if t == 0 and l == 0:
                    for c in range(4):
                        P.op("pool", lambda e, c=c, base=base: e.memset(self.mixT.h[:, base + c, :], 0.0),
                             [], [self.mixT.bufs[base + c]])
        for dg in range(4):
            ws = self.load_w(self.w_out_d[l, :, dg * 512:(dg + 1) * 512])
            for b in range(NB):
                ps = self.next_ps()
                fns = [self.mm(ps.h[:, :], self.mixT.h[:, k, b * 128:(b + 1) * 128], ws.h[:, k, 0:512], k == 0, k == NK - 1)
                       for k in range(NK)]
                P.op("pe", fns, [ws, self.mixT.bufs], [ps])
                xv = self.x[b].h[:, dg * 512:(dg + 1) * 512]
                self.tt("dve", xv, xv, ps.h[:, :], ALU.add, [ps, self.x[b]], [self.x[b]])
        self.rmsnorm_T(self.rb_d[l:l + 1, RB_OFF["ffn_norm"]:RB_OFF["ffn_norm"] + D])
        hs = [pool.get() for _ in range(NF // 2)]

        def hT(c):
            return pool.bf(hs[c // 2])[:, (c % 2) * 512:(c % 2) * 512 + TT], pool.buf(hs[c // 2])
        for fg in range(FFN // 512):
            wg = self.load_w(self.wg_d[l, :, fg * 512:(fg + 1) * 512])
            wu = self.load_w(self.wu_d[l, :, fg * 512:(fg + 1) * 512])
            for j in range(4):
                c = fg * 4 + j
                pg = self.inproj_fm(wg, j * 128, 128)
                pu = self.inproj_fm(wu, j * 128, 128)
                sg = pool.get()
                self.act(pool.f32(sg)[:, 0:TT], pg.h[:, 0:TT], AF.Silu, [pg], [pool.buf(sg)])
                h_ap, h_buf = hT(c)
                self.tt("dve", h_ap, pool.f32(sg)[:, 0:TT], pu.h[:, 0:TT], ALU.mult, [pu, pool.buf(sg)], [h_buf])
                pool.put(sg)
        fsl = [(0, 16), (16, 16), (32, 12)]
        for dg in range(4):
            pss = [self.next_ps() for _ in range(NB)]
            for si, (f0, nf) in enumerate(fsl):
                ws = self.load_w(self.wd_d[l, f0 * 128:(f0 + nf) * 128, dg * 512:(dg + 1) * 512], nk=nf)
                for b in range(NB):
                    fns = []
                    rd = [ws]
                    for kk in range(nf):
                        h_ap, h_buf = hT(f0 + kk)
                        rd.append(h_buf)
                        fns.append(self.mm(pss[b].h[:, :], h_ap[:, b * 128:(b + 1) * 128], ws.h[:, kk, 0:512],
                                           si == 0 and kk == 0, si == 2 and kk == nf - 1))
                    P.op("pe", fns, rd, [pss[b]])
            for b in range(NB):
                xv = self.x[b].h[:, dg * 512:(dg + 1) * 512]
                self.tt("dve", xv, xv, pss[b].h[:, :], ALU.add, [pss[b], self.x[b]], [self.x[b]])
        pool.put(*hs)

    def final_norm_store(self, t):
        P, cfg = self.P, self.cfg
        wr, wrb, wi = self.wrow_get(self.fin_d[0:1, :])
        for b in range(cfg.NB):
            xb = self.x[b]
            self.cb_rr += 1
            rs, rsb, xs = self.row_rstd(xb)
            self.stt(xb.h[:], xb.h[:], rs, wr, ALU.mult, ALU.mult, [xb, rsb, wrb], [xb])
            r0 = t * cfg.TT + b * 128
            P.dma("sp", self.out_d[r0:r0 + 128, :], xb.h[:], reads=[xb])
        self.wrow_put(wi)

    def setup_mixers(self):
        P, cfg = self.P, self.cfg
        L = cfg.depth
        self.tok = P.sb("tok", [128, 64], F32)
        self.rown = P.sb("rown", [128, 512], F32)
        for name in ("gdn", "mlstm", "rwkv"):
            if name in cfg.mixers:
                globals()["setup_" + name](self)
        self.halo = [P.sb("halo%d" % l, [128, 72], F32, nbufs=24) for l in range(L)]
        self.rg_h = [P.sb("rg_h%d" % l, [128, 4], F32, nbufs=4) for l in range(L)]
        self.rg_w = [P.sb("rg_w%d" % l, [128, 8, 128], BF16) for l in range(L)]
        self.rg_cp = [P.sb("rg_cp%d" % l, [128, 8], F32) for l in range(L)]
        for l in range(L):
            P.op("pool", lambda e, l=l: e.memset(self.halo[l].h[:], 0.0), [], self.halo[l].bufs)
            P.op("pool", lambda e, l=l: e.memset(self.rg_h[l].h[:], 0.0), [], self.rg_h[l].bufs)
            P.dma("pool", self.rg_w[l].h[:, 0:4, :], self.rg_wa_d[l].rearrange("n i j -> i n j"), writes=[self.rg_w[l]])
            P.dma("pool", self.rg_w[l].h[:, 4:8, :], self.rg_wx_d[l].rearrange("n i j -> i n j"), writes=[self.rg_w[l]])
            lam = self.pp[l].h[:, PP_OFF["rglru_lambda"]:PP_OFF["rglru_lambda"] + 4]
            cpt = self.rg_cp[l]
            self.act(cpt.h[:, 0:4], lam, AF.Exp, [self.pp[l]], [cpt], scale=-1.0)
            self.act(cpt.h[:, 0:4], cpt.h[:, 0:4], AF.Ln, [cpt], [cpt], bias=1.0)
            self.ts("dve", cpt.h[:, 4:8], cpt.h[:, 0:4], -2.0 * RGLRU_C, None, ALU.mult, None, [cpt], [cpt])
            self.ts("dve", cpt.h[:, 0:4], cpt.h[:, 0:4], -RGLRU_C, None, ALU.mult, None, [cpt], [cpt])

    def rglru(self, l, t):
        P, cfg, pool = self.P, self.cfg, self.pool
        TT = cfg.TT
        wx = self.load_w(self.w_in_d[l, :, RG0:RG0 + 512])
        wgt = self.load_w(self.w_in_d[l, :, RG0 + 512:RG0 + 1024])
        pl = self.pp[l]
        for c in range(4):
            ps = self.inproj_fm(wx, c * 128, 128)
            hb = 20 + c
            xo = self.conv4(l, ps, 128, "rglru_conv", c, self.halo[l], self.halo[l].bufs[hb], hb * 3,
                            bias=self.ppc(l, "rglru_conv_b", c))
            xf = pool.f32(xo)[:, 0:TT]
            xbf = pool.get()
            xb16 = pool.bf(xbf)[:, 0:TT]
            self.cp("act", xb16, xf, [pool.buf(xo)], [pool.buf(xbf)])
            pr = self.next_ps()
            P.op("pe", [self.mm(pr.h[:, 0:TT], self.rg_w[l].h[:, c, :], xb16, True, True)], [self.rg_w[l], pool.buf(xbf)], [pr])
            pi = self.next_ps()
            P.op("pe", [self.mm(pi.h[:, 0:TT], self.rg_w[l].h[:, 4 + c, :], xb16, True, True)], [self.rg_w[l], pool.buf(xbf)], [pi])
            r_ = pool.get()
            i_ = pool.get()
            rf, if_ = pool.f32(r_)[:, 0:TT], pool.f32(i_)[:, 0:TT]
            self.act(rf, pr.h[:, 0:TT], AF.Sigmoid, [pr, pl], [pool.buf(r_)], bias=self.ppc(l, "rglru_b_a", c))
            self.act(if_, pi.h[:, 0:TT], AF.Sigmoid, [pi, pl], [pool.buf(i_)], bias=self.ppc(l, "rglru_b_x", c))
            a_ = xbf
            af = pool.f32(a_)[:, 0:TT]
            self.act(af, rf, AF.Exp, [pool.buf(r_), self.rg_cp[l], pool.buf(a_)], [pool.buf(a_)], scale=self.rg_cp[l].h[:, c:c + 1])
            self.act(rf, rf, AF.Exp, [pool.buf(r_), self.rg_cp[l]], [pool.buf(r_)], scale=self.rg_cp[l].h[:, 4 + c:5 + c])
            self.ts("dve", rf, rf, -1.0, 1.0, ALU.mult, ALU.add, [pool.buf(r_)], [pool.buf(r_)])
            self.act(rf, rf, AF.Sqrt, [pool.buf(r_)], [pool.buf(r_)])
            self.tt("dve", if_, if_, xf, ALU.mult, [pool.buf(i_), pool.buf(xo)], [pool.buf(i_)])
            self.tt("dve", if_, if_, rf, ALU.mult, [pool.buf(i_), pool.buf(r_)], [pool.buf(i_)])
            hst = self.rg_h[l].h[:, c:c + 1]
            hstb = self.rg_h[l].bufs[c]
            P.op("dve", lambda e, xf=xf, af=af, if_=if_, hst=hst: e.tensor_tensor_scan(
                out=xf, data0=af, data1=if_, initial=hst, op0=ALU.mult, op1=ALU.add),
                [pool.buf(a_), pool.buf(i_), hstb], [pool.buf(xo)])
            self.cp("dve", hst, xf[:, TT - 1:TT], [pool.buf(xo)], [hstb])
            pg = self.inproj_fm(wgt, c * 128, 128)
            g = pg.h[:, 0:TT]
            self.act(rf, g, AF.Square, [pg], [pool.buf(r_)])
            self.ts("dve", rf, rf, 0.044715, 1.0, ALU.mult, ALU.add, [pool.buf(r_)], [pool.buf(r_)])
            self.tt("dve", rf, rf, g, ALU.mult, [pool.buf(r_), pg], [pool.buf(r_)])
            self.act(rf, rf, AF.Sigmoid, [pool.buf(r_)], [pool.buf(r_)], scale=GELU_C)
            self.tt("dve", rf, rf, g, ALU.mult, [pool.buf(r_), pg], [pool.buf(r_)])
            self.tt("dve", self.mixT.h[:, 8 + c, :], rf, xf, ALU.mult, [pool.buf(r_), pool.buf(xo)], [self.mixT.bufs[8 + c]])
            pool.put(xo, a_, r_, i_)

    def gdn(self, l, t):
        raise NotImplementedError

    def rwkv(self, l, t):
        raise NotImplementedError


_W_NAMES = ("w_in", "w_out", "ffn_w_gate", "ffn_w_up", "ffn_w_down", "rglru_w_a", "rglru_w_x",
            "rwkv_w2", "rwkv_a2", "rwkv_g2")


def make_in_maps(inputs, cfg, n_cores):
    L = cfg.depth
    pp, rb = pack_params(inputs, L)
    consts = make_consts()
    shared = {k: np.ascontiguousarray(np.asarray(inputs[k], np.float32)) for k in _W_NAMES}
    shared["pp"] = pp
    shared["rb"] = rb
    shared["final_norm"] = np.ascontiguousarray(np.asarray(inputs["final_norm"], np.float32).reshape(1, D))
    shared["consts"] = consts
    x = np.asarray(inputs["x"], np.float32)
    B = x.shape[0]
    maps = []
    for c in range(n_cores):
        m = dict(shared)
        m["x"] = np.ascontiguousarray(x[c % B])
        maps.append(m)
    return maps


def kernel(**inputs):
    x = np.asarray(inputs["x"])
    B, T, _ = x.shape
    L = np.asarray(inputs["w_in"]).shape[0]
    cfg = Cfg(T=T, depth=L)
    nc = Kern(cfg).build()
    n_cores = B
    in_maps = make_in_maps(inputs, cfg, n_cores)
    res = run_bass_kernel_spmd(nc, in_maps, core_ids=list(range(n_cores)))
    out = np.stack([np.asarray(res.results[c]["out"], np.float32) for c in range(B)], axis=0)
    return out

def _cs(ap, j, n=128):
    return ap[:, j * n:(j + 1) * n]


def setup_mlstm(self):
    P, cfg = self.P, self.cfg
    L = cfg.depth
    self.ml_C = [P.sb("ml_C%d" % l, [128, 4, 129], F32, nbufs=4) for l in range(L)]
    self.ml_Cb = [P.sb("ml_Cb%d" % l, [128, 4, 130], BF16, nbufs=4) for l in range(L)]
    self.ml_sc = [P.sb("ml_sc%d" % l, [128, 2], F32) for l in range(L)]
    self.vp = [P.sb("vp%d" % c, [128, 4, 130], BF16) for c in range(4)]
    for c in range(4):
        P.op("pool", lambda e, c=c: e.memset(self.vp[c].h[:], 1.0), [], [self.vp[c]])
    for l in range(L):
        P.op("pool", lambda e, l=l: e.memset(self.ml_C[l].h[:], 0.0), [], self.ml_C[l].bufs)
        P.op("pool", lambda e, l=l: e.memset(self.ml_Cb[l].h[:], 0.0), [], self.ml_Cb[l].bufs)
        self.ts("dve", self.ml_sc[l].h[:, 0:1], self.ppc(l, "mlstm_b_f"), -1.0, None, ALU.mult, None,
                [self.pp[l]], [self.ml_sc[l]])


def gate_rows_to_bcast(self, rows_ap, rows_buf, h):
    ps = self.next_ps()
    TT = self.cfg.TT
    sel = self.consts.h[0:4, C_SEL + h * 128:C_SEL + (h + 1) * 128]
    self.P.op("pe", [self.mm(ps.h[:, 0:TT], sel, rows_ap, True, True)], [self.consts, rows_buf], [ps])
    return ps


def rows_to_tok(self, rows_list, col0):
    P = self.P
    NCH = self.cfg.TT // 128
    ps = self.next_ps()
    fns = []
    rd = [self.consts]
    for q, (ap, buf) in enumerate(rows_list):
        rd.append(buf)
        for c in range(NCH):
            o = (q * NCH + c) * 4
            fns.append(self.trn(ps.h[:, o:o + 4], ap[0:4, c * 128:(c + 1) * 128], self.consts.h[0:4, C_IDENT:C_IDENT + 4]))
    P.op("pe", fns, rd, [ps])
    n = len(rows_list) * NCH * 4
    self.cp("dve", self.tok.h[:, col0:col0 + n], ps.h[:, 0:n], [ps], [self.tok])


def mlstm(self, l, t):
    P, cfg, pool = self.P, self.cfg, self.pool
    TT = cfg.TT
    NCH = TT // 128
    pl = self.pp[l]
    cst, cbf = self.ml_C[l], self.ml_Cb[l]
    self.load_rowbcast(self.rown, 0, self.rb_d[l:l + 1, RB_OFF["mlstm_norm"]:RB_OFF["mlstm_norm"] + 512])
    wo = self.load_w(self.w_in_d[l, :, ML0 + 1536:ML0 + 2056], ncols=520)
    ps_i = self.inproj_fm(wo, 512, 4)
    ps_f = self.inproj_fm(wo, 516, 4)
    s_i, s_b, s_c = pool.get(), pool.get(), pool.get()
    r_i, r_b, r_c = pool.f32(s_i)[0:4, 0:TT], pool.f32(s_b)[0:4, 0:TT], pool.f32(s_c)[0:4, 0:TT]
    self.act(r_i, ps_i.h[0:4, 0:TT], AF.Identity, [ps_i, pl], [pool.buf(s_i)], bias=self.ppc(l, "mlstm_b_i")[0:4])
    self.act(r_b, ps_f.h[0:4, 0:TT], AF.Exp, [ps_f, self.ml_sc[l]], [pool.buf(s_b)], bias=self.ml_sc[l].h[0:4, 0:1], scale=-1.0)
    self.act(r_b, r_b, AF.Ln, [pool.buf(s_b)], [pool.buf(s_b)], bias=1.0)
    cm = self.consts.h[0:4, C_CMASK:C_CMASK + TT]
    P.op("dve", lambda e: e.tensor_tensor_scan(out=r_c, data0=cm, data1=r_b, initial=0.0, op0=ALU.mult, op1=ALU.add),
         [self.consts, pool.buf(s_b)], [pool.buf(s_c)])
    self.ts("dve", r_b, r_c, -1.0, None, ALU.mult, None, [pool.buf(s_c)], [pool.buf(s_b)])
    self.tt("dve", r_c, r_i, r_b, ALU.subtract, [pool.buf(s_i), pool.buf(s_b)], [pool.buf(s_c)])
    rows_to_tok(self, [(r_c, pool.buf(s_c))], 0)
    bbc = []
    for h in range(4):
        ps = gate_rows_to_bcast(self, r_b, pool.buf(s_b), h)
        o = pool.get()
        self.cp("act", pool.f32(o)[:, 0:TT], ps.h[:, 0:TT], [ps], [pool.buf(o)])
        bbc.append(o)
    pool.put(s_i, s_b, s_c)
    wv = self.load_w(self.w_in_d[l, :, ML0 + 1024:ML0 + 1536])
    osg = [pool.get() for _ in range(NCH // 2)]

    def osig(c):
        return pool.bf(osg[c // 2])[:, (c % 2) * 512:(c % 2) * 512 + 512], pool.buf(osg[c // 2])
    for c in range(NCH):
        ps_v = self.inproj_tm(wv, 0, 512, c)
        self.cp("act", self.vp[c].h[:, :, 0:128], ps_v.h[:, 0:512].rearrange("p (h e) -> p h e", h=4), [ps_v], [self.vp[c]])
        ps_o = self.inproj_tm(wo, 0, 512, c)
        oa, ob = osig(c)
        self.act(oa, ps_o.h[:, 0:512], AF.Sigmoid, [ps_o], [ob])
    wq = self.load_w(self.w_in_d[l, :, ML0:ML0 + 512])
    wk = self.load_w(self.w_in_d[l, :, ML0 + 512:ML0 + 1024])
    qk = [pool.get() for _ in range(4)]
    qd = [pool.get() for _ in range(2)]
    for h in range(4):
        ps = self.inproj_fm(wq, h * 128, 128)
        o = self.conv4(l, ps, 128, "mlstm_conv", h, self.halo[l], self.halo[l].bufs[12 + h], (12 + h) * 3)
        qf = pool.f32(o)[:, 0:TT]
        self.act(qf, qf, AF.Silu, [pool.buf(o)], [pool.buf(o)])
        self.cp("act", pool.bf(qk[h])[:, 0:TT], qf, [pool.buf(o)], [pool.buf(qk[h])])
        e_ = pool.get()
        self.act(pool.f32(e_)[:, 0:TT], pool.f32(bbc[h])[:, 0:TT], AF.Exp, [pool.buf(bbc[h])], [pool.buf(e_)])
        self.tt("dve", pool.bf(qd[h // 2])[:, (h % 2) * 512:(h % 2) * 512 + TT], qf, pool.f32(e_)[:, 0:TT], ALU.mult,
                [pool.buf(o), pool.buf(e_)], [pool.buf(qd[h // 2])])
        pool.put(o, e_)
        ps = self.inproj_fm(wk, h * 128, 128)
        o = self.conv4(l, ps, 128, "mlstm_conv", 4 + h, self.halo[l], self.halo[l].bufs[16 + h], (16 + h) * 3)
        kf = pool.f32(o)[:, 0:TT]
        self.act(kf, kf, AF.Silu, [pool.buf(o)], [pool.buf(o)])
        self.ts("dve", pool.bf(qk[h])[:, 512:512 + TT], kf, 128.0 ** -0.5, None, ALU.mult, None, [pool.buf(o)], [pool.buf(qk[h])])
        pool.put(o)
    for c in range(NCH):
        cs = slice(c * 128, (c + 1) * 128)
        st_ps = self.next_ps()
        fns = [self.mm(st_ps.h[:, h * 128:(h + 1) * 128], pool.bf(qk[h])[:, 512 + c * 128:512 + (c + 1) * 128],
                       pool.bf(qk[h])[:, cs], True, True) for h in range(4)]
        P.op("pe", fns, [pool.buf(qk[h]) for h in range(4)], [st_ps])
        dt_ = pool.get()
        dtf = pool.f32(dt_)
        for h in range(4):
            self.act(dtf[:, h * 128:(h + 1) * 128], pool.f32(bbc[h])[:, cs], AF.Exp, [pool.buf(bbc[h]), self.tok],
                     [pool.buf(dt_)], bias=self.tok.h[:, c * 4 + h:c * 4 + h + 1])
        for h in range(4):
            self.tt("dve", dtf[:, h * 128:(h + 1) * 128], dtf[:, h * 128:(h + 1) * 128],
                    self.consts.h[:, C_MIU:C_MIU + 128], ALU.mult, [pool.buf(dt_), self.consts], [pool.buf(dt_)])
        st_ = pool.get()
        stb = pool.bf(st_)[:, 0:512]
        self.tt("dve", stb, dtf, st_ps.h[:, 0:512], ALU.mult, [pool.buf(dt_), st_ps], [pool.buf(st_)])
        kw, kwb = self.scal4()
        for h in range(4):
            self.act(kw[:, h:h + 1], self.tok.h[:, c * 4 + h:c * 4 + h + 1], AF.Exp, [self.tok, pool.buf(bbc[h])], [kwb],
                     bias=pool.f32(bbc[h])[:, c * 128 + 127:c * 128 + 128])
        eg, egb = self.scal4()
        for h in range(4):
            self.act(eg[:, h:h + 1], pool.f32(bbc[h])[:, c * 128 + 127:c * 128 + 128], AF.Exp, [pool.buf(bbc[h])], [egb])
        kt_ps = self.next_ps()
        ktb = kt_ps.h[:].bitcast(BF16)
        fns = [self.trn(ktb[:, h * 128:(h + 1) * 128], pool.bf(qk[h])[:, 512 + c * 128:512 + (c + 1) * 128], self.ident_b.h[:])
               for h in range(4)]
        P.op("pe", fns, [pool.buf(qk[h]) for h in range(4)] + [self.ident_b], [kt_ps])
        kw_ = pool.get()
        kwt = pool.bf(kw_)[:, 0:512]
        for h in range(4):
            self.ts("dve", kwt[:, h * 128:(h + 1) * 128], ktb[:, h * 128:(h + 1) * 128], kw[:, h:h + 1], None, ALU.mult, None,
                    [kt_ps, kwb], [pool.buf(kw_)])
        y_ = pool.get()
        yb = pool.bf(y_)[:, 0:512]
        oa, ob = osig(c)
        for h in range(4):
            pn = self.next_ps()
            P.op("pe", [self.mm(pn.h[:, 0:129], pool.bf(qd[h // 2])[:, (h % 2) * 512 + c * 128:(h % 2) * 512 + (c + 1) * 128],
                                cbf.h[:, h, 0:129], True, False),
                        self.mm(pn.h[:, 0:129], stb[:, h * 128:(h + 1) * 128], self.vp[c].h[:, h, 0:129], False, True)],
                 [pool.buf(qd[h // 2]), cbf.bufs[h], pool.buf(st_), self.vp[c]], [pn])
            dn, dnb = self.scal()
            self.act(dn, pn.h[:, 128:129], AF.Abs, [pn], [dnb])
            self.ts("dve", dn, dn, 1.0, None, ALU.max, None, [dnb], [dnb])
            P.op("dve", lambda e, dn=dn: e.reciprocal(out=dn, in_=dn), [dnb], [dnb])
            hh_ = pool.get()
            hf = pool.f32(hh_)[:, 0:128]
            self.stt(hf, pn.h[:, 0:128], dn, oa[:, h * 128:(h + 1) * 128], ALU.mult, ALU.mult, [pn, dnb, ob], [pool.buf(hh_)])
            ss, ssb = self.scal()
            self.act(pool.f32(hh_)[:, 128:256], hf, AF.Square, [pool.buf(hh_)], [pool.buf(hh_), ssb], accum_out=ss)
            self.act(ss, ss, AF.Sqrt, [ssb], [ssb], bias=NORM_EPS, scale=1.0 / 128)
            P.op("dve", lambda e, ss=ss: e.reciprocal(out=ss, in_=ss), [ssb], [ssb])
            self.stt(yb[:, h * 128:(h + 1) * 128], hf, ss, self.rown.h[:, h * 128:(h + 1) * 128], ALU.mult, ALU.mult,
                     [pool.buf(hh_), ssb, self.rown], [pool.buf(y_)])
            pool.put(hh_)
            pu = self.next_ps()
            P.op("pe", [self.mm(pu.h[:, 0:129], kwt[:, h * 128:(h + 1) * 128], self.vp[c].h[:, h, 0:129], True, True)],
                 [pool.buf(kw_), self.vp[c]], [pu])
            self.stt(cst.h[:, h, :], cst.h[:, h, :], eg[:, h:h + 1], pu.h[:, 0:129], ALU.mult, ALU.add,
                     [cst.bufs[h], egb, pu], [cst.bufs[h]])
            self.cp("act", cbf.h[:, h, 0:129], cst.h[:, h, :], [cst.bufs[h]], [cbf.bufs[h]])
        yt_ps = self.next_ps()
        ytb = yt_ps.h[:].bitcast(BF16)
        fns = [self.trn(ytb[:, h * 128:(h + 1) * 128], yb[:, h * 128:(h + 1) * 128], self.ident_b.h[:]) for h in range(4)]
        P.op("pe", fns, [pool.buf(y_), self.ident_b], [yt_ps])
        self.cp("act", self.mixT.h[:, 4:8, cs], ytb[:, 0:512].rearrange("p (h c) -> p h c", h=4), [yt_ps],
                [self.mixT.bufs[4 + h] for h in range(4)])
        pool.put(dt_, st_, kw_, y_)
    pool.put(*bbc)
    pool.put(*osg)
    pool.put(*qk)
    pool.put(*qd)


def scal4(self):
    i = self.small_rr % 16
    self.small_rr += 1
    return self.small4.h[:, i * 4:(i + 1) * 4], self.small4.bufs[i]


Kern.mlstm = mlstm
Kern.scal4 = scal4

def setup_gdn(self):
    P, cfg = self.P, self.cfg
    L = cfg.depth
    self.gd_S = [P.sb("gd_S%d" % l, [128, 4, 128], F32, nbufs=4) for l in range(L)]
    self.gd_Sb = [P.sb("gd_Sb%d" % l, [128, 4, 128], BF16, nbufs=4) for l in range(L)]
    self.gd_sc = [P.sb("gd_sc%d" % l, [128, 2], F32) for l in range(L)]
    self.ones_b = P.sb("ones_b", [128, 128], BF16)
    self.cp("dve", self.ones_b.h[:], self.consts.h[:, C_ONES:C_ONES + 128], [self.consts], [self.ones_b])
    for l in range(L):
        P.op("pool", lambda e, l=l: e.memset(self.gd_S[l].h[:], 0.0), [], self.gd_S[l].bufs)
        P.op("pool", lambda e, l=l: e.memset(self.gd_Sb[l].h[:], 0.0), [], self.gd_Sb[l].bufs)
        self.act(self.gd_sc[l].h[:, 0:1], self.ppc(l, "gdn_a_log"), AF.Exp, [self.pp[l]], [self.gd_sc[l]])
        self.ts("dve", self.gd_sc[l].h[:, 0:1], self.gd_sc[l].h[:, 0:1], -1.0, None, ALU.mult, None,
                [self.gd_sc[l]], [self.gd_sc[l]])


def l2norm_fm(self, xo, scale):
    P, pool = self.P, self.pool
    TT = self.cfg.TT
    xf = pool.f32(xo)[:, 0:TT]
    sq = pool.get()
    self.act(pool.bf(sq)[:, 0:TT], xf, AF.Square, [pool.buf(xo)], [pool.buf(sq)])
    ps = self.next_ps()
    P.op("pe", [self.mm(ps.h[:, 0:TT], self.ones_b.h[:], pool.bf(sq)[:, 0:TT], True, True)], [self.ones_b, pool.buf(sq)], [ps])
    rn = pool.f32(sq)[:, 0:TT]
    self.act(rn, ps.h[:, 0:TT], AF.Sqrt, [ps, pool.buf(sq)], [pool.buf(sq)], bias=1e-6)
    P.op("dve", lambda e: e.reciprocal(out=rn, in_=rn), [pool.buf(sq)], [pool.buf(sq)])
    self.stt(xf, xf, float(scale), rn, ALU.mult, ALU.mult, [pool.buf(xo), pool.buf(sq)], [pool.buf(xo)])
    pool.put(sq)


def tri_inverse_T(self, a_, u_, n=4):
    P, pool = self.P, self.pool
    W_ = n * 128
    ident = self.consts.h[:, C_IDENT:C_IDENT + 128]
    r_ = pool.get()
    rf = pool.f32(r_)
    for h in range(n):
        self.tt("dve", rf[:, h * 128:(h + 1) * 128], ident, pool.f32(u_)[:, h * 128:(h + 1) * 128], ALU.subtract,
                [self.consts, pool.buf(u_)], [pool.buf(r_)])
    p_, q_ = u_, a_
    for k in range(1, 7):
        last = k == 6
        q2 = pool.get()
        psq = self.next_ps()
        P.op("pe", [self.mm(psq.h[:, h * 128:(h + 1) * 128], pool.f32(p_)[:, h * 128:(h + 1) * 128],
                            pool.f32(q_)[:, h * 128:(h + 1) * 128], True, True) for h in range(n)],
             [pool.buf(p_), pool.buf(q_)], [psq])
        self.cp("act", pool.f32(q2)[:, 0:W_], psq.h[:, 0:W_], [psq], [pool.buf(q2)])
        if not last:
            p2 = pool.get()
            psp = self.next_ps()
            P.op("pe", [self.mm(psp.h[:, h * 128:(h + 1) * 128], pool.f32(q_)[:, h * 128:(h + 1) * 128],
                                pool.f32(p_)[:, h * 128:(h + 1) * 128], True, True) for h in range(n)],
                 [pool.buf(p_), pool.buf(q_)], [psp])
            self.cp("dve", pool.f32(p2)[:, 0:W_], psp.h[:, 0:W_], [psp], [pool.buf(p2)])
        psr = self.next_ps()
        P.op("pe", [self.mm(psr.h[:, h * 128:(h + 1) * 128], pool.f32(q2)[:, h * 128:(h + 1) * 128],
                            rf[:, h * 128:(h + 1) * 128], True, True) for h in range(n)],
             [pool.buf(q2), pool.buf(r_)], [psr])
        self.tt("dve", rf[:, 0:W_], rf[:, 0:W_], psr.h[:, 0:W_], ALU.add, [pool.buf(r_), psr], [pool.buf(r_)])
        pool.put(p_, q_)
        if not last:
            p_, q_ = p2, q2
        else:
            pool.put(q2)
    return r_


def gdn(self, l, t):
    P, cfg, pool = self.P, self.cfg, self.pool
    TT = cfg.TT
    NCH = TT // 128
    pl = self.pp[l]
    S, Sb = self.gd_S[l], self.gd_Sb[l]
    for h in range(4):
        self.load_rowbcast(self.rown, h * 128, self.rb_d[l:l + 1, RB_OFF["gdn_norm"]:RB_OFF["gdn_norm"] + 128])
    wz = self.load_w(self.w_in_d[l, :, GDN0 + 1536:GDN0 + 2056], ncols=520)
    ps_a = self.inproj_fm(wz, 512, 4)
    ps_b = self.inproj_fm(wz, 516, 4)
    sl = [pool.get() for _ in range(5)]
    r_beta, r_g, r_ng, r_bg, r_kd = [pool.f32(s)[0:4, 0:TT] for s in sl]
    b_beta, b_g, b_ng, b_bg, b_kd = [pool.buf(s) for s in sl]
    self.act(r_beta, ps_b.h[0:4, 0:TT], AF.Sigmoid, [ps_b], [b_beta])
    self.act(r_ng, ps_a.h[0:4, 0:TT], AF.Exp, [ps_a, pl], [b_ng], bias=self.ppc(l, "gdn_dt_bias")[0:4])
    self.act(r_ng, r_ng, AF.Ln, [b_ng], [b_ng], bias=1.0)
    self.ts("dve", r_ng, r_ng, self.gd_sc[l].h[0:4, 0:1], None, ALU.mult, None, [b_ng, self.gd_sc[l]], [b_ng])
    cm = self.consts.h[0:4, C_CMASK:C_CMASK + TT]
    P.op("dve", lambda e: e.tensor_tensor_scan(out=r_g, data0=cm, data1=r_ng, initial=0.0, op0=ALU.mult, op1=ALU.add),
         [self.consts, b_ng], [b_g])
    self.ts("dve", r_ng, r_g, -1.0, None, ALU.mult, None, [b_g], [b_ng])
    self.act(r_bg, r_g, AF.Exp, [b_g], [b_bg])
    self.tt("dve", r_bg, r_bg, r_beta, ALU.mult, [b_bg, b_beta], [b_bg])
    for c in range(NCH):
        self.ts("dve", r_kd[:, c * 128:(c + 1) * 128], r_ng[:, c * 128:(c + 1) * 128], r_g[:, c * 128 + 127:c * 128 + 128],
                None, ALU.add, None, [b_ng, b_g], [b_kd])
    self.act(r_kd, r_kd, AF.Exp, [b_kd], [b_kd])
    rows_to_tok(self, [(r_ng, b_ng), (r_beta, b_beta), (r_bg, b_bg), (r_kd, b_kd)], 0)

    def tk(q, c, h):
        o = (q * NCH + c) * 4 + h
        return self.tok.h[:, o:o + 1]
    gbc = []
    for h in range(4):
        ps = gate_rows_to_bcast(self, r_g, b_g, h)
        o = pool.get()
        self.cp("act", pool.f32(o)[:, 0:TT], ps.h[:, 0:TT], [ps], [pool.buf(o)])
        gbc.append(o)
    pool.put(*sl)
    gzs = [pool.get() for _ in range(NCH // 2)]

    def gz(c):
        return pool.bf(gzs[c // 2])[:, (c % 2) * 512:(c % 2) * 512 + 512], pool.buf(gzs[c // 2])
    for c in range(NCH):
        ps_z = self.inproj_tm(wz, 0, 512, c)
        zt = pool.get()
        self.act(pool.f32(zt), ps_z.h[:, 0:512], AF.Silu, [ps_z], [pool.buf(zt)])
        ga, gb = gz(c)
        self.tt("dve", ga, pool.f32(zt), self.rown.h[:, 0:512], ALU.mult, [pool.buf(zt), self.rown], [gb])
        pool.put(zt)
    wq = self.load_w(self.w_in_d[l, :, GDN0:GDN0 + 512])
    wk = self.load_w(self.w_in_d[l, :, GDN0 + 512:GDN0 + 1024])
    wv = self.load_w(self.w_in_d[l, :, GDN0 + 1024:GDN0 + 1536])
    qk = [pool.get() for _ in range(4)]
    vq = [pool.get() for _ in range(4)]
    for h in range(4):
        ps = self.inproj_fm(wq, h * 128, 128)
        o = self.conv4(l, ps, 128, "gdn_conv", h, self.halo[l], self.halo[l].bufs[h], h * 3)
        qf = pool.f32(o)[:, 0:TT]
        self.act(qf, qf, AF.Silu, [pool.buf(o)], [pool.buf(o)])
        l2norm_fm(self, o, 128.0 ** -0.5)
        self.cp("act", pool.bf(qk[h])[:, 0:TT], qf, [pool.buf(o)], [pool.buf(qk[h])])
        e_ = pool.get()
        self.act(pool.f32(e_)[:, 0:TT], pool.f32(gbc[h])[:, 0:TT], AF.Exp, [pool.buf(gbc[h])], [pool.buf(e_)])
        self.tt("dve", pool.bf(vq[h])[:, 512:512 + TT], qf, pool.f32(e_)[:, 0:TT], ALU.mult,
                [pool.buf(o), pool.buf(e_)], [pool.buf(vq[h])])
        pool.put(o, e_)
        ps = self.inproj_fm(wk, h * 128, 128)
        o = self.conv4(l, ps, 128, "gdn_conv", 4 + h, self.halo[l], self.halo[l].bufs[4 + h], (4 + h) * 3)
        kf = pool.f32(o)[:, 0:TT]
        self.act(kf, kf, AF.Silu, [pool.buf(o)], [pool.buf(o)])
        l2norm_fm(self, o, 1.0)
        self.cp("act", pool.bf(qk[h])[:, 512:512 + TT], kf, [pool.buf(o)], [pool.buf(qk[h])])
        pool.put(o)
        ps = self.inproj_fm(wv, h * 128, 128)
        o = self.conv4(l, ps, 128, "gdn_conv", 8 + h, self.halo[l], self.halo[l].bufs[8 + h], (8 + h) * 3)
        vf = pool.f32(o)[:, 0:TT]
        self.act(pool.bf(vq[h])[:, 0:TT], vf, AF.Silu, [pool.buf(o)], [pool.buf(vq[h])])
        pool.put(o)
    ident = self.consts.h[:, C_IDENT:C_IDENT + 128]
    for c in range(NCH):
        cs = slice(c * 128, (c + 1) * 128)
        ks = slice(512 + c * 128, 512 + (c + 1) * 128)
        kk_ps = self.next_ps()
        P.op("pe", [self.mm(kk_ps.h[:, h * 128:(h + 1) * 128], pool.bf(qk[h])[:, ks], pool.bf(qk[h])[:, ks], True, True)
                    for h in range(4)], [pool.buf(qk[h]) for h in range(4)], [kk_ps])
        kq_ps = self.next_ps()
        P.op("pe", [self.mm(kq_ps.h[:, h * 128:(h + 1) * 128], pool.bf(qk[h])[:, ks], pool.bf(qk[h])[:, cs], True, True)
                    for h in range(4)], [pool.buf(qk[h]) for h in range(4)], [kq_ps])
        a_ = pool.get()
        af = pool.f32(a_)
        d_ = pool.get()
        df = pool.f32(d_)
        for h in range(4):
            hs = slice(h * 128, (h + 1) * 128)
            self.ts("dve", af[:, hs], pool.f32(gbc[h])[:, cs], tk(0, c, h), 0.0, ALU.add, ALU.max,
                    [pool.buf(gbc[h]), self.tok], [pool.buf(a_)])
        for h in range(4):
            hs = slice(h * 128, (h + 1) * 128)
            self.ts("dve", df[:, hs], pool.f32(gbc[h])[:, cs], tk(0, c, h), 0.0, ALU.add, ALU.min,
                    [pool.buf(gbc[h]), self.tok], [pool.buf(d_)])
        self.act(af, af, AF.Exp, [pool.buf(a_)], [pool.buf(a_)], scale=-1.0)
        self.act(df, df, AF.Exp, [pool.buf(d_)], [pool.buf(d_)])
        for h in range(4):
            hs = slice(h * 128, (h + 1) * 128)
            self.stt(af[:, hs], af[:, hs], tk(1, c, h), self.consts.h[:, C_MSL:C_MSL + 128], ALU.mult, ALU.mult,
                     [pool.buf(a_), self.tok, self.consts], [pool.buf(a_)])
        for h in range(4):
            hs = slice(h * 128, (h + 1) * 128)
            self.tt("dve", df[:, hs], df[:, hs], self.consts.h[:, C_MIU:C_MIU + 128], ALU.mult,
                    [pool.buf(d_), self.consts], [pool.buf(d_)])
        self.tt("dve", af, af, kk_ps.h[:, 0:512], ALU.mult, [pool.buf(a_), kk_ps], [pool.buf(a_)])
        at_ = pool.get()
        atb = pool.bf(at_)[:, 0:512]
        self.tt("dve", atb, df, kq_ps.h[:, 0:512], ALU.mult, [pool.buf(d_), kq_ps], [pool.buf(at_)])
        pool.put(d_)
        u_ = pool.get()
        ut_ps = self.next_ps()
        P.op("pe", [self.trn(ut_ps.h[:, h * 128:(h + 1) * 128], af[:, h * 128:(h + 1) * 128], ident) for h in range(4)],
             [pool.buf(a_), self.consts], [ut_ps])
        self.cp("act", pool.f32(u_), ut_ps.h[:, 0:512], [ut_ps], [pool.buf(u_)])
        r_ = tri_inverse_T(self, a_, u_)
        rf = pool.f32(r_)
        kt_ps = self.next_ps()
        ktb = kt_ps.h[:].bitcast(BF16)
        P.op("pe", [self.trn(ktb[:, h * 128:(h + 1) * 128], pool.bf(qk[h])[:, ks], self.ident_b.h[:]) for h in range(4)],
             [pool.buf(qk[h]) for h in range(4)] + [self.ident_b], [kt_ps])
        vt_ps = self.next_ps()
        vtb = vt_ps.h[:].bitcast(BF16)
        P.op("pe", [self.trn(vtb[:, h * 128:(h + 1) * 128], pool.bf(vq[h])[:, cs], self.ident_b.h[:]) for h in range(4)],
             [pool.buf(vq[h]) for h in range(4)] + [self.ident_b], [vt_ps])
        vb_, kg_, kd_ = pool.get(), pool.get(), pool.get()
        for h in range(4):
            hs = slice(h * 128, (h + 1) * 128)
            self.ts("dve", pool.f32(vb_)[:, hs], vtb[:, hs], tk(1, c, h), None, ALU.mult, None, [vt_ps, self.tok], [pool.buf(vb_)])
            self.ts("dve", pool.f32(kg_)[:, hs], ktb[:, hs], tk(2, c, h), None, ALU.mult, None, [kt_ps, self.tok], [pool.buf(kg_)])
            self.ts("dve", pool.bf(kd_)[:, hs], ktb[:, hs], tk(3, c, h), None, ALU.mult, None, [kt_ps, self.tok], [pool.buf(kd_)])
        u_ps = self.next_ps()
        P.op("pe", [self.mm(u_ps.h[:, h * 128:(h + 1) * 128], rf[:, h * 128:(h + 1) * 128], pool.f32(vb_)[:, h * 128:(h + 1) * 128],
                            True, True) for h in range(4)], [pool.buf(r_), pool.buf(vb_)], [u_ps])
        w_ps = self.next_ps()
        P.op("pe", [self.mm(w_ps.h[:, h * 128:(h + 1) * 128], pool.f32(kg_)[:, h * 128:(h + 1) * 128], rf[:, h * 128:(h + 1) * 128],
                            True, True) for h in range(4)], [pool.buf(r_), pool.buf(kg_)], [w_ps])
        self.cp("act", pool.f32(vb_), u_ps.h[:, 0:512], [u_ps], [pool.buf(vb_)])
        self.cp("dve", pool.bf(kg_)[:, 0:512], w_ps.h[:, 0:512], [w_ps], [pool.buf(kg_)])
        pool.put(r_)
        uf = pool.f32(vb_)
        wtb = pool.bf(kg_)[:, 0:512]
        kdb = pool.bf(kd_)[:, 0:512]
        y_ = pool.get()
        yb = pool.bf(y_)[:, 0:512]
        vn_ = pool.get()
        ga, gb = gz(c)
        vnb = pool.bf(vn_)[:, 0:512]
        H4 = [slice(h * 128, (h + 1) * 128) for h in range(4)]
        ws_ps = self.next_ps()
        P.op("pe", [self.mm(ws_ps.h[:, H4[h]], wtb[:, H4[h]], Sb.h[:, h, :], True, True) for h in range(4)],
             [pool.buf(kg_)] + Sb.bufs, [ws_ps])
        self.tt("dve", vnb, uf, ws_ps.h[:, 0:512], ALU.subtract, [pool.buf(vb_), ws_ps], [pool.buf(vn_)])
        o_ps = self.next_ps()
        fns = []
        for h in range(4):
            fns.append(self.mm(o_ps.h[:, H4[h]], pool.bf(vq[h])[:, ks], Sb.h[:, h, :], True, False))
            fns.append(self.mm(o_ps.h[:, H4[h]], atb[:, H4[h]], vnb[:, H4[h]], False, True))
        P.op("pe", fns, [pool.buf(vq[h]) for h in range(4)] + Sb.bufs + [pool.buf(at_), pool.buf(vn_)], [o_ps])
        up_ps = self.next_ps()
        P.op("pe", [self.mm(up_ps.h[:, H4[h]], kdb[:, H4[h]], vnb[:, H4[h]], True, True) for h in range(4)],
             [pool.buf(kd_), pool.buf(vn_)], [up_ps])
        eg, egb = self.scal4()
        for h in range(4):
            self.act(eg[:, h:h + 1], pool.f32(gbc[h])[:, c * 128 + 127:c * 128 + 128], AF.Exp, [pool.buf(gbc[h])], [egb])
        for h in range(4):
            self.stt(S.h[:, h, :], S.h[:, h, :], eg[:, h:h + 1], up_ps.h[:, H4[h]], ALU.mult, ALU.add,
                     [S.bufs[h], egb, up_ps], [S.bufs[h]])
        self.cp("act", Sb.h[:], S.h[:], S.bufs, Sb.bufs)
        ss, ssb = self.scal4()
        j_ = pool.get()
        for h in range(4):
            self.act(pool.f32(j_)[:, H4[h]], o_ps.h[:, H4[h]], AF.Square, [o_ps], [pool.buf(j_), ssb], accum_out=ss[:, h:h + 1])
        pool.put(j_)
        self.act(ss, ss, AF.Sqrt, [ssb], [ssb], bias=NORM_EPS, scale=1.0 / 128)
        P.op("dve", lambda e, ss=ss: e.reciprocal(out=ss, in_=ss), [ssb], [ssb])
        for h in range(4):
            self.stt(yb[:, H4[h]], o_ps.h[:, H4[h]], ss[:, h:h + 1], ga[:, H4[h]], ALU.mult, ALU.mult, [o_ps, ssb, gb], [pool.buf(y_)])
        yt_ps = self.next_ps()
        ytb = yt_ps.h[:].bitcast(BF16)
        P.op("pe", [self.trn(ytb[:, h * 128:(h + 1) * 128], yb[:, h * 128:(h + 1) * 128], self.ident_b.h[:]) for h in range(4)],
             [pool.buf(y_), self.ident_b], [yt_ps])
        self.cp("act", self.mixT.h[:, 0:4, cs], ytb[:, 0:512].rearrange("p (h c) -> p h c", h=4), [yt_ps],
                [self.mixT.bufs[h] for h in range(4)])
        pool.put(at_, vb_, kg_, kd_, y_, vn_)
    pool.put(*gbc)
    pool.put(*gzs)
    pool.put(*qk)
    pool.put(*vq)


Kern.gdn = gdn

RW_LW = 0.6065306597126334


def setup_rwkv(self):
    P, cfg = self.P, self.cfg
    L = cfg.depth
    self.rw_M = [P.sb("rw_M%d" % l, [128, 4, 64], F32, nbufs=4) for l in range(L)]
    self.rw_Mb = [P.sb("rw_Mb%d" % l, [128, 4, 64], BF16, nbufs=4) for l in range(L)]
    self.rw_halo = [P.sb("rw_halo%d" % l, [128, 14], F32, nbufs=14) for l in range(L)]
    self.rw_sc = [P.sb("rw_sc%d" % l, [128, 4], F32) for l in range(L)]
    self.rw_w2p = P.sb("rw_w2p", [128, 512], BF16)
    self.rw_a2p = P.sb("rw_a2p", [128, 512], BF16)
    self.rw_g2 = P.sb("rw_g2", [128, 512], BF16)
    self.rowb = P.sb("rowb", [128, 512], F32)
    self.blk_b = P.sb("blk_b", [128, 128], BF16)
    self.sel2_b = P.sb("sel2_b", [128, 2], BF16)
    self.rkd = P.sb("rkd", [128, 4, 8], F32, nbufs=4)
    self.gC = P.sb("gC", [128, 4], F32)
    P.op("pool", lambda e: e.memset(self.rw_w2p.h[:], 0.0), [], [self.rw_w2p])
    P.op("pool", lambda e: e.memset(self.rw_a2p.h[:], 0.0), [], [self.rw_a2p])
    P.op("pool", lambda e: e.memset(self.blk_b.h[:], 0.0), [], [self.blk_b])
    P.op("pool", lambda e: e.memset(self.blk_b.h[0:64, 0:64], 1.0), [], [self.blk_b])
    P.op("pool", lambda e: e.memset(self.blk_b.h[64:128, 64:128], 1.0), [], [self.blk_b])
    self.cp("dve", self.sel2_b.h[:], self.consts.h[:, C_SEL2:C_SEL2 + 2], [self.consts], [self.sel2_b])
    for l in range(L):
        P.op("pool", lambda e, l=l: e.memset(self.rw_M[l].h[:], 0.0), [], self.rw_M[l].bufs)
        P.op("pool", lambda e, l=l: e.memset(self.rw_Mb[l].h[:], 0.0), [], self.rw_Mb[l].bufs)
        P.op("pool", lambda e, l=l: e.memset(self.rw_halo[l].h[:], 0.0), [], self.rw_halo[l].bufs)
        ka = self.pp[l].h[:, PP_OFF["rwkv_k_a"]:PP_OFF["rwkv_k_a"] + 4]
        self.ts("dve", self.rw_sc[l].h[:], ka, -1.0, 1.0, ALU.mult, ALU.add, [self.pp[l]], [self.rw_sc[l]])


def shift_lerp(self, l, ps, ci):
    pool = self.pool
    TT = self.cfg.TT
    cb = self.cbuf[self.cb_rr % 2]
    self.cb_rr += 1
    hl = self.rw_halo[l]
    self.cp("act", cb.h[:, 1:1 + TT], ps.h[:, 0:TT], [ps], [cb])
    self.cp("dve", cb.h[:, 0:1], hl.h[:, ci:ci + 1], [hl.bufs[ci]], [cb])
    o = pool.get()
    of = pool.f32(o)[:, 0:TT]
    self.tt("dve", of, cb.h[:, 0:TT], cb.h[:, 1:1 + TT], ALU.subtract, [cb], [pool.buf(o)])
    self.stt(of, of, self.ppc(l, "rwkv_mu", ci), cb.h[:, 1:1 + TT], ALU.mult, ALU.add, [pool.buf(o), self.pp[l], cb], [pool.buf(o)])
    self.cp("dve", hl.h[:, ci:ci + 1], cb.h[:, TT:TT + 1], [cb], [hl.bufs[ci]])
    return o


def tri_inverse_bf(self, a_, u_, n=2):
    P, pool = self.P, self.pool
    W_ = n * 128
    ident = self.consts.h[:, C_IDENT:C_IDENT + 128]
    r_ = pool.get()
    rf = pool.f32(r_)[:, 0:W_]
    rb = pool.bf(r_)[:, 512:512 + W_]
    for h in range(n):
        self.tt("dve", rf[:, h * 128:(h + 1) * 128], ident, pool.bf(u_)[:, h * 128:(h + 1) * 128], ALU.subtract,
                [self.consts, pool.buf(u_)], [pool.buf(r_)])
    self.cp("act", rb, rf, [pool.buf(r_)], [pool.buf(r_)])
    p_, q_ = u_, a_
    for k in range(1, 7):
        last = k == 6
        q2 = pool.get()
        psq = self.next_ps()
        P.op("pe", [self.mm(psq.h[:, h * 128:(h + 1) * 128], pool.bf(p_)[:, h * 128:(h + 1) * 128],
                            pool.bf(q_)[:, h * 128:(h + 1) * 128], True, True) for h in range(n)],
             [pool.buf(p_), pool.buf(q_)], [psq])
        self.cp("act", pool.bf(q2)[:, 0:W_], psq.h[:, 0:W_], [psq], [pool.buf(q2)])
        if not last:
            p2 = pool.get()
            psp = self.next_ps()
            P.op("pe", [self.mm(psp.h[:, h * 128:(h + 1) * 128], pool.bf(q_)[:, h * 128:(h + 1) * 128],
                                pool.bf(p_)[:, h * 128:(h + 1) * 128], True, True) for h in range(n)],
                 [pool.buf(p_), pool.buf(q_)], [psp])
            self.cp("dve", pool.bf(p2)[:, 0:W_], psp.h[:, 0:W_], [psp], [pool.buf(p2)])
        psr = self.next_ps()
        P.op("pe", [self.mm(psr.h[:, h * 128:(h + 1) * 128], pool.bf(q2)[:, h * 128:(h + 1) * 128],
                            rb[:, h * 128:(h + 1) * 128], True, True) for h in range(n)],
             [pool.buf(q2), pool.buf(r_)], [psr])
        self.tt("dve", rf, rf, psr.h[:, 0:W_], ALU.add, [pool.buf(r_), psr], [pool.buf(r_)])
        self.cp("act", rb, rf, [pool.buf(r_)], [pool.buf(r_)])
        pool.put(p_, q_)
        if not last:
            p_, q_ = p2, q2
        else:
            pool.put(q2)
    return r_


def rwkv(self, l, t):
    P, cfg, pool = self.P, self.cfg, self.pool
    TT = cfg.TT
    NCH = TT // 128
    pl = self.pp[l]
    M, Mb = self.rw_M[l], self.rw_Mb[l]
    ident = self.consts.h[:, C_IDENT:C_IDENT + 128]
    msl = self.consts.h[:, C_MSL:C_MSL + 128]
    msu = self.consts.h[:, C_MSU:C_MSU + 128]
    miu = self.consts.h[:, C_MIU:C_MIU + 128]
    P.dma("pool", self.rw_w2p.h[0:64, :], self.rw_w2_d[l], writes=[self.rw_w2p])
    P.dma("pool", self.rw_a2p.h[64:128, :], self.rw_a2_d[l], writes=[self.rw_a2p])
    P.dma("pool", self.rw_g2.h[:], self.rw_g2_d[l], writes=[self.rw_g2])
    self.load_rowbcast(self.rown, 0, self.rb_d[l:l + 1, RB_OFF["rwkv_ln_w"]:RB_OFF["rwkv_ln_w"] + 512])
    self.load_rowbcast(self.rowb, 0, self.rb_d[l:l + 1, RB_OFF["rwkv_ln_b"]:RB_OFF["rwkv_ln_b"] + 512])
    wl = self.load_w(self.w_in_d[l, :, RW0 + 1536:RW0 + 1792], ncols=256)
    ps = self.inproj_fm(wl, 0, 128)
    lo = shift_lerp(self, l, ps, 12)
    lo_ = pool.get()
    self.act(pool.bf(lo_)[:, 0:TT], pool.f32(lo)[:, 0:TT], AF.Tanh, [pool.buf(lo)], [pool.buf(lo_)])
    self.cp("dve", pool.bf(lo_)[:, 512:512 + TT], pool.f32(lo)[:, 0:TT], [pool.buf(lo)], [pool.buf(lo_)])
    pool.put(lo)
    ps = self.inproj_fm(wl, 128, 128)
    go = shift_lerp(self, l, ps, 13)
    sg_ = pool.get()
    self.act(pool.bf(sg_)[:, 0:TT], pool.f32(go)[:, 0:TT], AF.Sigmoid, [pool.buf(go)], [pool.buf(sg_)])
    pool.put(go)
    wr = self.load_w(self.w_in_d[l, :, RW0:RW0 + 512])
    wk = self.load_w(self.w_in_d[l, :, RW0 + 512:RW0 + 1024])
    wv = self.load_w(self.w_in_d[l, :, RW0 + 1024:RW0 + 1536])
    ytok = [pool.get() for _ in range(NCH)]
    vtk = [pool.get() for _ in range(NCH // 2)]

    def vtok(b):
        return pool.bf(vtk[b // 2])[:, (b % 2) * 512:(b % 2) * 512 + 512], pool.buf(vtk[b // 2])
    for cp in range(4):
        pcs = slice(cp * 128, (cp + 1) * 128)
        ps = self.inproj_fm(wr, cp * 128, 128)
        r_ = shift_lerp(self, l, ps, cp)
        ps = self.inproj_fm(wk, cp * 128, 128)
        k_ = shift_lerp(self, l, ps, 4 + cp)
        ps = self.inproj_fm(wv, cp * 128, 128)
        v_ = shift_lerp(self, l, ps, 8 + cp)
        rf, kf, vf = [pool.f32(s)[:, 0:TT] for s in (r_, k_, v_)]
        vk_ = pool.get()
        self.cp("act", pool.bf(vk_)[:, 0:TT], vf, [pool.buf(v_)], [pool.buf(vk_)])
        pool.put(v_)
        ps_w = self.next_ps()
        P.op("pe", [self.mm(ps_w.h[:, 0:TT], self.rw_w2p.h[:, pcs], pool.bf(lo_)[:, 0:TT], True, True)],
             [self.rw_w2p, pool.buf(lo_)], [ps_w])
        sw_ = pool.get()
        swf = pool.f32(sw_)[:, 0:TT]
        self.act(swf, ps_w.h[:, 0:TT], AF.Sigmoid, [ps_w, pl], [pool.buf(sw_)], bias=self.ppc(l, "rwkv_w0", cp))
        ps_a = self.next_ps()
        P.op("pe", [self.mm(ps_a.h[:, 0:TT], self.rw_a2p.h[:, pcs], pool.bf(lo_)[:, 512:512 + TT], True, True)],
             [self.rw_a2p, pool.buf(lo_)], [ps_a])
        a_ = pool.get()
        af = pool.f32(a_)[:, 0:TT]
        self.act(af, ps_a.h[:, 0:TT], AF.Sigmoid, [ps_a, pl], [pool.buf(a_)], bias=self.ppc(l, "rwkv_a0", cp))
        kk_ = pool.get()
        kkf = pool.f32(kk_)[:, 0:TT]
        self.ts("dve", kkf, kf, self.ppc(l, "rwkv_k_k", cp), None, ALU.mult, None, [pool.buf(k_), pl], [pool.buf(kk_)])
        sq = pool.get()
        self.act(pool.bf(sq)[:, 0:TT], kkf, AF.Square, [pool.buf(kk_)], [pool.buf(sq)])
        pss = self.next_ps()
        P.op("pe", [self.mm(pss.h[:, 0:TT], self.blk_b.h[:], pool.bf(sq)[:, 0:TT], True, True)], [self.blk_b, pool.buf(sq)], [pss])
        rn = pool.f32(sq)[:, 0:TT]
        self.act(rn, pss.h[:, 0:TT], AF.Sqrt, [pss, pool.buf(sq)], [pool.buf(sq)], bias=1e-6)
        P.op("dve", lambda e, rn=rn: e.reciprocal(out=rn, in_=rn), [pool.buf(sq)], [pool.buf(sq)])
        self.tt("dve", kkf, kkf, rn, ALU.mult, [pool.buf(kk_), pool.buf(sq)], [pool.buf(kk_)])
        fac = pool.f32(sq)[:, 0:TT]
        self.ts("dve", fac, af, self.ppc(l, "rwkv_k_a", cp), self.rw_sc[l].h[:, cp:cp + 1], ALU.mult, ALU.add,
                [pool.buf(a_), pl, self.rw_sc[l], pool.buf(sq)], [pool.buf(sq)])
        self.tt("dve", kf, kf, fac, ALU.mult, [pool.buf(k_), pool.buf(sq)], [pool.buf(k_)])
        self.stt(pool.bf(vk_)[:, 512:512 + TT], rf, self.ppc(l, "rwkv_r_k", cp), kf, ALU.mult, ALU.mult,
                 [pool.buf(r_), pl, pool.buf(k_)], [pool.buf(vk_)])
        self.tt("dve", af, af, kkf, ALU.mult, [pool.buf(a_), pool.buf(kk_)], [pool.buf(a_)])
        cs_ = pool.get()
        csf = pool.f32(cs_)[:, 0:TT]
        cm = self.consts.h[:, C_CMASK:C_CMASK + TT]
        P.op("dve", lambda e, csf=csf, cm=cm, swf=swf: e.tensor_tensor_scan(out=csf, data0=cm, data1=swf, initial=0.0,
                                                                               op0=ALU.mult, op1=ALU.add),
             [self.consts, pool.buf(sw_)], [pool.buf(cs_)])
        self.tt("dve", swf, csf, swf, ALU.subtract, [pool.buf(cs_), pool.buf(sw_)], [pool.buf(sw_)])
        ex = pool.f32(sq)[:, 0:TT]
        for b in range(NCH):
            self.act(self.gC.h[:, b:b + 1], csf[:, b * 128 + 127:b * 128 + 128], AF.Exp, [pool.buf(cs_)], [self.gC], scale=-RW_LW)
        ar_ = [pool.get(), pool.get()]
        for e in range(2):
            zap = pool.bf(ar_[e])[(1 - e) * 64:(2 - e) * 64, :]
            P.op("pool", lambda en, zap=zap: en.memset(zap, 0.0), [], [pool.buf(ar_[e])])
        self.act(ex, swf, AF.Exp, [pool.buf(sw_), pool.buf(sq)], [pool.buf(sq)], scale=-RW_LW)
        for e in range(2):
            hp = slice(e * 64, (e + 1) * 64)
            self.stt(pool.bf(ar_[e])[hp, 0:TT], kkf[hp], -1.0, ex[hp], ALU.mult, ALU.mult,
                     [pool.buf(kk_), pool.buf(sq)], [pool.buf(ar_[e])])
        self.act(ex, csf, AF.Exp, [pool.buf(cs_), pool.buf(sq)], [pool.buf(sq)], scale=-RW_LW)
        for e in range(2):
            hp = slice(e * 64, (e + 1) * 64)
            self.tt("dve", pool.bf(ar_[e])[hp, 512:512 + TT], rf[hp], ex[hp], ALU.mult,
                    [pool.buf(r_), pool.buf(sq)], [pool.buf(ar_[e])])
        self.act(ex, csf, AF.Exp, [pool.buf(cs_), pool.buf(sq)], [pool.buf(sq)], scale=RW_LW)
        bk_ = pool.get()
        self.tt("dve", pool.bf(bk_)[:, 0:TT], af, ex, ALU.mult, [pool.buf(a_), pool.buf(sq)], [pool.buf(bk_)])
        self.tt("dve", pool.bf(bk_)[:, 512:512 + TT], kf, ex, ALU.mult, [pool.buf(k_), pool.buf(sq)], [pool.buf(bk_)])
        pool.put(r_, k_, sw_, a_, kk_, sq, cs_)
        bt = pool.bf(bk_)[:, 0:TT]
        kt = pool.bf(bk_)[:, 512:512 + TT]
        for b in range(NCH):
            bs = slice(b * 128, (b + 1) * 128)
            at = [pool.bf(ar_[e])[:, b * 128:(b + 1) * 128] for e in range(2)]
            rt = [pool.bf(ar_[e])[:, 512 + b * 128:512 + (b + 1) * 128] for e in range(2)]
            arb = [pool.buf(ar_[0]), pool.buf(ar_[1])]
            bkb = pool.buf(bk_)
            l_ps, u_ps, ak_ps, rb_ps, rk_ps = [self.next_ps() for _ in range(5)]
            P.op("pe", [self.mm(l_ps.h[:, e * 128:(e + 1) * 128], at[e], bt[:, bs], True, True) for e in range(2)], arb + [bkb], [l_ps])
            P.op("pe", [self.mm(u_ps.h[:, e * 128:(e + 1) * 128], bt[:, bs], at[e], True, True) for e in range(2)], arb + [bkb], [u_ps])
            P.op("pe", [self.mm(ak_ps.h[:, e * 128:(e + 1) * 128], kt[:, bs], at[e], True, True) for e in range(2)], arb + [bkb], [ak_ps])
            P.op("pe", [self.mm(rb_ps.h[:, e * 128:(e + 1) * 128], bt[:, bs], rt[e], True, True) for e in range(2)], arb + [bkb], [rb_ps])
            P.op("pe", [self.mm(rk_ps.h[:, e * 128:(e + 1) * 128], kt[:, bs], rt[e], True, True) for e in range(2)], arb + [bkb], [rk_ps])
            la_, ua_, m3_ = pool.get(), pool.get(), pool.get()
            m3 = pool.bf(m3_)
            for e in range(2):
                es = slice(e * 128, (e + 1) * 128)
                self.stt(pool.bf(la_)[:, es], l_ps.h[:, es], -1.0, msl, ALU.mult, ALU.mult, [l_ps, self.consts], [pool.buf(la_)])
                self.stt(pool.bf(ua_)[:, es], u_ps.h[:, es], -1.0, msu, ALU.mult, ALU.mult, [u_ps, self.consts], [pool.buf(ua_)])
                self.tt("dve", m3[:, e * 128:(e + 1) * 128], ak_ps.h[:, es], msu, ALU.mult, [ak_ps, self.consts], [pool.buf(m3_)])
                self.tt("dve", m3[:, 256 + e * 128:256 + (e + 1) * 128], rb_ps.h[:, es], miu, ALU.mult, [rb_ps, self.consts], [pool.buf(m3_)])
                self.tt("dve", m3[:, 512 + e * 128:512 + (e + 1) * 128], rk_ps.h[:, es], miu, ALU.mult, [rk_ps, self.consts], [pool.buf(m3_)])
            r2_ = tri_inverse_bf(self, la_, ua_, n=2)
            tr_ps = self.next_ps()
            trb = tr_ps.h[:].bitcast(BF16)
            P.op("pe", [self.trn(trb[:, 0:128], bt[:, bs], self.ident_b.h[:]),
                        self.trn(trb[:, 128:256], kt[:, bs], self.ident_b.h[:]),
                        self.trn(trb[:, 256:384], pool.bf(vk_)[:, bs], self.ident_b.h[:])],
                 [bkb, pool.buf(vk_), self.ident_b], [tr_ps])
            tk_ = pool.get()
            tkb = pool.bf(tk_)
            self.cp("act", tkb[:, 0:384], trb[:, 0:384], [tr_ps], [pool.buf(tk_)])
            va, vbuf = vtok(b)
            self.cp("dve", va[:, pcs], trb[:, 256:384], [tr_ps], [vbuf])
            rk2 = self.next_ps()
            P.op("pe", [self.mm(rk2.h[:, 0:2], pool.bf(vk_)[:, 512 + b * 128:512 + (b + 1) * 128], self.sel2_b.h[:], True, True)],
                 [pool.buf(vk_), self.sel2_b], [rk2])
            self.cp("dve", self.rkd.h[:, b, cp * 2:cp * 2 + 2], rk2.h[:, 0:2], [rk2], [self.rkd.bufs[b]])
            x_ps = self.next_ps()
            fns = []
            for e in range(2):
                es64 = slice(e * 64, (e + 1) * 64)
                fns.append(self.mm(x_ps.h[:, es64], at[e], Mb.h[:, cp, :], True, False))
                fns.append(self.mm(x_ps.h[:, es64], m3[:, e * 128:(e + 1) * 128], tkb[:, 256 + e * 64:256 + (e + 1) * 64], False, True))
            P.op("pe", fns, arb + [Mb.bufs[cp], pool.buf(m3_), pool.buf(tk_)], [x_ps])
            x_ = pool.get()
            xf = pool.bf(x_)[:, 0:128]
            self.cp("act", xf, x_ps.h[:, 0:128], [x_ps], [pool.buf(x_)])
            p_ps = self.next_ps()
            P.op("pe", [self.mm(p_ps.h[:, e * 64:(e + 1) * 64], pool.bf(r2_)[:, 512 + e * 128:512 + (e + 1) * 128], xf[:, e * 64:(e + 1) * 64], True, True)
                        for e in range(2)], [pool.buf(r2_), pool.buf(x_)], [p_ps])
            self.cp("act", tkb[:, 384:512], p_ps.h[:, 0:128], [p_ps], [pool.buf(tk_)])
            pool.put(r2_, x_)
            y_ps = self.next_ps()
            fns = []
            for e in range(2):
                es64 = slice(e * 64, (e + 1) * 64)
                fns.append(self.mm(y_ps.h[:, es64], rt[e], Mb.h[:, cp, :], True, False))
                fns.append(self.mm(y_ps.h[:, es64], m3[:, 256 + e * 128:256 + (e + 1) * 128], tkb[:, 384 + e * 64:384 + (e + 1) * 64], False, False))
                fns.append(self.mm(y_ps.h[:, es64], m3[:, 512 + e * 128:512 + (e + 1) * 128], tkb[:, 256 + e * 64:256 + (e + 1) * 64], False, True))
            P.op("pe", fns, arb + [Mb.bufs[cp], pool.buf(m3_), pool.buf(tk_)], [y_ps])
            self.cp("act", pool.f32(ytok[b])[:, pcs], y_ps.h[:, 0:128], [y_ps], [pool.buf(ytok[b])])
            m_ps = self.next_ps()
            fns = []
            for e in range(2):
                es64 = slice(e * 64, (e + 1) * 64)
                fns.append(self.mm(m_ps.h[:, es64], tkb[:, 0:128], tkb[:, 384 + e * 64:384 + (e + 1) * 64], True, False))
                fns.append(self.mm(m_ps.h[:, es64], tkb[:, 128:256], tkb[:, 256 + e * 64:256 + (e + 1) * 64], False, True))
            P.op("pe", fns, [pool.buf(tk_)], [m_ps])
            for e in range(2):
                es64 = slice(e * 64, (e + 1) * 64)
                self.tt("dve", M.h[es64, cp, :], M.h[es64, cp, :], m_ps.h[es64, e * 64:(e + 1) * 64], ALU.add,
                        [M.bufs[cp], m_ps], [M.bufs[cp]])
            self.ts("dve", M.h[:, cp, :], M.h[:, cp, :], self.gC.h[:, b:b + 1], None, ALU.mult, None, [M.bufs[cp], self.gC], [M.bufs[cp]])
            self.cp("act", Mb.h[:, cp, :], M.h[:, cp, :], [M.bufs[cp]], [Mb.bufs[cp]])
            pool.put(m3_, tk_)
        pool.put(vk_, bk_, *ar_)
    pool.put(lo_)
    for b in range(NCH):
        bs = slice(b * 128, (b + 1) * 128)
        yf = pool.f32(ytok[b])
        y3 = yf.rearrange("p (h i) -> p h i", h=8)
        st, stb = self.scal8()
        mean, ex2 = st[:, 0:8], st[:, 8:16]
        P.op("dve", lambda e, mean=mean, y3=y3: e.tensor_reduce(out=mean, in_=y3, axis=AX.X, op=ALU.add), [pool.buf(ytok[b])], [stb])
        t_ = pool.get()
        tf = pool.f32(t_)
        t3 = tf.rearrange("p (h i) -> p h i", h=8)
        self.act(tf, yf, AF.Square, [pool.buf(ytok[b])], [pool.buf(t_)])
        P.op("dve", lambda e, ex2=ex2, t3=t3: e.tensor_reduce(out=ex2, in_=t3, axis=AX.X, op=ALU.add), [pool.buf(t_)], [stb])
        self.ts("dve", mean, mean, 1.0 / 64, None, ALU.mult, None, [stb], [stb])
        self.ts("dve", ex2, ex2, 1.0 / 64, None, ALU.mult, None, [stb], [stb])
        msq = st[:, 16:24]
        self.tt("dve", msq, mean, mean, ALU.mult, [stb], [stb])
        self.tt("dve", ex2, ex2, msq, ALU.subtract, [stb], [stb])
        self.act(ex2, ex2, AF.Sqrt, [stb], [stb], bias=RWKV_LN_EPS)
        P.op("dve", lambda e, ex2=ex2: e.reciprocal(out=ex2, in_=ex2), [stb], [stb])
        for h in range(8):
            hs = slice(h * 64, (h + 1) * 64)
            self.ts("dve", tf[:, hs], yf[:, hs], mean[:, h:h + 1], ex2[:, h:h + 1], ALU.subtract, ALU.mult,
                    [pool.buf(ytok[b]), stb], [pool.buf(t_)])
        self.tt("dve", tf, tf, self.rown.h[:, 0:512], ALU.mult, [pool.buf(t_), self.rown], [pool.buf(t_)])
        self.tt("dve", tf, tf, self.rowb.h[:, 0:512], ALU.add, [pool.buf(t_), self.rowb], [pool.buf(t_)])
        va, vbuf = vtok(b)
        for h in range(8):
            hs = slice(h * 64, (h + 1) * 64)
            self.stt(tf[:, hs], va[:, hs], self.rkd.h[:, b, h:h + 1], tf[:, hs], ALU.mult, ALU.add,
                     [vbuf, self.rkd.bufs[b], pool.buf(t_)], [pool.buf(t_)])
        g_ps = self.next_ps()
        P.op("pe", [self.mm(g_ps.h[:, 0:512], pool.bf(sg_)[:, bs], self.rw_g2.h[:], True, True)], [pool.buf(sg_), self.rw_g2], [g_ps])
        yb_ = pool.get()
        yb = pool.bf(yb_)[:, 0:512]
        self.tt("dve", yb, tf, g_ps.h[:, 0:512], ALU.mult, [pool.buf(t_), g_ps], [pool.buf(yb_)])
        yt_ps = self.next_ps()
        ytb = yt_ps.h[:].bitcast(BF16)
        P.op("pe", [self.trn(ytb[:, c * 128:(c + 1) * 128], yb[:, c * 128:(c + 1) * 128], self.ident_b.h[:]) for c in range(4)],
             [pool.buf(yb_), self.ident_b], [yt_ps])
        self.cp("act", self.mixT.h[:, 12:16, bs], ytb[:, 0:512].rearrange("p (h c) -> p h c", h=4), [yt_ps],
                [self.mixT.bufs[12 + c] for c in range(4)])
        pool.put(t_, yb_)
    pool.put(sg_, *ytok)
    pool.put(*vtk)


def scal8(self):
    i = self.small_rr % 2
    self.small_rr += 1
    return self.small8.h[:, i * 32:(i + 1) * 32], self.small8.bufs[i]


Kern.rwkv = rwkv
Kern.scal8 = scal8
```

```python
import math
from contextlib import ExitStack

import numpy as np
import concourse.bass as bass
import concourse.mybir as mybir
from concourse.bass_utils import run_bass_kernel_spmd

F32 = mybir.dt.float32
BF16 = mybir.dt.bfloat16
AF = mybir.ActivationFunctionType
ALU = mybir.AluOpType
AX = mybir.AxisListType

D = 2048
NK = D // 128
W = 512
FFN = 5632
NF = FFN // 128
IN_COLS = 6928
GDN0 = 0
ML0 = 2056
RG0 = 4112
RW0 = 5136
NORM_EPS = 1e-6
RWKV_LN_EPS = 64e-5
RGLRU_C = 8.0


class Cfg:
    def __init__(self, T=4096, depth=2, TT=512, mixers=("gdn", "mlstm", "rglru", "rwkv")):
        self.T, self.depth, self.TT = T, depth, TT
        self.NB = TT // 128
        self.NT = T // TT
        self.mixers = mixers


PP_SPEC = [
    ("gdn_conv", 48), ("mlstm_conv", 32), ("rglru_conv", 16), ("rglru_conv_b", 4),
    ("rglru_b_a", 4), ("rglru_b_x", 4), ("rglru_lambda", 4), ("rwkv_mu", 14),
    ("rwkv_w0", 4), ("rwkv_a0", 4), ("rwkv_k_k", 4), ("rwkv_k_a", 4), ("rwkv_r_k", 4),
    ("gdn_a_log", 1), ("gdn_dt_bias", 1), ("mlstm_b_i", 1), ("mlstm_b_f", 1),
]
PP_OFF = {}
_o = 0
for _n, _c in PP_SPEC:
    PP_OFF[_n] = _o
    _o += _c
NPP = _o

RB_SPEC = [("attn_norm", 2048), ("ffn_norm", 2048), ("gdn_norm", 128), ("mlstm_norm", 512),
           ("rwkv_ln_w", 512), ("rwkv_ln_b", 512)]
RB_OFF = {}
_o = 0
for _n, _c in RB_SPEC:
    RB_OFF[_n] = _o
    _o += _c
NRB = _o


def _chunks(v, n):
    return np.ascontiguousarray(np.asarray(v, np.float32).reshape(n, 128).T)


def pack_params(inp, L):
    pp = np.zeros((L, 128, NPP), np.float32)
    rb = np.zeros((L, NRB), np.float32)
    for l in range(L):
        def put(name, arr):
            o = PP_OFF[name]
            pp[l, :, o:o + arr.shape[1]] = arr
        for name, nch in (("gdn_conv", 12), ("mlstm_conv", 8), ("rglru_conv", 4)):
            cw = np.asarray(inp[name][l], np.float32)
            a = cw.reshape(4, nch, 128).transpose(2, 1, 0).reshape(128, nch * 4)
            put(name, a)
        for name in ("rglru_conv_b", "rglru_b_a", "rglru_b_x", "rglru_lambda", "rwkv_w0", "rwkv_a0",
                     "rwkv_k_k", "rwkv_k_a"):
            put(name, _chunks(inp[name][l], 4))
        put("rwkv_r_k", _chunks(np.asarray(inp["rwkv_r_k"][l]).reshape(-1), 4))
        put("rwkv_mu", _chunks(inp["rwkv_mu"][l], 14))
        for name in ("gdn_a_log", "gdn_dt_bias", "mlstm_b_i", "mlstm_b_f"):
            col = np.zeros((128, 1), np.float32)
            col[0:4, 0] = np.asarray(inp[name][l], np.float32)
            put(name, col)
        for name, n in RB_SPEC:
            rb[l, RB_OFF[name]:RB_OFF[name] + n] = np.asarray(inp[name][l], np.float32).reshape(-1)
    return pp, rb


C_IDENT = 0
C_MSU = 128
C_MIU = 256
C_MSL = 384
C_SEL = 512
C_CMASK = 1024
C_ONES = 1536
C_SEL2 = 1664
NCONST = 1666


def make_consts():
    c = np.zeros((128, NCONST), np.float32)
    s = np.arange(128)
    c[:, C_IDENT:C_IDENT + 128] = np.eye(128)
    c[:, C_MSU:C_MSU + 128] = (s[:, None] < s[None, :])
    c[:, C_MIU:C_MIU + 128] = (s[:, None] <= s[None, :])
    c[:, C_MSL:C_MSL + 128] = (s[None, :] < s[:, None])
    for h in range(4):
        c[h, C_SEL + h * 128:C_SEL + (h + 1) * 128] = 1.0
    c[:, C_CMASK:C_CMASK + 512] = 1.0
    c[:, C_CMASK:C_CMASK + 512:128] = 0.0
    c[:, C_ONES:C_ONES + 128] = 1.0
    c[0:64, C_SEL2] = 1.0
    c[64:128, C_SEL2 + 1] = 1.0
    return c


class Buf:
    __slots__ = ("name", "w", "r")

    def __init__(self, name):
        self.name = name
        self.w = None
        self.r = {}


class Tile:
    def __init__(self, h, buf, bufs=None):
        self.h = h
        self.buf = buf
        self.bufs = bufs


class Prog:
    ENG = ("pe", "dve", "act", "pool", "sp")
    NDMA = 6
    SAME_ENGINE_SYNC = True

    def __init__(self, nc, stack):
        self.nc = nc
        self.stack = stack
        self.q = {e: [] for e in self.ENG}
        self.sem = {}
        self.cnt = {}
        self.waited = {e: {} for e in self.ENG}
        for e in ("pe", "dve", "act", "pool"):
            self.sem[e] = stack.enter_context(nc.semaphore("s_" + e))
            self.cnt[e] = 0
        self.dma_rr = {}
        for e in ("sp", "pool", "act"):
            self.dma_rr[e] = 0
            for j in range(self.NDMA):
                k = (e, j)
                self.sem[k] = stack.enter_context(nc.semaphore("d_%s%d" % (e, j)))
                self.cnt[k] = 0
        self.n_inst = 0
        self.n_wait = 0

    def sb(self, name, shape, dtype, nbufs=0):
        h = self.stack.enter_context(self.nc.sbuf_tensor("sb_" + name, list(shape), dtype))
        bufs = [Buf("%s#%d" % (name, i)) for i in range(nbufs)] if nbufs else None
        return Tile(h, Buf(name), bufs)

    def ps(self, name, shape, dtype=F32):
        h = self.stack.enter_context(self.nc.psum_tensor("pm_" + name, list(shape), dtype))
        return Tile(h, Buf(name))

    def _need(self, e, tok):
        if tok is None:
            return
        k, v = tok
        if k == e:
            if e == "pe" or not self.SAME_ENGINE_SYNC:
                return
        if self.waited[e].get(k, 0) >= v:
            return
        self.waited[e][k] = v
        sem = self.sem[k]
        self.q[e].append(lambda eng, sem=sem, v=v: eng.wait_ge(sem, v))
        self.n_wait += 1

    def _deps(self, e, reads, writes):
        for b in reads:
            self._need(e, b.w)
        for b in writes:
            self._need(e, b.w)
            for k, v in b.r.items():
                self._need(e, (k, v))

    def _mark(self, tok, reads, writes):
        k, v = tok
        for b in reads:
            if b.r.get(k, 0) < v:
                b.r[k] = v
        for b in writes:
            b.w = tok
            b.r = {}

    @staticmethod
    def _bufs(xs):
        out = []
        for x in xs:
            if isinstance(x, Tile):
                if x.buf is None:
                    out.extend(x.bufs)
                else:
                    out.append(x.buf)
            elif isinstance(x, Buf):
                out.append(x)
            elif x is None:
                pass
            else:
                out.extend(Prog._bufs(x))
        return out

    def op(self, e, fns, reads=(), writes=()):
        if e == "pool":
            e = "dve"
        reads = self._bufs(reads)
        writes = self._bufs(writes)
        if not isinstance(fns, (list, tuple)):
            fns = [fns]
        self._deps(e, reads, writes)
        sem = self.sem[e]
        n = len(fns)
        for i, fn in enumerate(fns):
            if i == n - 1:
                self.q[e].append(lambda eng, fn=fn, sem=sem: fn(eng).then_inc(sem, 1))
            else:
                self.q[e].append(fn)
        self.n_inst += n
        self.cnt[e] += 1
        self._mark((e, self.cnt[e]), reads, writes)

    def dma(self, e, out, in_, reads=(), writes=()):
        reads = self._bufs(reads)
        writes = self._bufs(writes)
        j = self.dma_rr[e] % self.NDMA
        self.dma_rr[e] += 1
        k = (e, j)
        if self.cnt[k] > 0:
            self._need(e, (k, self.cnt[k]))
        self._deps(e, reads, writes)
        sem = self.sem[k]
        self.q[e].append(lambda eng, out=out, in_=in_, sem=sem: eng.dma_start(out=out, in_=in_).then_inc(sem, 16))
        self.n_inst += 1
        self.cnt[k] += 16
        self._mark((k, self.cnt[k]), reads, writes)

    def finish(self):
        for k, v in self.cnt.items():
            if isinstance(k, tuple) and v > 0:
                self._need("sp", (k, v))
        for e in ("pe", "dve", "act", "pool"):
            if self.cnt[e] > 0:
                self._need("sp", (e, self.cnt[e]))

    def emit(self):
        nc = self.nc
        q = self.q
        with nc.Block() as block:
            @block.tensor
            def _(eng):
                for fn in q["pe"]:
                    fn(eng)

            @block.vector
            def _(eng):
                for fn in q["dve"]:
                    fn(eng)

            @block.scalar
            def _(eng):
                for fn in q["act"]:
                    fn(eng)

            @block.gpsimd
            def _(eng):
                for fn in q["pool"]:
                    fn(eng)

            @block.sync
            def _(eng):
                for fn in q["sp"]:
                    fn(eng)


class Pool:
    def __init__(self, P, n):
        self.t = P.sb("arena", [128, n, 512], F32, nbufs=n)
        self.free = list(range(n))
        self.n = n

    def get(self):
        assert self.free, "arena exhausted"
        return self.free.pop(0)

    def get_block(self, n):
        for i in range(self.n - n + 1):
            if all((i + j) in self.free for j in range(n)):
                for j in range(n):
                    self.free.remove(i + j)
                return i
        raise AssertionError("no contiguous arena block")

    def blk_f32(self, i, n):
        return self.t.h[:, i:i + n, :].rearrange("p n c -> p (n c)")

    def put(self, *idx):
        for i in idx:
            assert i not in self.free
            self.free.append(i)

    def f32(self, i):
        return self.t.h[:, i, :]

    def bf(self, i):
        return self.t.h[:, i, :].bitcast(BF16)

    def buf(self, i):
        return self.t.bufs[i]


GELU_C = 1.5957691216057308


class Kern:
    def __init__(self, cfg):
        self.cfg = cfg
        self.stack = ExitStack()
        nc = bass.Bass("TRN2", target_bir_lowering=False)
        self.nc = nc
        L, T = cfg.depth, cfg.T
        dt = nc.dram_tensor
        self.x_d = dt("x", [T, D], F32, kind="ExternalInput").ap()
        self.out_d = dt("out", [T, D], F32, kind="ExternalOutput").ap()
        self.w_in_d = dt("w_in", [L, D, IN_COLS], F32, kind="ExternalInput").ap()
        self.w_out_d = dt("w_out", [L, D, D], F32, kind="ExternalInput").ap()
        self.wg_d = dt("ffn_w_gate", [L, D, FFN], F32, kind="ExternalInput").ap()
        self.wu_d = dt("ffn_w_up", [L, D, FFN], F32, kind="ExternalInput").ap()
        self.wd_d = dt("ffn_w_down", [L, FFN, D], F32, kind="ExternalInput").ap()
        self.pp_d = dt("pp", [L, 128, NPP], F32, kind="ExternalInput").ap()
        self.rb_d = dt("rb", [L, NRB], F32, kind="ExternalInput").ap()
        self.fin_d = dt("final_norm", [1, D], F32, kind="ExternalInput").ap()
        self.const_d = dt("consts", [128, NCONST], F32, kind="ExternalInput").ap()
        self.rg_wa_d = dt("rglru_w_a", [L, 4, 128, 128], F32, kind="ExternalInput").ap()
        self.rg_wx_d = dt("rglru_w_x", [L, 4, 128, 128], F32, kind="ExternalInput").ap()
        self.rw_w2_d = dt("rwkv_w2", [L, 64, 512], F32, kind="ExternalInput").ap()
        self.rw_a2_d = dt("rwkv_a2", [L, 64, 512], F32, kind="ExternalInput").ap()
        self.rw_g2_d = dt("rwkv_g2", [L, 128, 512], F32, kind="ExternalInput").ap()

    def build(self):
        with self.stack:
            self._build()
        return self.nc

    def act(self, out, in_, func, reads, writes, **kw):
        self.P.op("act", lambda e: e.activation(out=out, in_=in_, func=func, **kw), reads, writes)

    def tt(self, eng, out, in0, in1, op, reads, writes):
        self.P.op(eng, lambda e: e.tensor_tensor(out=out, in0=in0, in1=in1, op=op), reads, writes)

    def ts(self, eng, out, in0, s1, s2, op0, op1, reads, writes, **kw):
        if s2 is None:
            self.P.op(eng, lambda e: e.tensor_scalar(out=out, in0=in0, scalar1=s1, scalar2=None, op0=op0, **kw),
                      reads, writes)
        else:
            self.P.op(eng, lambda e: e.tensor_scalar(out=out, in0=in0, scalar1=s1, scalar2=s2, op0=op0, op1=op1, **kw),
                      reads, writes)

    def stt(self, out, in0, scalar, in1, op0, op1, reads, writes):
        self.P.op("dve", lambda e: e.scalar_tensor_tensor(out=out, in0=in0, scalar=scalar, in1=in1, op0=op0, op1=op1),
                  reads, writes)

    def cp(self, eng, out, in_, reads, writes):
        if eng == "act":
            self.P.op("act", lambda e: e.copy(out=out, in_=in_), reads, writes)
        else:
            self.P.op(eng, lambda e: e.tensor_copy(out=out, in_=in_), reads, writes)

    @staticmethod
    def trn(out, in_, identity):
        return lambda e: e.transpose(out=out, in_=in_, identity=identity)

    @staticmethod
    def mm(out, lhsT, rhs, start, stop):
        return lambda e: e.matmul(out=out, lhsT=lhsT, rhs=rhs, start=start, stop=stop)

    def _build(self):
        cfg = self.cfg
        nc = self.nc
        P = Prog(nc, self.stack)
        self.P = P
        NB, TT, L = cfg.NB, cfg.TT, cfg.depth
        self.x = [P.sb("x%d" % b, [128, D], F32) for b in range(NB)]
        self.nT = P.sb("nT", [128, NK, TT], BF16, nbufs=NB)
        self.mixT = P.sb("mixT", [128, NK, TT], BF16, nbufs=NK)
        self.NSLOT = 3
        self.wslot = [P.sb("wslot%d" % i, [128, NK, 520], BF16) for i in range(self.NSLOT)]
        self.wrr = 0
        self.wrr_f = 0
        self.wslot_x = Tile(self.mixT.h, None, self.mixT.bufs)
        self.xs = [P.sb("xs%d" % i, [128, D], BF16) for i in range(1)]
        self.cbuf = [P.sb("cbuf%d" % i, [128, 515], F32) for i in range(2)]
        self.cb_rr = 0
        self.pp = [P.sb("pp%d" % l, [128, NPP], F32) for l in range(L)]
        self.consts = P.sb("consts", [128, NCONST], F32)
        self.ident_b = P.sb("ident_b", [128, 128], BF16)
        self.small = P.sb("small", [128, 64], F32, nbufs=64)
        self.small_rr = 0
        self.psum = [P.ps("ps%d" % i, [128, 512], F32) for i in range(8)]
        self.ps_rr = 0
        self.pool = Pool(P, 23)
        self.small8 = P.sb("small8", [128, 64], F32, nbufs=2)
        self.small4 = P.sb("small4", [128, 64], F32, nbufs=16)

        P.dma("sp", self.consts.h[:], self.const_d[:, :], writes=[self.consts])
        for l in range(L):
            P.dma("sp", self.pp[l].h[:], self.pp_d[l], writes=[self.pp[l]])
        self.cp("dve", self.ident_b.h[:], self.consts.h[:, 0:128], [self.consts], [self.ident_b])
        self.setup_mixers()

        for t in range(cfg.NT):
            self.load_x(t)
            for l in range(L):
                self.layer(l, t)
            self.final_norm_store(t)
        P.finish()
        self.sbuf_left = nc.sbuf_bytes_remaining
        P.emit()

    def next_ps(self):
        i = self.ps_rr % 8
        self.ps_rr += 1
        return self.psum[i]

    def scal(self):
        i = self.small_rr % 64
        self.small_rr += 1
        return self.small.h[:, i:i + 1], self.small.bufs[i]

    def ppc(self, l, name, j=0):
        o = PP_OFF[name] + j
        return self.pp[l].h[:, o:o + 1]

    def load_w(self, src_ap, nk=NK, ncols=512, ffn=False):
        if ffn:
            ring = self.wslot + [self.wslot_x]
            s = ring[self.wrr_f % 4]
            self.wrr_f += 1
        else:
            s = self.wslot[self.wrr % self.NSLOT]
            self.wrr += 1
        self.P.dma("pool", s.h[:, 0:nk, 0:ncols], src_ap.rearrange("(k p) c -> p k c", p=128), writes=[s])
        return s

    def load_x(self, t):
        P, cfg = self.P, self.cfg
        for b in range(cfg.NB):
            r0 = t * cfg.TT + b * 128
            P.dma("sp", self.x[b].h[:], self.x_d[r0:r0 + 128, :], writes=[self.x[b]])

    def load_rowbcast(self, dst, dcol0, src_row_ap):
        n = src_row_ap.shape[-1]
        self.P.dma("sp", dst.h[:, dcol0:dcol0 + n], src_row_ap.partition_broadcast(128), writes=[dst])

    def wrow_get(self, src_row_ap):
        i = self.pool.get_block(4)
        ap = self.pool.blk_f32(i, 4)
        bufs = [self.pool.buf(i + j) for j in range(4)]
        self.P.dma("sp", ap, src_row_ap.partition_broadcast(128), writes=bufs)
        return ap, bufs, i

    def wrow_put(self, i):
        self.pool.put(i, i + 1, i + 2, i + 3)

    def row_rstd(self, xb):
        xs = self.xs[0]
        ss, ssb = self.scal()
        rs, rsb = self.scal()
        self.act(xs.h[:], xb.h[:], AF.Square, [xb], [xs, ssb], accum_out=ss)
        self.act(rs, ss, AF.Sqrt, [ssb], [rsb], bias=NORM_EPS, scale=1.0 / D)
        self.P.op("dve", lambda e: e.reciprocal(out=rs, in_=rs), [rsb], [rsb])
        return rs, rsb, xs

    def rmsnorm_T(self, src_row_ap):
        P, cfg = self.P, self.cfg
        wr, wrb, wi = self.wrow_get(src_row_ap)
        for b in range(cfg.NB):
            xb = self.x[b]
            self.cb_rr += 1
            rs, rsb, xs = self.row_rstd(xb)
            self.stt(xs.h[:], xb.h[:], rs, wr, ALU.mult, ALU.mult, [xb, rsb, wrb], [xs])
            for g in range(4):
                ps = self.next_ps()
                psb = ps.h[:].bitcast(BF16)
                fns = []
                for j in range(4):
                    k = g * 4 + j
                    fns.append(lambda e, psb=psb, j=j, k=k, xs=xs: e.transpose(
                        out=psb[:, j * 128:(j + 1) * 128], in_=xs.h[:, k * 128:(k + 1) * 128],
                        identity=self.ident_b.h[:]))
                P.op("pe", fns, [xs, self.ident_b], [ps])
                dst = self.nT.h[:, g * 4:(g + 1) * 4, b * 128:(b + 1) * 128]
                srcv = psb[:, 0:512].rearrange("p (j c) -> p j c", j=4)
                self.cp("act" if g % 2 else "dve", dst, srcv, [ps], [self.nT.bufs[b]])
        self.wrow_put(wi)

    def inproj_fm(self, ws, wc, m):
        ps = self.next_ps()
        TT = self.cfg.TT
        fns = [self.mm(ps.h[0:m, 0:TT], ws.h[:, k, wc:wc + m], self.nT.h[:, k, :], k == 0, k == NK - 1)
               for k in range(NK)]
        self.P.op("pe", fns, [ws, self.nT.bufs], [ps])
        return ps

    def inproj_tm(self, ws, wc, n, b):
        ps = self.next_ps()
        fns = [self.mm(ps.h[:, 0:n], self.nT.h[:, k, b * 128:(b + 1) * 128], ws.h[:, k, wc:wc + n], k == 0, k == NK - 1)
               for k in range(NK)]
        self.P.op("pe", fns, [ws, self.nT.bufs[b]], [ps])
        return ps

    def conv4(self, l, ps, m, cname, cidx, halo, halo_buf, hcol, bias=None):
        TT = self.cfg.TT
        pool = self.pool
        cb = self.cbuf[self.cb_rr % 2]
        self.cb_rr += 1
        self.cp("act", cb.h[0:m, 3:3 + TT], ps.h[0:m, 0:TT], [ps], [cb])
        self.cp("dve", cb.h[0:m, 0:3], halo.h[0:m, hcol:hcol + 3], [halo_buf], [cb])
        o = pool.get()
        acc = pool.f32(o)[0:m, 0:TT]
        w = lambda j: self.ppc(l, cname, cidx * 4 + j)[0:m]
        if bias is None:
            self.ts("dve", acc, cb.h[0:m, 3:3 + TT], w(3), None, ALU.mult, None, [cb, self.pp[l]], [pool.buf(o)])
        else:
            self.ts("dve", acc, cb.h[0:m, 3:3 + TT], w(3), bias, ALU.mult, ALU.add, [cb, self.pp[l]], [pool.buf(o)])
        for j in (2, 1, 0):
            self.stt(acc, cb.h[0:m, j:j + TT], w(j), acc, ALU.mult, ALU.add, [cb, self.pp[l], pool.buf(o)], [pool.buf(o)])
        self.cp("dve", halo.h[0:m, hcol:hcol + 3], cb.h[0:m, TT:TT + 3], [cb], [halo_buf])
        return o

    def layer(self, l, t):
        P, cfg, pool = self.P, self.cfg, self.pool
        NB, TT = cfg.NB, cfg.TT
        self.rmsnorm_T(self.rb_d[l:l + 1, RB_OFF["attn_norm"]:RB_OFF["attn_norm"] + D])
        for name in ("gdn", "mlstm", "rglru", "rwkv"):
            if name in cfg.mixers:
                getattr(self, name)(l, t)
            else:
                base = {"gdn": 0, "mlstm": 4, "rglru": 8, "rwkv": 12}[name]
                if t == 0 and l == 0:
                    for c in range(4):
                        P.op("pool", lambda e, c=c, base=base: e.memset(self.mixT.h[:, base + c, :], 0.0),
                             [], [self.mixT.bufs[base + c]])
        for dg in range(4):
            ws = self.load_w(self.w_out_d[l, :, dg * 512:(dg + 1) * 512])
            for b in range(NB):
                ps = self.next_ps()
                fns = [self.mm(ps.h[:, :], self.mixT.h[:, k, b * 128:(b + 1) * 128], ws.h[:, k, 0:512], k == 0, k == NK - 1)
                       for k in range(NK)]
                P.op("pe", fns, [ws, self.mixT.bufs], [ps])
                xv = self.x[b].h[:, dg * 512:(dg + 1) * 512]
                self.tt("dve", xv, xv, ps.h[:, :], ALU.add, [ps, self.x[b]], [self.x[b]])
        self.rmsnorm_T(self.rb_d[l:l + 1, RB_OFF["ffn_norm"]:RB_OFF["ffn_norm"] + D])
        hs = [pool.get() for _ in range(NF // 2)]

        def hT(c):
            return pool.bf(hs[c // 2])[:, (c % 2) * 512:(c % 2) * 512 + TT], pool.buf(hs[c // 2])
        for fg in range(FFN // 512):
            wg = self.load_w(self.wg_d[l, :, fg * 512:(fg + 1) * 512], ffn=True)
            wu = self.load_w(self.wu_d[l, :, fg * 512:(fg + 1) * 512], ffn=True)
            for j in range(4):
                c = fg * 4 + j
                pg = self.inproj_fm(wg, j * 128, 128)
                pu = self.inproj_fm(wu, j * 128, 128)
                sg = pool.get()
                self.act(pool.f32(sg)[:, 0:TT], pg.h[:, 0:TT], AF.Silu, [pg], [pool.buf(sg)])
                h_ap, h_buf = hT(c)
                self.tt("dve", h_ap, pool.f32(sg)[:, 0:TT], pu.h[:, 0:TT], ALU.mult, [pu, pool.buf(sg)], [h_buf])
                pool.put(sg)
        fsl = [(0, 16), (16, 16), (32, 12)]
        for dg in range(4):
            pss = [self.next_ps() for _ in range(NB)]
            for si, (f0, nf) in enumerate(fsl):
                ws = self.load_w(self.wd_d[l, f0 * 128:(f0 + nf) * 128, dg * 512:(dg + 1) * 512], nk=nf, ffn=True)
                for b in range(NB):
                    fns = []
                    rd = [ws]
                    for kk in range(nf):
                        h_ap, h_buf = hT(f0 + kk)
                        rd.append(h_buf)
                        fns.append(self.mm(pss[b].h[:, :], h_ap[:, b * 128:(b + 1) * 128], ws.h[:, kk, 0:512],
                                           si == 0 and kk == 0, si == 2 and kk == nf - 1))
                    P.op("pe", fns, rd, [pss[b]])
            for b in range(NB):
                xv = self.x[b].h[:, dg * 512:(dg + 1) * 512]
                self.tt("dve", xv, xv, pss[b].h[:, :], ALU.add, [pss[b], self.x[b]], [self.x[b]])
        pool.put(*hs)

    def final_norm_store(self, t):
        P, cfg = self.P, self.cfg
        wr, wrb, wi = self.wrow_get(self.fin_d[0:1, :])
        for b in range(cfg.NB):
            xb = self.x[b]
            self.cb_rr += 1
            rs, rsb, xs = self.row_rstd(xb)
            self.stt(xb.h[:], xb.h[:], rs, wr, ALU.mult, ALU.mult, [xb, rsb, wrb], [xb])
            r0 = t * cfg.TT + b * 128
            P.dma("sp", self.out_d[r0:r0 + 128, :], xb.h[:], reads=[xb])
        self.wrow_put(wi)

    def setup_mixers(self):
        P, cfg = self.P, self.cfg
        L = cfg.depth
        self.tok = P.sb("tok", [128, 64], F32)
        self.rown = P.sb("rown", [128, 512], F32)
        for name in ("gdn", "mlstm", "rwkv"):
            if name in cfg.mixers:
                globals()["setup_" + name](self)
        self.halo = [P.sb("halo%d" % l, [128, 72], F32, nbufs=24) for l in range(L)]
        self.rg_h = [P.sb("rg_h%d" % l, [128, 4], F32, nbufs=4) for l in range(L)]
        self.rg_w = [P.sb("rg_w%d" % l, [128, 8, 128], BF16) for l in range(L)]
        self.rg_cp = [P.sb("rg_cp%d" % l, [128, 8], F32) for l in range(L)]
        for l in range(L):
            P.op("pool", lambda e, l=l: e.memset(self.halo[l].h[:], 0.0), [], self.halo[l].bufs)
            P.op("pool", lambda e, l=l: e.memset(self.rg_h[l].h[:], 0.0), [], self.rg_h[l].bufs)
            P.dma("pool", self.rg_w[l].h[:, 0:4, :], self.rg_wa_d[l].rearrange("n i j -> i n j"), writes=[self.rg_w[l]])
            P.dma("pool", self.rg_w[l].h[:, 4:8, :], self.rg_wx_d[l].rearrange("n i j -> i n j"), writes=[self.rg_w[l]])
            lam = self.pp[l].h[:, PP_OFF["rglru_lambda"]:PP_OFF["rglru_lambda"] + 4]
            cpt = self.rg_cp[l]
            self.act(cpt.h[:, 0:4], lam, AF.Exp, [self.pp[l]], [cpt], scale=-1.0)
            self.act(cpt.h[:, 0:4], cpt.h[:, 0:4], AF.Ln, [cpt], [cpt], bias=1.0)
            self.ts("dve", cpt.h[:, 4:8], cpt.h[:, 0:4], -2.0 * RGLRU_C, None, ALU.mult, None, [cpt], [cpt])
            self.ts("dve", cpt.h[:, 0:4], cpt.h[:, 0:4], -RGLRU_C, None, ALU.mult, None, [cpt], [cpt])

    def rglru(self, l, t):
        P, cfg, pool = self.P, self.cfg, self.pool
        TT = cfg.TT
        wx = self.load_w(self.w_in_d[l, :, RG0:RG0 + 512])
        wgt = self.load_w(self.w_in_d[l, :, RG0 + 512:RG0 + 1024])
        pl = self.pp[l]
        for c in range(4):
            ps = self.inproj_fm(wx, c * 128, 128)
            hb = 20 + c
            xo = self.conv4(l, ps, 128, "rglru_conv", c, self.halo[l], self.halo[l].bufs[hb], hb * 3,
                            bias=self.ppc(l, "rglru_conv_b", c))
            xf = pool.f32(xo)[:, 0:TT]
            xbf = pool.get()
            xb16 = pool.bf(xbf)[:, 0:TT]
            self.cp("act", xb16, xf, [pool.buf(xo)], [pool.buf(xbf)])
            pr = self.next_ps()
            P.op("pe", [self.mm(pr.h[:, 0:TT], self.rg_w[l].h[:, c, :], xb16, True, True)], [self.rg_w[l], pool.buf(xbf)], [pr])
            pi = self.next_ps()
            P.op("pe", [self.mm(pi.h[:, 0:TT], self.rg_w[l].h[:, 4 + c, :], xb16, True, True)], [self.rg_w[l], pool.buf(xbf)], [pi])
            r_ = pool.get()
            i_ = pool.get()
            rf, if_ = pool.f32(r_)[:, 0:TT], pool.f32(i_)[:, 0:TT]
            self.act(rf, pr.h[:, 0:TT], AF.Sigmoid, [pr, pl], [pool.buf(r_)], bias=self.ppc(l, "rglru_b_a", c))
            self.act(if_, pi.h[:, 0:TT], AF.Sigmoid, [pi, pl], [pool.buf(i_)], bias=self.ppc(l, "rglru_b_x", c))
            a_ = xbf
            af = pool.f32(a_)[:, 0:TT]
            self.act(af, rf, AF.Exp, [pool.buf(r_), self.rg_cp[l], pool.buf(a_)], [pool.buf(a_)], scale=self.rg_cp[l].h[:, c:c + 1])
            self.act(rf, rf, AF.Exp, [pool.buf(r_), self.rg_cp[l]], [pool.buf(r_)], scale=self.rg_cp[l].h[:, 4 + c:5 + c])
            self.ts("dve", rf, rf, -1.0, 1.0, ALU.mult, ALU.add, [pool.buf(r_)], [pool.buf(r_)])
            self.act(rf, rf, AF.Sqrt, [pool.buf(r_)], [pool.buf(r_)])
            self.tt("dve", if_, if_, xf, ALU.mult, [pool.buf(i_), pool.buf(xo)], [pool.buf(i_)])
            self.tt("dve", if_, if_, rf, ALU.mult, [pool.buf(i_), pool.buf(r_)], [pool.buf(i_)])
            hst = self.rg_h[l].h[:, c:c + 1]
            hstb = self.rg_h[l].bufs[c]
            P.op("dve", lambda e, xf=xf, af=af, if_=if_, hst=hst: e.tensor_tensor_scan(
                out=xf, data0=af, data1=if_, initial=hst, op0=ALU.mult, op1=ALU.add),
                [pool.buf(a_), pool.buf(i_), hstb], [pool.buf(xo)])
            self.cp("dve", hst, xf[:, TT - 1:TT], [pool.buf(xo)], [hstb])
            pg = self.inproj_fm(wgt, c * 128, 128)
            g = pg.h[:, 0:TT]
            self.act(rf, g, AF.Square, [pg], [pool.buf(r_)])
            self.ts("dve", rf, rf, 0.044715, 1.0, ALU.mult, ALU.add, [pool.buf(r_)], [pool.buf(r_)])
            self.tt("dve", rf, rf, g, ALU.mult, [pool.buf(r_), pg], [pool.buf(r_)])
            self.act(rf, rf, AF.Sigmoid, [pool.buf(r_)], [pool.buf(r_)], scale=GELU_C)
            self.tt("dve", rf, rf, g, ALU.mult, [pool.buf(r_), pg], [pool.buf(r_)])
            self.tt("dve", self.mixT.h[:, 8 + c, :], rf, xf, ALU.mult, [pool.buf(r_), pool.buf(xo)], [self.mixT.bufs[8 + c]])
            pool.put(xo, a_, r_, i_)

    def gdn(self, l, t):
        raise NotImplementedError

    def rwkv(self, l, t):
        raise NotImplementedError


_W_NAMES = ("w_in", "w_out", "ffn_w_gate", "ffn_w_up", "ffn_w_down", "rglru_w_a", "rglru_w_x",
            "rwkv_w2", "rwkv_a2", "rwkv_g2")


def make_in_maps(inputs, cfg, n_cores):
    L = cfg.depth
    pp, rb = pack_params(inputs, L)
    consts = make_consts()
    shared = {k: np.ascontiguousarray(np.asarray(inputs[k], np.float32)) for k in _W_NAMES}
    shared["pp"] = pp
    shared["rb"] = rb
    shared["final_norm"] = np.ascontiguousarray(np.asarray(inputs["final_norm"], np.float32).reshape(1, D))
    shared["consts"] = consts
    x = np.asarray(inputs["x"], np.float32)
    B = x.shape[0]
    maps = []
    for c in range(n_cores):
        m = dict(shared)
        m["x"] = np.ascontiguousarray(x[c % B])
        maps.append(m)
    return maps


def kernel(**inputs):
    x = np.asarray(inputs["x"])
    B, T, _ = x.shape
    L = np.asarray(inputs["w_in"]).shape[0]
    cfg = Cfg(T=T, depth=L)
    nc = Kern(cfg).build()
    n_cores = B
    in_maps = make_in_maps(inputs, cfg, n_cores)
    res = run_bass_kernel_spmd(nc, in_maps, core_ids=list(range(n_cores)))
    out = np.stack([np.asarray(res.results[c]["out"], np.float32) for c in range(B)], axis=0)
    return out

def _cs(ap, j, n=128):
    return ap[:, j * n:(j + 1) * n]


def setup_mlstm(self):
    P, cfg = self.P, self.cfg
    L = cfg.depth
    self.ml_C = [P.sb("ml_C%d" % l, [128, 4, 129], F32, nbufs=4) for l in range(L)]
    self.ml_Cb = [P.sb("ml_Cb%d" % l, [128, 4, 130], BF16, nbufs=4) for l in range(L)]
    self.ml_sc = [P.sb("ml_sc%d" % l, [128, 2], F32) for l in range(L)]
    self.vp = [P.sb("vp%d" % c, [128, 4, 130], BF16) for c in range(4)]
    for c in range(4):
        P.op("pool", lambda e, c=c: e.memset(self.vp[c].h[:], 1.0), [], [self.vp[c]])
    for l in range(L):
        P.op("pool", lambda e, l=l: e.memset(self.ml_C[l].h[:], 0.0), [], self.ml_C[l].bufs)
        P.op("pool", lambda e, l=l: e.memset(self.ml_Cb[l].h[:], 0.0), [], self.ml_Cb[l].bufs)
        self.ts("dve", self.ml_sc[l].h[:, 0:1], self.ppc(l, "mlstm_b_f"), -1.0, None, ALU.mult, None,
                [self.pp[l]], [self.ml_sc[l]])


def gate_rows_to_bcast(self, rows_ap, rows_buf, h):
    ps = self.next_ps()
    TT = self.cfg.TT
    sel = self.consts.h[0:4, C_SEL + h * 128:C_SEL + (h + 1) * 128]
    self.P.op("pe", [self.mm(ps.h[:, 0:TT], sel, rows_ap, True, True)], [self.consts, rows_buf], [ps])
    return ps


def rows_to_tok(self, rows_list, col0):
    P = self.P
    NCH = self.cfg.TT // 128
    ps = self.next_ps()
    fns = []
    rd = [self.consts]
    for q, (ap, buf) in enumerate(rows_list):
        rd.append(buf)
        for c in range(NCH):
            o = (q * NCH + c) * 4
            fns.append(self.trn(ps.h[:, o:o + 4], ap[0:4, c * 128:(c + 1) * 128], self.consts.h[0:4, C_IDENT:C_IDENT + 4]))
    P.op("pe", fns, rd, [ps])
    n = len(rows_list) * NCH * 4
    self.cp("dve", self.tok.h[:, col0:col0 + n], ps.h[:, 0:n], [ps], [self.tok])


def mlstm(self, l, t):
    P, cfg, pool = self.P, self.cfg, self.pool
    TT = cfg.TT
    NCH = TT // 128
    pl = self.pp[l]
    cst, cbf = self.ml_C[l], self.ml_Cb[l]
    self.load_rowbcast(self.rown, 0, self.rb_d[l:l + 1, RB_OFF["mlstm_norm"]:RB_OFF["mlstm_norm"] + 512])
    wo = self.load_w(self.w_in_d[l, :, ML0 + 1536:ML0 + 2056], ncols=520)
    ps_i = self.inproj_fm(wo, 512, 4)
    ps_f = self.inproj_fm(wo, 516, 4)
    s_i, s_b, s_c = pool.get(), pool.get(), pool.get()
    r_i, r_b, r_c = pool.f32(s_i)[0:4, 0:TT], pool.f32(s_b)[0:4, 0:TT], pool.f32(s_c)[0:4, 0:TT]
    self.act(r_i, ps_i.h[0:4, 0:TT], AF.Identity, [ps_i, pl], [pool.buf(s_i)], bias=self.ppc(l, "mlstm_b_i")[0:4])
    self.act(r_b, ps_f.h[0:4, 0:TT], AF.Exp, [ps_f, self.ml_sc[l]], [pool.buf(s_b)], bias=self.ml_sc[l].h[0:4, 0:1], scale=-1.0)
    self.act(r_b, r_b, AF.Ln, [pool.buf(s_b)], [pool.buf(s_b)], bias=1.0)
    cm = self.consts.h[0:4, C_CMASK:C_CMASK + TT]
    P.op("dve", lambda e: e.tensor_tensor_scan(out=r_c, data0=cm, data1=r_b, initial=0.0, op0=ALU.mult, op1=ALU.add),
         [self.consts, pool.buf(s_b)], [pool.buf(s_c)])
    self.ts("dve", r_b, r_c, -1.0, None, ALU.mult, None, [pool.buf(s_c)], [pool.buf(s_b)])
    self.tt("dve", r_c, r_i, r_b, ALU.subtract, [pool.buf(s_i), pool.buf(s_b)], [pool.buf(s_c)])
    rows_to_tok(self, [(r_c, pool.buf(s_c))], 0)
    bbc = []
    for h in range(4):
        ps = gate_rows_to_bcast(self, r_b, pool.buf(s_b), h)
        o = pool.get()
        self.cp("act", pool.f32(o)[:, 0:TT], ps.h[:, 0:TT], [ps], [pool.buf(o)])
        bbc.append(o)
    pool.put(s_i, s_b, s_c)
    wv = self.load_w(self.w_in_d[l, :, ML0 + 1024:ML0 + 1536])
    osg = [pool.get() for _ in range(NCH // 2)]

    def osig(c):
        return pool.bf(osg[c // 2])[:, (c % 2) * 512:(c % 2) * 512 + 512], pool.buf(osg[c // 2])
    for c in range(NCH):
        ps_v = self.inproj_tm(wv, 0, 512, c)
        self.cp("act", self.vp[c].h[:, :, 0:128], ps_v.h[:, 0:512].rearrange("p (h e) -> p h e", h=4), [ps_v], [self.vp[c]])
        ps_o = self.inproj_tm(wo, 0, 512, c)
        oa, ob = osig(c)
        self.act(oa, ps_o.h[:, 0:512], AF.Sigmoid, [ps_o], [ob])
    wq = self.load_w(self.w_in_d[l, :, ML0:ML0 + 512])
    wk = self.load_w(self.w_in_d[l, :, ML0 + 512:ML0 + 1024])
    qk = [pool.get() for _ in range(4)]
    qd = [pool.get() for _ in range(2)]
    for h in range(4):
        ps = self.inproj_fm(wq, h * 128, 128)
        o = self.conv4(l, ps, 128, "mlstm_conv", h, self.halo[l], self.halo[l].bufs[12 + h], (12 + h) * 3)
        qf = pool.f32(o)[:, 0:TT]
        self.act(qf, qf, AF.Silu, [pool.buf(o)], [pool.buf(o)])
        self.cp("act", pool.bf(qk[h])[:, 0:TT], qf, [pool.buf(o)], [pool.buf(qk[h])])
        e_ = pool.get()
        self.act(pool.f32(e_)[:, 0:TT], pool.f32(bbc[h])[:, 0:TT], AF.Exp, [pool.buf(bbc[h])], [pool.buf(e_)])
        self.tt("dve", pool.bf(qd[h // 2])[:, (h % 2) * 512:(h % 2) * 512 + TT], qf, pool.f32(e_)[:, 0:TT], ALU.mult,
                [pool.buf(o), pool.buf(e_)], [pool.buf(qd[h // 2])])
        pool.put(o, e_)
        ps = self.inproj_fm(wk, h * 128, 128)
        o = self.conv4(l, ps, 128, "mlstm_conv", 4 + h, self.halo[l], self.halo[l].bufs[16 + h], (16 + h) * 3)
        kf = pool.f32(o)[:, 0:TT]
        self.act(kf, kf, AF.Silu, [pool.buf(o)], [pool.buf(o)])
        self.ts("dve", pool.bf(qk[h])[:, 512:512 + TT], kf, 128.0 ** -0.5, None, ALU.mult, None, [pool.buf(o)], [pool.buf(qk[h])])
        pool.put(o)
    for c in range(NCH):
        cs = slice(c * 128, (c + 1) * 128)
        st_ps = self.next_ps()
        fns = [self.mm(st_ps.h[:, h * 128:(h + 1) * 128], pool.bf(qk[h])[:, 512 + c * 128:512 + (c + 1) * 128],
                       pool.bf(qk[h])[:, cs], True, True) for h in range(4)]
        P.op("pe", fns, [pool.buf(qk[h]) for h in range(4)], [st_ps])
        dt_ = pool.get()
        dtf = pool.f32(dt_)
        for h in range(4):
            self.act(dtf[:, h * 128:(h + 1) * 128], pool.f32(bbc[h])[:, cs], AF.Exp, [pool.buf(bbc[h]), self.tok],
                     [pool.buf(dt_)], bias=self.tok.h[:, c * 4 + h:c * 4 + h + 1])
        for h in range(4):
            self.tt("dve", dtf[:, h * 128:(h + 1) * 128], dtf[:, h * 128:(h + 1) * 128],
                    self.consts.h[:, C_MIU:C_MIU + 128], ALU.mult, [pool.buf(dt_), self.consts], [pool.buf(dt_)])
        st_ = pool.get()
        stb = pool.bf(st_)[:, 0:512]
        self.tt("dve", stb, dtf, st_ps.h[:, 0:512], ALU.mult, [pool.buf(dt_), st_ps], [pool.buf(st_)])
        kw, kwb = self.scal4()
        for h in range(4):
            self.act(kw[:, h:h + 1], self.tok.h[:, c * 4 + h:c * 4 + h + 1], AF.Exp, [self.tok, pool.buf(bbc[h])], [kwb],
                     bias=pool.f32(bbc[h])[:, c * 128 + 127:c * 128 + 128])
        eg, egb = self.scal4()
        for h in range(4):
            self.act(eg[:, h:h + 1], pool.f32(bbc[h])[:, c * 128 + 127:c * 128 + 128], AF.Exp, [pool.buf(bbc[h])], [egb])
        kt_ps = self.next_ps()
        ktb = kt_ps.h[:].bitcast(BF16)
        fns = [self.trn(ktb[:, h * 128:(h + 1) * 128], pool.bf(qk[h])[:, 512 + c * 128:512 + (c + 1) * 128], self.ident_b.h[:])
               for h in range(4)]
        P.op("pe", fns, [pool.buf(qk[h]) for h in range(4)] + [self.ident_b], [kt_ps])
        kw_ = pool.get()
        kwt = pool.bf(kw_)[:, 0:512]
        for h in range(4):
            self.ts("dve", kwt[:, h * 128:(h + 1) * 128], ktb[:, h * 128:(h + 1) * 128], kw[:, h:h + 1], None, ALU.mult, None,
                    [kt_ps, kwb], [pool.buf(kw_)])
        y_ = pool.get()
        yb = pool.bf(y_)[:, 0:512]
        oa, ob = osig(c)
        for h in range(4):
            pn = self.next_ps()
            P.op("pe", [self.mm(pn.h[:, 0:129], pool.bf(qd[h // 2])[:, (h % 2) * 512 + c * 128:(h % 2) * 512 + (c + 1) * 128],
                                cbf.h[:, h, 0:129], True, False),
                        self.mm(pn.h[:, 0:129], stb[:, h * 128:(h + 1) * 128], self.vp[c].h[:, h, 0:129], False, True)],
                 [pool.buf(qd[h // 2]), cbf.bufs[h], pool.buf(st_), self.vp[c]], [pn])
            dn, dnb = self.scal()
            self.act(dn, pn.h[:, 128:129], AF.Abs, [pn], [dnb])
            self.ts("dve", dn, dn, 1.0, None, ALU.max, None, [dnb], [dnb])
            P.op("dve", lambda e, dn=dn: e.reciprocal(out=dn, in_=dn), [dnb], [dnb])
            hh_ = pool.get()
            hf = pool.f32(hh_)[:, 0:128]
            self.stt(hf, pn.h[:, 0:128], dn, oa[:, h * 128:(h + 1) * 128], ALU.mult, ALU.mult, [pn, dnb, ob], [pool.buf(hh_)])
            ss, ssb = self.scal()
            self.act(pool.f32(hh_)[:, 128:256], hf, AF.Square, [pool.buf(hh_)], [pool.buf(hh_), ssb], accum_out=ss)
            self.act(ss, ss, AF.Sqrt, [ssb], [ssb], bias=NORM_EPS, scale=1.0 / 128)
            P.op("dve", lambda e, ss=ss: e.reciprocal(out=ss, in_=ss), [ssb], [ssb])
            self.stt(yb[:, h * 128:(h + 1) * 128], hf, ss, self.rown.h[:, h * 128:(h + 1) * 128], ALU.mult, ALU.mult,
                     [pool.buf(hh_), ssb, self.rown], [pool.buf(y_)])
            pool.put(hh_)
            pu = self.next_ps()
            P.op("pe", [self.mm(pu.h[:, 0:129], kwt[:, h * 128:(h + 1) * 128], self.vp[c].h[:, h, 0:129], True, True)],
                 [pool.buf(kw_), self.vp[c]], [pu])
            self.stt(cst.h[:, h, :], cst.h[:, h, :], eg[:, h:h + 1], pu.h[:, 0:129], ALU.mult, ALU.add,
                     [cst.bufs[h], egb, pu], [cst.bufs[h]])
            self.cp("act", cbf.h[:, h, 0:129], cst.h[:, h, :], [cst.bufs[h]], [cbf.bufs[h]])
        yt_ps = self.next_ps()
        ytb = yt_ps.h[:].bitcast(BF16)
        fns = [self.trn(ytb[:, h * 128:(h + 1) * 128], yb[:, h * 128:(h + 1) * 128], self.ident_b.h[:]) for h in range(4)]
        P.op("pe", fns, [pool.buf(y_), self.ident_b], [yt_ps])
        self.cp("act", self.mixT.h[:, 4:8, cs], ytb[:, 0:512].rearrange("p (h c) -> p h c", h=4), [yt_ps],
                [self.mixT.bufs[4 + h] for h in range(4)])
        pool.put(dt_, st_, kw_, y_)
    pool.put(*bbc)
    pool.put(*osg)
    pool.put(*qk)
    pool.put(*qd)


def scal4(self):
    i = self.small_rr % 16
    self.small_rr += 1
    return self.small4.h[:, i * 4:(i + 1) * 4], self.small4.bufs[i]


Kern.mlstm = mlstm
Kern.scal4 = scal4

def setup_gdn(self):
    P, cfg = self.P, self.cfg
    L = cfg.depth
    self.gd_S = [P.sb("gd_S%d" % l, [128, 4, 128], F32, nbufs=4) for l in range(L)]
    self.gd_Sb = [P.sb("gd_Sb%d" % l, [128, 4, 128], BF16, nbufs=4) for l in range(L)]
    self.gd_sc = [P.sb("gd_sc%d" % l, [128, 2], F32) for l in range(L)]
    self.ones_b = P.sb("ones_b", [128, 128], BF16)
    self.cp("dve", self.ones_b.h[:], self.consts.h[:, C_ONES:C_ONES + 128], [self.consts], [self.ones_b])
    for l in range(L):
        P.op("pool", lambda e, l=l: e.memset(self.gd_S[l].h[:], 0.0), [], self.gd_S[l].bufs)
        P.op("pool", lambda e, l=l: e.memset(self.gd_Sb[l].h[:], 0.0), [], self.gd_Sb[l].bufs)
        self.act(self.gd_sc[l].h[:, 0:1], self.ppc(l, "gdn_a_log"), AF.Exp, [self.pp[l]], [self.gd_sc[l]])
        self.ts("dve", self.gd_sc[l].h[:, 0:1], self.gd_sc[l].h[:, 0:1], -1.0, None, ALU.mult, None,
                [self.gd_sc[l]], [self.gd_sc[l]])


def l2norm_fm(self, xo, scale):
    P, pool = self.P, self.pool
    TT = self.cfg.TT
    xf = pool.f32(xo)[:, 0:TT]
    sq = pool.get()
    self.act(pool.bf(sq)[:, 0:TT], xf, AF.Square, [pool.buf(xo)], [pool.buf(sq)])
    ps = self.next_ps()
    P.op("pe", [self.mm(ps.h[:, 0:TT], self.ones_b.h[:], pool.bf(sq)[:, 0:TT], True, True)], [self.ones_b, pool.buf(sq)], [ps])
    rn = pool.f32(sq)[:, 0:TT]
    self.act(rn, ps.h[:, 0:TT], AF.Sqrt, [ps, pool.buf(sq)], [pool.buf(sq)], bias=1e-6)
    P.op("dve", lambda e: e.reciprocal(out=rn, in_=rn), [pool.buf(sq)], [pool.buf(sq)])
    self.stt(xf, xf, float(scale), rn, ALU.mult, ALU.mult, [pool.buf(xo), pool.buf(sq)], [pool.buf(xo)])
    pool.put(sq)


def tri_inverse_T(self, a_, u_, n=4):
    P, pool = self.P, self.pool
    W_ = n * 128
    ident = self.consts.h[:, C_IDENT:C_IDENT + 128]
    r_ = pool.get()
    rf = pool.f32(r_)
    for h in range(n):
        self.tt("dve", rf[:, h * 128:(h + 1) * 128], ident, pool.f32(u_)[:, h * 128:(h + 1) * 128], ALU.subtract,
                [self.consts, pool.buf(u_)], [pool.buf(r_)])
    p_, q_ = u_, a_
    for k in range(1, 7):
        last = k == 6
        q2 = pool.get()
        psq = self.next_ps()
        P.op("pe", [self.mm(psq.h[:, h * 128:(h + 1) * 128], pool.f32(p_)[:, h * 128:(h + 1) * 128],
                            pool.f32(q_)[:, h * 128:(h + 1) * 128], True, True) for h in range(n)],
             [pool.buf(p_), pool.buf(q_)], [psq])
        self.cp("act", pool.f32(q2)[:, 0:W_], psq.h[:, 0:W_], [psq], [pool.buf(q2)])
        if not last:
            p2 = pool.get()
            psp = self.next_ps()
            P.op("pe", [self.mm(psp.h[:, h * 128:(h + 1) * 128], pool.f32(q_)[:, h * 128:(h + 1) * 128],
                                pool.f32(p_)[:, h * 128:(h + 1) * 128], True, True) for h in range(n)],
                 [pool.buf(p_), pool.buf(q_)], [psp])
            self.cp("dve", pool.f32(p2)[:, 0:W_], psp.h[:, 0:W_], [psp], [pool.buf(p2)])
        psr = self.next_ps()
        P.op("pe", [self.mm(psr.h[:, h * 128:(h + 1) * 128], pool.f32(q2)[:, h * 128:(h + 1) * 128],
                            rf[:, h * 128:(h + 1) * 128], True, True) for h in range(n)],
             [pool.buf(q2), pool.buf(r_)], [psr])
        self.tt("dve", rf[:, 0:W_], rf[:, 0:W_], psr.h[:, 0:W_], ALU.add, [pool.buf(r_), psr], [pool.buf(r_)])
        pool.put(p_, q_)
        if not last:
            p_, q_ = p2, q2
        else:
            pool.put(q2)
    return r_


def gdn(self, l, t):
    P, cfg, pool = self.P, self.cfg, self.pool
    TT = cfg.TT
    NCH = TT // 128
    pl = self.pp[l]
    S, Sb = self.gd_S[l], self.gd_Sb[l]
    for h in range(4):
        self.load_rowbcast(self.rown, h * 128, self.rb_d[l:l + 1, RB_OFF["gdn_norm"]:RB_OFF["gdn_norm"] + 128])
    wz = self.load_w(self.w_in_d[l, :, GDN0 + 1536:GDN0 + 2056], ncols=520)
    ps_a = self.inproj_fm(wz, 512, 4)
    ps_b = self.inproj_fm(wz, 516, 4)
    sl = [pool.get() for _ in range(5)]
    r_beta, r_g, r_ng, r_bg, r_kd = [pool.f32(s)[0:4, 0:TT] for s in sl]
    b_beta, b_g, b_ng, b_bg, b_kd = [pool.buf(s) for s in sl]
    self.act(r_beta, ps_b.h[0:4, 0:TT], AF.Sigmoid, [ps_b], [b_beta])
    self.act(r_ng, ps_a.h[0:4, 0:TT], AF.Exp, [ps_a, pl], [b_ng], bias=self.ppc(l, "gdn_dt_bias")[0:4])
    self.act(r_ng, r_ng, AF.Ln, [b_ng], [b_ng], bias=1.0)
    self.ts("dve", r_ng, r_ng, self.gd_sc[l].h[0:4, 0:1], None, ALU.mult, None, [b_ng, self.gd_sc[l]], [b_ng])
    cm = self.consts.h[0:4, C_CMASK:C_CMASK + TT]
    P.op("dve", lambda e: e.tensor_tensor_scan(out=r_g, data0=cm, data1=r_ng, initial=0.0, op0=ALU.mult, op1=ALU.add),
         [self.consts, b_ng], [b_g])
    self.ts("dve", r_ng, r_g, -1.0, None, ALU.mult, None, [b_g], [b_ng])
    self.act(r_bg, r_g, AF.Exp, [b_g], [b_bg])
    self.tt("dve", r_bg, r_bg, r_beta, ALU.mult, [b_bg, b_beta], [b_bg])
    for c in range(NCH):
        self.ts("dve", r_kd[:, c * 128:(c + 1) * 128], r_ng[:, c * 128:(c + 1) * 128], r_g[:, c * 128 + 127:c * 128 + 128],
                None, ALU.add, None, [b_ng, b_g], [b_kd])
    self.act(r_kd, r_kd, AF.Exp, [b_kd], [b_kd])
    rows_to_tok(self, [(r_ng, b_ng), (r_beta, b_beta), (r_bg, b_bg), (r_kd, b_kd)], 0)

    def tk(q, c, h):
        o = (q * NCH + c) * 4 + h
        return self.tok.h[:, o:o + 1]
    gbc = []
    for h in range(4):
        ps = gate_rows_to_bcast(self, r_g, b_g, h)
        o = pool.get()
        self.cp("act", pool.f32(o)[:, 0:TT], ps.h[:, 0:TT], [ps], [pool.buf(o)])
        gbc.append(o)
    pool.put(*sl)
    gzs = [pool.get() for _ in range(NCH // 2)]

    def gz(c):
        return pool.bf(gzs[c // 2])[:, (c % 2) * 512:(c % 2) * 512 + 512], pool.buf(gzs[c // 2])
    for c in range(NCH):
        ps_z = self.inproj_tm(wz, 0, 512, c)
        zt = pool.get()
        self.act(pool.f32(zt), ps_z.h[:, 0:512], AF.Silu, [ps_z], [pool.buf(zt)])
        ga, gb = gz(c)
        self.tt("dve", ga, pool.f32(zt), self.rown.h[:, 0:512], ALU.mult, [pool.buf(zt), self.rown], [gb])
        pool.put(zt)
    wq = self.load_w(self.w_in_d[l, :, GDN0:GDN0 + 512])
    wk = self.load_w(self.w_in_d[l, :, GDN0 + 512:GDN0 + 1024])
    wv = self.load_w(self.w_in_d[l, :, GDN0 + 1024:GDN0 + 1536])
    qk = [pool.get() for _ in range(4)]
    vq = [pool.get() for _ in range(4)]
    for h in range(4):
        ps = self.inproj_fm(wq, h * 128, 128)
        o = self.conv4(l, ps, 128, "gdn_conv", h, self.halo[l], self.halo[l].bufs[h], h * 3)
        qf = pool.f32(o)[:, 0:TT]
        self.act(qf, qf, AF.Silu, [pool.buf(o)], [pool.buf(o)])
        l2norm_fm(self, o, 128.0 ** -0.5)
        self.cp("act", pool.bf(qk[h])[:, 0:TT], qf, [pool.buf(o)], [pool.buf(qk[h])])
        e_ = pool.get()
        self.act(pool.f32(e_)[:, 0:TT], pool.f32(gbc[h])[:, 0:TT], AF.Exp, [pool.buf(gbc[h])], [pool.buf(e_)])
        self.tt("dve", pool.bf(vq[h])[:, 512:512 + TT], qf, pool.f32(e_)[:, 0:TT], ALU.mult,
                [pool.buf(o), pool.buf(e_)], [pool.buf(vq[h])])
        pool.put(o, e_)
        ps = self.inproj_fm(wk, h * 128, 128)
        o = self.conv4(l, ps, 128, "gdn_conv", 4 + h, self.halo[l], self.halo[l].bufs[4 + h], (4 + h) * 3)
        kf = pool.f32(o)[:, 0:TT]
        self.act(kf, kf, AF.Silu, [pool.buf(o)], [pool.buf(o)])
        l2norm_fm(self, o, 1.0)
        self.cp("act", pool.bf(qk[h])[:, 512:512 + TT], kf, [pool.buf(o)], [pool.buf(qk[h])])
        pool.put(o)
        ps = self.inproj_fm(wv, h * 128, 128)
        o = self.conv4(l, ps, 128, "gdn_conv", 8 + h, self.halo[l], self.halo[l].bufs[8 + h], (8 + h) * 3)
        vf = pool.f32(o)[:, 0:TT]
        self.act(pool.bf(vq[h])[:, 0:TT], vf, AF.Silu, [pool.buf(o)], [pool.buf(vq[h])])
        pool.put(o)
    ident = self.consts.h[:, C_IDENT:C_IDENT + 128]
    for c in range(NCH):
        cs = slice(c * 128, (c + 1) * 128)
        ks = slice(512 + c * 128, 512 + (c + 1) * 128)
        kk_ps = self.next_ps()
        P.op("pe", [self.mm(kk_ps.h[:, h * 128:(h + 1) * 128], pool.bf(qk[h])[:, ks], pool.bf(qk[h])[:, ks], True, True)
                    for h in range(4)], [pool.buf(qk[h]) for h in range(4)], [kk_ps])
        kq_ps = self.next_ps()
        P.op("pe", [self.mm(kq_ps.h[:, h * 128:(h + 1) * 128], pool.bf(qk[h])[:, ks], pool.bf(qk[h])[:, cs], True, True)
                    for h in range(4)], [pool.buf(qk[h]) for h in range(4)], [kq_ps])
        a_ = pool.get()
        af = pool.f32(a_)
        d_ = pool.get()
        df = pool.f32(d_)
        for h in range(4):
            hs = slice(h * 128, (h + 1) * 128)
            self.ts("dve", af[:, hs], pool.f32(gbc[h])[:, cs], tk(0, c, h), 0.0, ALU.add, ALU.max,
                    [pool.buf(gbc[h]), self.tok], [pool.buf(a_)])
        for h in range(4):
            hs = slice(h * 128, (h + 1) * 128)
            self.ts("dve", df[:, hs], pool.f32(gbc[h])[:, cs], tk(0, c, h), 0.0, ALU.add, ALU.min,
                    [pool.buf(gbc[h]), self.tok], [pool.buf(d_)])
        self.act(af, af, AF.Exp, [pool.buf(a_)], [pool.buf(a_)], scale=-1.0)
        self.act(df, df, AF.Exp, [pool.buf(d_)], [pool.buf(d_)])
        for h in range(4):
            hs = slice(h * 128, (h + 1) * 128)
            self.stt(af[:, hs], af[:, hs], tk(1, c, h), self.consts.h[:, C_MSL:C_MSL + 128], ALU.mult, ALU.mult,
                     [pool.buf(a_), self.tok, self.consts], [pool.buf(a_)])
        for h in range(4):
            hs = slice(h * 128, (h + 1) * 128)
            self.tt("dve", df[:, hs], df[:, hs], self.consts.h[:, C_MIU:C_MIU + 128], ALU.mult,
                    [pool.buf(d_), self.consts], [pool.buf(d_)])
        self.tt("dve", af, af, kk_ps.h[:, 0:512], ALU.mult, [pool.buf(a_), kk_ps], [pool.buf(a_)])
        at_ = pool.get()
        atb = pool.bf(at_)[:, 0:512]
        self.tt("dve", atb, df, kq_ps.h[:, 0:512], ALU.mult, [pool.buf(d_), kq_ps], [pool.buf(at_)])
        pool.put(d_)
        u_ = pool.get()
        ut_ps = self.next_ps()
        P.op("pe", [self.trn(ut_ps.h[:, h * 128:(h + 1) * 128], af[:, h * 128:(h + 1) * 128], ident) for h in range(4)],
             [pool.buf(a_), self.consts], [ut_ps])
        self.cp("act", pool.f32(u_), ut_ps.h[:, 0:512], [ut_ps], [pool.buf(u_)])
        r_ = tri_inverse_T(self, a_, u_)
        rf = pool.f32(r_)
        kt_ps = self.next_ps()
        ktb = kt_ps.h[:].bitcast(BF16)
        P.op("pe", [self.trn(ktb[:, h * 128:(h + 1) * 128], pool.bf(qk[h])[:, ks], self.ident_b.h[:]) for h in range(4)],
             [pool.buf(qk[h]) for h in range(4)] + [self.ident_b], [kt_ps])
        vt_ps = self.next_ps()
        vtb = vt_ps.h[:].bitcast(BF16)
        P.op("pe", [self.trn(vtb[:, h * 128:(h + 1) * 128], pool.bf(vq[h])[:, cs], self.ident_b.h[:]) for h in range(4)],
             [pool.buf(vq[h]) for h in range(4)] + [self.ident_b], [vt_ps])
        vb_, kg_, kd_ = pool.get(), pool.get(), pool.get()
        for h in range(4):
            hs = slice(h * 128, (h + 1) * 128)
            self.ts("dve", pool.f32(vb_)[:, hs], vtb[:, hs], tk(1, c, h), None, ALU.mult, None, [vt_ps, self.tok], [pool.buf(vb_)])
            self.ts("dve", pool.f32(kg_)[:, hs], ktb[:, hs], tk(2, c, h), None, ALU.mult, None, [kt_ps, self.tok], [pool.buf(kg_)])
            self.ts("dve", pool.bf(kd_)[:, hs], ktb[:, hs], tk(3, c, h), None, ALU.mult, None, [kt_ps, self.tok], [pool.buf(kd_)])
        u_ps = self.next_ps()
        P.op("pe", [self.mm(u_ps.h[:, h * 128:(h + 1) * 128], rf[:, h * 128:(h + 1) * 128], pool.f32(vb_)[:, h * 128:(h + 1) * 128],
                            True, True) for h in range(4)], [pool.buf(r_), pool.buf(vb_)], [u_ps])
        w_ps = self.next_ps()
        P.op("pe", [self.mm(w_ps.h[:, h * 128:(h + 1) * 128], pool.f32(kg_)[:, h * 128:(h + 1) * 128], rf[:, h * 128:(h + 1) * 128],
                            True, True) for h in range(4)], [pool.buf(r_), pool.buf(kg_)], [w_ps])
        self.cp("act", pool.f32(vb_), u_ps.h[:, 0:512], [u_ps], [pool.buf(vb_)])
        self.cp("dve", pool.bf(kg_)[:, 0:512], w_ps.h[:, 0:512], [w_ps], [pool.buf(kg_)])
        pool.put(r_)
        uf = pool.f32(vb_)
        wtb = pool.bf(kg_)[:, 0:512]
        kdb = pool.bf(kd_)[:, 0:512]
        y_ = pool.get()
        yb = pool.bf(y_)[:, 0:512]
        vn_ = pool.get()
        ga, gb = gz(c)
        vnb = pool.bf(vn_)[:, 0:512]
        H4 = [slice(h * 128, (h + 1) * 128) for h in range(4)]
        ws_ps = self.next_ps()
        P.op("pe", [self.mm(ws_ps.h[:, H4[h]], wtb[:, H4[h]], Sb.h[:, h, :], True, True) for h in range(4)],
             [pool.buf(kg_)] + Sb.bufs, [ws_ps])
        self.tt("dve", vnb, uf, ws_ps.h[:, 0:512], ALU.subtract, [pool.buf(vb_), ws_ps], [pool.buf(vn_)])
        o_ps = self.next_ps()
        fns = []
        for h in range(4):
            fns.append(self.mm(o_ps.h[:, H4[h]], pool.bf(vq[h])[:, ks], Sb.h[:, h, :], True, False))
            fns.append(self.mm(o_ps.h[:, H4[h]], atb[:, H4[h]], vnb[:, H4[h]], False, True))
        P.op("pe", fns, [pool.buf(vq[h]) for h in range(4)] + Sb.bufs + [pool.buf(at_), pool.buf(vn_)], [o_ps])
        up_ps = self.next_ps()
        P.op("pe", [self.mm(up_ps.h[:, H4[h]], kdb[:, H4[h]], vnb[:, H4[h]], True, True) for h in range(4)],
             [pool.buf(kd_), pool.buf(vn_)], [up_ps])
        eg, egb = self.scal4()
        for h in range(4):
            self.act(eg[:, h:h + 1], pool.f32(gbc[h])[:, c * 128 + 127:c * 128 + 128], AF.Exp, [pool.buf(gbc[h])], [egb])
        for h in range(4):
            self.stt(S.h[:, h, :], S.h[:, h, :], eg[:, h:h + 1], up_ps.h[:, H4[h]], ALU.mult, ALU.add,
                     [S.bufs[h], egb, up_ps], [S.bufs[h]])
        self.cp("act", Sb.h[:], S.h[:], S.bufs, Sb.bufs)
        ss, ssb = self.scal4()
        j_ = pool.get()
        for h in range(4):
            self.act(pool.f32(j_)[:, H4[h]], o_ps.h[:, H4[h]], AF.Square, [o_ps], [pool.buf(j_), ssb], accum_out=ss[:, h:h + 1])
        pool.put(j_)
        self.act(ss, ss, AF.Sqrt, [ssb], [ssb], bias=NORM_EPS, scale=1.0 / 128)
        P.op("dve", lambda e, ss=ss: e.reciprocal(out=ss, in_=ss), [ssb], [ssb])
        for h in range(4):
            self.stt(yb[:, H4[h]], o_ps.h[:, H4[h]], ss[:, h:h + 1], ga[:, H4[h]], ALU.mult, ALU.mult, [o_ps, ssb, gb], [pool.buf(y_)])
        yt_ps = self.next_ps()
        ytb = yt_ps.h[:].bitcast(BF16)
        P.op("pe", [self.trn(ytb[:, h * 128:(h + 1) * 128], yb[:, h * 128:(h + 1) * 128], self.ident_b.h[:]) for h in range(4)],
             [pool.buf(y_), self.ident_b], [yt_ps])
        self.cp("act", self.mixT.h[:, 0:4, cs], ytb[:, 0:512].rearrange("p (h c) -> p h c", h=4), [yt_ps],
                [self.mixT.bufs[h] for h in range(4)])
        pool.put(at_, vb_, kg_, kd_, y_, vn_)
    pool.put(*gbc)
    pool.put(*gzs)
    pool.put(*qk)
    pool.put(*vq)


Kern.gdn = gdn

RW_LW = 0.6065306597126334


def setup_rwkv(self):
    P, cfg = self.P, self.cfg
    L = cfg.depth
    self.rw_M = [P.sb("rw_M%d" % l, [128, 4, 64], F32, nbufs=4) for l in range(L)]
    self.rw_Mb = [P.sb("rw_Mb%d" % l, [128, 4, 64], BF16, nbufs=4) for l in range(L)]
    self.rw_halo = [P.sb("rw_halo%d" % l, [128, 14], F32, nbufs=14) for l in range(L)]
    self.rw_sc = [P.sb("rw_sc%d" % l, [128, 4], F32) for l in range(L)]
    self.rw_w2p = P.sb("rw_w2p", [128, 512], BF16)
    self.rw_a2p = P.sb("rw_a2p", [128, 512], BF16)
    self.rw_g2 = P.sb("rw_g2", [128, 512], BF16)
    self.rowb = P.sb("rowb", [128, 512], F32)
    self.blk_b = P.sb("blk_b", [128, 128], BF16)
    self.sel2_b = P.sb("sel2_b", [128, 2], BF16)
    self.rkd = P.sb("rkd", [128, 4, 8], F32, nbufs=4)
    self.gC = P.sb("gC", [128, 4], F32)
    P.op("pool", lambda e: e.memset(self.rw_w2p.h[:], 0.0), [], [self.rw_w2p])
    P.op("pool", lambda e: e.memset(self.rw_a2p.h[:], 0.0), [], [self.rw_a2p])
    P.op("pool", lambda e: e.memset(self.blk_b.h[:], 0.0), [], [self.blk_b])
    P.op("pool", lambda e: e.memset(self.blk_b.h[0:64, 0:64], 1.0), [], [self.blk_b])
    P.op("pool", lambda e: e.memset(self.blk_b.h[64:128, 64:128], 1.0), [], [self.blk_b])
    self.cp("dve", self.sel2_b.h[:], self.consts.h[:, C_SEL2:C_SEL2 + 2], [self.consts], [self.sel2_b])
    for l in range(L):
        P.op("pool", lambda e, l=l: e.memset(self.rw_M[l].h[:], 0.0), [], self.rw_M[l].bufs)
        P.op("pool", lambda e, l=l: e.memset(self.rw_Mb[l].h[:], 0.0), [], self.rw_Mb[l].bufs)
        P.op("pool", lambda e, l=l: e.memset(self.rw_halo[l].h[:], 0.0), [], self.rw_halo[l].bufs)
        ka = self.pp[l].h[:, PP_OFF["rwkv_k_a"]:PP_OFF["rwkv_k_a"] + 4]
        self.ts("dve", self.rw_sc[l].h[:], ka, -1.0, 1.0, ALU.mult, ALU.add, [self.pp[l]], [self.rw_sc[l]])


def shift_lerp(self, l, ps, ci):
    pool = self.pool
    TT = self.cfg.TT
    cb = self.cbuf[self.cb_rr % 2]
    self.cb_rr += 1
    hl = self.rw_halo[l]
    self.cp("act", cb.h[:, 1:1 + TT], ps.h[:, 0:TT], [ps], [cb])
    self.cp("dve", cb.h[:, 0:1], hl.h[:, ci:ci + 1], [hl.bufs[ci]], [cb])
    o = pool.get()
    of = pool.f32(o)[:, 0:TT]
    self.tt("dve", of, cb.h[:, 0:TT], cb.h[:, 1:1 + TT], ALU.subtract, [cb], [pool.buf(o)])
    self.stt(of, of, self.ppc(l, "rwkv_mu", ci), cb.h[:, 1:1 + TT], ALU.mult, ALU.add, [pool.buf(o), self.pp[l], cb], [pool.buf(o)])
    self.cp("dve", hl.h[:, ci:ci + 1], cb.h[:, TT:TT + 1], [cb], [hl.bufs[ci]])
    return o


def tri_inverse_bf(self, a_, u_, n=2):
    P, pool = self.P, self.pool
    W_ = n * 128
    ident = self.consts.h[:, C_IDENT:C_IDENT + 128]
    r_ = pool.get()
    rf = pool.f32(r_)[:, 0:W_]
    rb = pool.bf(r_)[:, 512:512 + W_]
    for h in range(n):
        self.tt("dve", rf[:, h * 128:(h + 1) * 128], ident, pool.bf(u_)[:, h * 128:(h + 1) * 128], ALU.subtract,
                [self.consts, pool.buf(u_)], [pool.buf(r_)])
    self.cp("act", rb, rf, [pool.buf(r_)], [pool.buf(r_)])
    p_, q_ = u_, a_
    for k in range(1, 7):
        last = k == 6
        q2 = pool.get()
        psq = self.next_ps()
        P.op("pe", [self.mm(psq.h[:, h * 128:(h + 1) * 128], pool.bf(p_)[:, h * 128:(h + 1) * 128],
                            pool.bf(q_)[:, h * 128:(h + 1) * 128], True, True) for h in range(n)],
             [pool.buf(p_), pool.buf(q_)], [psq])
        self.cp("act", pool.bf(q2)[:, 0:W_], psq.h[:, 0:W_], [psq], [pool.buf(q2)])
        if not last:
            p2 = pool.get()
            psp = self.next_ps()
            P.op("pe", [self.mm(psp.h[:, h * 128:(h + 1) * 128], pool.bf(q_)[:, h * 128:(h + 1) * 128],
                                pool.bf(p_)[:, h * 128:(h + 1) * 128], True, True) for h in range(n)],
                 [pool.buf(p_), pool.buf(q_)], [psp])
            self.cp("dve", pool.bf(p2)[:, 0:W_], psp.h[:, 0:W_], [psp], [pool.buf(p2)])
        psr = self.next_ps()
        P.op("pe", [self.mm(psr.h[:, h * 128:(h + 1) * 128], pool.bf(q2)[:, h * 128:(h + 1) * 128],
                            rb[:, h * 128:(h + 1) * 128], True, True) for h in range(n)],
             [pool.buf(q2), pool.buf(r_)], [psr])
        self.tt("dve", rf, rf, psr.h[:, 0:W_], ALU.add, [pool.buf(r_), psr], [pool.buf(r_)])
        self.cp("act", rb, rf, [pool.buf(r_)], [pool.buf(r_)])
        pool.put(p_, q_)
        if not last:
            p_, q_ = p2, q2
        else:
            pool.put(q2)
    return r_


def rwkv(self, l, t):
    P, cfg, pool = self.P, self.cfg, self.pool
    TT = cfg.TT
    NCH = TT // 128
    pl = self.pp[l]
    M, Mb = self.rw_M[l], self.rw_Mb[l]
    ident = self.consts.h[:, C_IDENT:C_IDENT + 128]
    msl = self.consts.h[:, C_MSL:C_MSL + 128]
    msu = self.consts.h[:, C_MSU:C_MSU + 128]
    miu = self.consts.h[:, C_MIU:C_MIU + 128]
    P.dma("pool", self.rw_w2p.h[0:64, :], self.rw_w2_d[l], writes=[self.rw_w2p])
    P.dma("pool", self.rw_a2p.h[64:128, :], self.rw_a2_d[l], writes=[self.rw_a2p])
    P.dma("pool", self.rw_g2.h[:], self.rw_g2_d[l], writes=[self.rw_g2])
    self.load_rowbcast(self.rown, 0, self.rb_d[l:l + 1, RB_OFF["rwkv_ln_w"]:RB_OFF["rwkv_ln_w"] + 512])
    self.load_rowbcast(self.rowb, 0, self.rb_d[l:l + 1, RB_OFF["rwkv_ln_b"]:RB_OFF["rwkv_ln_b"] + 512])
    wl = self.load_w(self.w_in_d[l, :, RW0 + 1536:RW0 + 1792], ncols=256)
    ps = self.inproj_fm(wl, 0, 128)
    lo = shift_lerp(self, l, ps, 12)
    lo_ = pool.get()
    self.act(pool.bf(lo_)[:, 0:TT], pool.f32(lo)[:, 0:TT], AF.Tanh, [pool.buf(lo)], [pool.buf(lo_)])
    self.cp("dve", pool.bf(lo_)[:, 512:512 + TT], pool.f32(lo)[:, 0:TT], [pool.buf(lo)], [pool.buf(lo_)])
    pool.put(lo)
    ps = self.inproj_fm(wl, 128, 128)
    go = shift_lerp(self, l, ps, 13)
    sg_ = pool.get()
    self.act(pool.bf(sg_)[:, 0:TT], pool.f32(go)[:, 0:TT], AF.Sigmoid, [pool.buf(go)], [pool.buf(sg_)])
    pool.put(go)
    wr = self.load_w(self.w_in_d[l, :, RW0:RW0 + 512])
    wk = self.load_w(self.w_in_d[l, :, RW0 + 512:RW0 + 1024])
    wv = self.load_w(self.w_in_d[l, :, RW0 + 1024:RW0 + 1536])
    ytok = [pool.get() for _ in range(NCH)]
    vtk = [pool.get() for _ in range(NCH // 2)]

    def vtok(b):
        return pool.bf(vtk[b // 2])[:, (b % 2) * 512:(b % 2) * 512 + 512], pool.buf(vtk[b // 2])
    for cp in range(4):
        pcs = slice(cp * 128, (cp + 1) * 128)
        ps = self.inproj_fm(wr, cp * 128, 128)
        r_ = shift_lerp(self, l, ps, cp)
        ps = self.inproj_fm(wk, cp * 128, 128)
        k_ = shift_lerp(self, l, ps, 4 + cp)
        ps = self.inproj_fm(wv, cp * 128, 128)
        v_ = shift_lerp(self, l, ps, 8 + cp)
        rf, kf, vf = [pool.f32(s)[:, 0:TT] for s in (r_, k_, v_)]
        vk_ = pool.get()
        self.cp("act", pool.bf(vk_)[:, 0:TT], vf, [pool.buf(v_)], [pool.buf(vk_)])
        pool.put(v_)
        ps_w = self.next_ps()
        P.op("pe", [self.mm(ps_w.h[:, 0:TT], self.rw_w2p.h[:, pcs], pool.bf(lo_)[:, 0:TT], True, True)],
             [self.rw_w2p, pool.buf(lo_)], [ps_w])
        sw_ = pool.get()
        swf = pool.f32(sw_)[:, 0:TT]
        self.act(swf, ps_w.h[:, 0:TT], AF.Sigmoid, [ps_w, pl], [pool.buf(sw_)], bias=self.ppc(l, "rwkv_w0", cp))
        ps_a = self.next_ps()
        P.op("pe", [self.mm(ps_a.h[:, 0:TT], self.rw_a2p.h[:, pcs], pool.bf(lo_)[:, 512:512 + TT], True, True)],
             [self.rw_a2p, pool.buf(lo_)], [ps_a])
        a_ = pool.get()
        af = pool.f32(a_)[:, 0:TT]
        self.act(af, ps_a.h[:, 0:TT], AF.Sigmoid, [ps_a, pl], [pool.buf(a_)], bias=self.ppc(l, "rwkv_a0", cp))
        kk_ = pool.get()
        kkf = pool.f32(kk_)[:, 0:TT]
        self.ts("dve", kkf, kf, self.ppc(l, "rwkv_k_k", cp), None, ALU.mult, None, [pool.buf(k_), pl], [pool.buf(kk_)])
        sq = pool.get()
        self.act(pool.bf(sq)[:, 0:TT], kkf, AF.Square, [pool.buf(kk_)], [pool.buf(sq)])
        pss = self.next_ps()
        P.op("pe", [self.mm(pss.h[:, 0:TT], self.blk_b.h[:], pool.bf(sq)[:, 0:TT], True, True)], [self.blk_b, pool.buf(sq)], [pss])
        rn = pool.f32(sq)[:, 0:TT]
        self.act(rn, pss.h[:, 0:TT], AF.Sqrt, [pss, pool.buf(sq)], [pool.buf(sq)], bias=1e-6)
        P.op("dve", lambda e, rn=rn: e.reciprocal(out=rn, in_=rn), [pool.buf(sq)], [pool.buf(sq)])
        self.tt("dve", kkf, kkf, rn, ALU.mult, [pool.buf(kk_), pool.buf(sq)], [pool.buf(kk_)])
        fac = pool.f32(sq)[:, 0:TT]
        self.ts("dve", fac, af, self.ppc(l, "rwkv_k_a", cp), self.rw_sc[l].h[:, cp:cp + 1], ALU.mult, ALU.add,
                [pool.buf(a_), pl, self.rw_sc[l], pool.buf(sq)], [pool.buf(sq)])
        self.tt("dve", kf, kf, fac, ALU.mult, [pool.buf(k_), pool.buf(sq)], [pool.buf(k_)])
        self.stt(pool.bf(vk_)[:, 512:512 + TT], rf, self.ppc(l, "rwkv_r_k", cp), kf, ALU.mult, ALU.mult,
                 [pool.buf(r_), pl, pool.buf(k_)], [pool.buf(vk_)])
        self.tt("dve", af, af, kkf, ALU.mult, [pool.buf(a_), pool.buf(kk_)], [pool.buf(a_)])
        cs_ = pool.get()
        csf = pool.f32(cs_)[:, 0:TT]
        cm = self.consts.h[:, C_CMASK:C_CMASK + TT]
        P.op("dve", lambda e, csf=csf, cm=cm, swf=swf: e.tensor_tensor_scan(out=csf, data0=cm, data1=swf, initial=0.0,
                                                                               op0=ALU.mult, op1=ALU.add),
             [self.consts, pool.buf(sw_)], [pool.buf(cs_)])
        self.tt("dve", swf, csf, swf, ALU.subtract, [pool.buf(cs_), pool.buf(sw_)], [pool.buf(sw_)])
        ex = pool.f32(sq)[:, 0:TT]
        for b in range(NCH):
            self.act(self.gC.h[:, b:b + 1], csf[:, b * 128 + 127:b * 128 + 128], AF.Exp, [pool.buf(cs_)], [self.gC], scale=-RW_LW)
        ar_ = [pool.get(), pool.get()]
        for e in range(2):
            zap = pool.bf(ar_[e])[(1 - e) * 64:(2 - e) * 64, :]
            P.op("pool", lambda en, zap=zap: en.memset(zap, 0.0), [], [pool.buf(ar_[e])])
        self.act(ex, swf, AF.Exp, [pool.buf(sw_), pool.buf(sq)], [pool.buf(sq)], scale=-RW_LW)
        for e in range(2):
            hp = slice(e * 64, (e + 1) * 64)
            self.stt(pool.bf(ar_[e])[hp, 0:TT], kkf[hp], -1.0, ex[hp], ALU.mult, ALU.mult,
                     [pool.buf(kk_), pool.buf(sq)], [pool.buf(ar_[e])])
        self.act(ex, csf, AF.Exp, [pool.buf(cs_), pool.buf(sq)], [pool.buf(sq)], scale=-RW_LW)
        for e in range(2):
            hp = slice(e * 64, (e + 1) * 64)
            self.tt("dve", pool.bf(ar_[e])[hp, 512:512 + TT], rf[hp], ex[hp], ALU.mult,
                    [pool.buf(r_), pool.buf(sq)], [pool.buf(ar_[e])])
        self.act(ex, csf, AF.Exp, [pool.buf(cs_), pool.buf(sq)], [pool.buf(sq)], scale=RW_LW)
        bk_ = pool.get()
        self.tt("dve", pool.bf(bk_)[:, 0:TT], af, ex, ALU.mult, [pool.buf(a_), pool.buf(sq)], [pool.buf(bk_)])
        self.tt("dve", pool.bf(bk_)[:, 512:512 + TT], kf, ex, ALU.mult, [pool.buf(k_), pool.buf(sq)], [pool.buf(bk_)])
        pool.put(r_, k_, sw_, a_, kk_, sq, cs_)
        bt = pool.bf(bk_)[:, 0:TT]
        kt = pool.bf(bk_)[:, 512:512 + TT]
        for b in range(NCH):
            bs = slice(b * 128, (b + 1) * 128)
            at = [pool.bf(ar_[e])[:, b * 128:(b + 1) * 128] for e in range(2)]
            rt = [pool.bf(ar_[e])[:, 512 + b * 128:512 + (b + 1) * 128] for e in range(2)]
            arb = [pool.buf(ar_[0]), pool.buf(ar_[1])]
            bkb = pool.buf(bk_)
            l_ps, u_ps, ak_ps, rb_ps, rk_ps = [self.next_ps() for _ in range(5)]
            P.op("pe", [self.mm(l_ps.h[:, e * 128:(e + 1) * 128], at[e], bt[:, bs], True, True) for e in range(2)], arb + [bkb], [l_ps])
            P.op("pe", [self.mm(u_ps.h[:, e * 128:(e + 1) * 128], bt[:, bs], at[e], True, True) for e in range(2)], arb + [bkb], [u_ps])
            P.op("pe", [self.mm(ak_ps.h[:, e * 128:(e + 1) * 128], kt[:, bs], at[e], True, True) for e in range(2)], arb + [bkb], [ak_ps])
            P.op("pe", [self.mm(rb_ps.h[:, e * 128:(e + 1) * 128], bt[:, bs], rt[e], True, True) for e in range(2)], arb + [bkb], [rb_ps])
            P.op("pe", [self.mm(rk_ps.h[:, e * 128:(e + 1) * 128], kt[:, bs], rt[e], True, True) for e in range(2)], arb + [bkb], [rk_ps])
            la_, ua_, m3_ = pool.get(), pool.get(), pool.get()
            m3 = pool.bf(m3_)
            for e in range(2):
                es = slice(e * 128, (e + 1) * 128)
                self.stt(pool.bf(la_)[:, es], l_ps.h[:, es], -1.0, msl, ALU.mult, ALU.mult, [l_ps, self.consts], [pool.buf(la_)])
                self.stt(pool.bf(ua_)[:, es], u_ps.h[:, es], -1.0, msu, ALU.mult, ALU.mult, [u_ps, self.consts], [pool.buf(ua_)])
                self.tt("dve", m3[:, e * 128:(e + 1) * 128], ak_ps.h[:, es], msu, ALU.mult, [ak_ps, self.consts], [pool.buf(m3_)])
                self.tt("dve", m3[:, 256 + e * 128:256 + (e + 1) * 128], rb_ps.h[:, es], miu, ALU.mult, [rb_ps, self.consts], [pool.buf(m3_)])
                self.tt("dve", m3[:, 512 + e * 128:512 + (e + 1) * 128], rk_ps.h[:, es], miu, ALU.mult, [rk_ps, self.consts], [pool.buf(m3_)])
            r2_ = tri_inverse_bf(self, la_, ua_, n=2)
            tr_ps = self.next_ps()
            trb = tr_ps.h[:].bitcast(BF16)
            P.op("pe", [self.trn(trb[:, 0:128], bt[:, bs], self.ident_b.h[:]),
                        self.trn(trb[:, 128:256], kt[:, bs], self.ident_b.h[:]),
                        self.trn(trb[:, 256:384], pool.bf(vk_)[:, bs], self.ident_b.h[:])],
                 [bkb, pool.buf(vk_), self.ident_b], [tr_ps])
            tk_ = pool.get()
            tkb = pool.bf(tk_)
            self.cp("act", tkb[:, 0:384], trb[:, 0:384], [tr_ps], [pool.buf(tk_)])
            va, vbuf = vtok(b)
            self.cp("dve", va[:, pcs], trb[:, 256:384], [tr_ps], [vbuf])
            rk2 = self.next_ps()
            P.op("pe", [self.mm(rk2.h[:, 0:2], pool.bf(vk_)[:, 512 + b * 128:512 + (b + 1) * 128], self.sel2_b.h[:], True, True)],
                 [pool.buf(vk_), self.sel2_b], [rk2])
            self.cp("dve", self.rkd.h[:, b, cp * 2:cp * 2 + 2], rk2.h[:, 0:2], [rk2], [self.rkd.bufs[b]])
            x_ps = self.next_ps()
            fns = []
            for e in range(2):
                es64 = slice(e * 64, (e + 1) * 64)
                fns.append(self.mm(x_ps.h[:, es64], at[e], Mb.h[:, cp, :], True, False))
                fns.append(self.mm(x_ps.h[:, es64], m3[:, e * 128:(e + 1) * 128], tkb[:, 256 + e * 64:256 + (e + 1) * 64], False, True))
            P.op("pe", fns, arb + [Mb.bufs[cp], pool.buf(m3_), pool.buf(tk_)], [x_ps])
            x_ = pool.get()
            xf = pool.bf(x_)[:, 0:128]
            self.cp("act", xf, x_ps.h[:, 0:128], [x_ps], [pool.buf(x_)])
            p_ps = self.next_ps()
            P.op("pe", [self.mm(p_ps.h[:, e * 64:(e + 1) * 64], pool.bf(r2_)[:, 512 + e * 128:512 + (e + 1) * 128], xf[:, e * 64:(e + 1) * 64], True, True)
                        for e in range(2)], [pool.buf(r2_), pool.buf(x_)], [p_ps])
            self.cp("act", tkb[:, 384:512], p_ps.h[:, 0:128], [p_ps], [pool.buf(tk_)])
            pool.put(r2_, x_)
            y_ps = self.next_ps()
            fns = []
            for e in range(2):
                es64 = slice(e * 64, (e + 1) * 64)
                fns.append(self.mm(y_ps.h[:, es64], rt[e], Mb.h[:, cp, :], True, False))
                fns.append(self.mm(y_ps.h[:, es64], m3[:, 256 + e * 128:256 + (e + 1) * 128], tkb[:, 384 + e * 64:384 + (e + 1) * 64], False, False))
                fns.append(self.mm(y_ps.h[:, es64], m3[:, 512 + e * 128:512 + (e + 1) * 128], tkb[:, 256 + e * 64:256 + (e + 1) * 64], False, True))
            P.op("pe", fns, arb + [Mb.bufs[cp], pool.buf(m3_), pool.buf(tk_)], [y_ps])
            self.cp("act", pool.f32(ytok[b])[:, pcs], y_ps.h[:, 0:128], [y_ps], [pool.buf(ytok[b])])
            m_ps = self.next_ps()
            fns = []
            for e in range(2):
                es64 = slice(e * 64, (e + 1) * 64)
                fns.append(self.mm(m_ps.h[:, es64], tkb[:, 0:128], tkb[:, 384 + e * 64:384 + (e + 1) * 64], True, False))
                fns.append(self.mm(m_ps.h[:, es64], tkb[:, 128:256], tkb[:, 256 + e * 64:256 + (e + 1) * 64], False, True))
            P.op("pe", fns, [pool.buf(tk_)], [m_ps])
            for e in range(2):
                es64 = slice(e * 64, (e + 1) * 64)
                self.tt("dve", M.h[es64, cp, :], M.h[es64, cp, :], m_ps.h[es64, e * 64:(e + 1) * 64], ALU.add,
                        [M.bufs[cp], m_ps], [M.bufs[cp]])
            self.ts("dve", M.h[:, cp, :], M.h[:, cp, :], self.gC.h[:, b:b + 1], None, ALU.mult, None, [M.bufs[cp], self.gC], [M.bufs[cp]])
            self.cp("act", Mb.h[:, cp, :], M.h[:, cp, :], [M.bufs[cp]], [Mb.bufs[cp]])
            pool.put(m3_, tk_)
        pool.put(vk_, bk_, *ar_)
    pool.put(lo_)
    for b in range(NCH):
        bs = slice(b * 128, (b + 1) * 128)
        yf = pool.f32(ytok[b])
        y3 = yf.rearrange("p (h i) -> p h i", h=8)
        st, stb = self.scal8()
        mean, ex2 = st[:, 0:8], st[:, 8:16]
        P.op("dve", lambda e, mean=mean, y3=y3: e.tensor_reduce(out=mean, in_=y3, axis=AX.X, op=ALU.add), [pool.buf(ytok[b])], [stb])
        t_ = pool.get()
        tf = pool.f32(t_)
        t3 = tf.rearrange("p (h i) -> p h i", h=8)
        self.act(tf, yf, AF.Square, [pool.buf(ytok[b])], [pool.buf(t_)])
        P.op("dve", lambda e, ex2=ex2, t3=t3: e.tensor_reduce(out=ex2, in_=t3, axis=AX.X, op=ALU.add), [pool.buf(t_)], [stb])
        self.ts("dve", mean, mean, 1.0 / 64, None, ALU.mult, None, [stb], [stb])
        self.ts("dve", ex2, ex2, 1.0 / 64, None, ALU.mult, None, [stb], [stb])
        msq = st[:, 16:24]
        self.tt("dve", msq, mean, mean, ALU.mult, [stb], [stb])
        self.tt("dve", ex2, ex2, msq, ALU.subtract, [stb], [stb])
        self.act(ex2, ex2, AF.Sqrt, [stb], [stb], bias=RWKV_LN_EPS)
        P.op("dve", lambda e, ex2=ex2: e.reciprocal(out=ex2, in_=ex2), [stb], [stb])
        for h in range(8):
            hs = slice(h * 64, (h + 1) * 64)
            self.ts("dve", tf[:, hs], yf[:, hs], mean[:, h:h + 1], ex2[:, h:h + 1], ALU.subtract, ALU.mult,
                    [pool.buf(ytok[b]), stb], [pool.buf(t_)])
        self.tt("dve", tf, tf, self.rown.h[:, 0:512], ALU.mult, [pool.buf(t_), self.rown], [pool.buf(t_)])
        self.tt("dve", tf, tf, self.rowb.h[:, 0:512], ALU.add, [pool.buf(t_), self.rowb], [pool.buf(t_)])
        va, vbuf = vtok(b)
        for h in range(8):
            hs = slice(h * 64, (h + 1) * 64)
            self.stt(tf[:, hs], va[:, hs], self.rkd.h[:, b, h:h + 1], tf[:, hs], ALU.mult, ALU.add,
                     [vbuf, self.rkd.bufs[b], pool.buf(t_)], [pool.buf(t_)])
        g_ps = self.next_ps()
        P.op("pe", [self.mm(g_ps.h[:, 0:512], pool.bf(sg_)[:, bs], self.rw_g2.h[:], True, True)], [pool.buf(sg_), self.rw_g2], [g_ps])
        yb_ = pool.get()
        yb = pool.bf(yb_)[:, 0:512]
        self.tt("dve", yb, tf, g_ps.h[:, 0:512], ALU.mult, [pool.buf(t_), g_ps], [pool.buf(yb_)])
        yt_ps = self.next_ps()
        ytb = yt_ps.h[:].bitcast(BF16)
        P.op("pe", [self.trn(ytb[:, c * 128:(c + 1) * 128], yb[:, c * 128:(c + 1) * 128], self.ident_b.h[:]) for c in range(4)],
             [pool.buf(yb_), self.ident_b], [yt_ps])
        self.cp("act", self.mixT.h[:, 12:16, bs], ytb[:, 0:512].rearrange("p (h c) -> p h c", h=4), [yt_ps],
                [self.mixT.bufs[12 + c] for c in range(4)])
        pool.put(t_, yb_)
    pool.put(sg_, *ytok)
    pool.put(*vtk)


def scal8(self):
    i = self.small_rr % 2
    self.small_rr += 1
    return self.small8.h[:, i * 32:(i + 1) * 32], self.small8.bufs[i]


Kern.rwkv = rwkv
Kern.scal8 = scal8
```
